# Optimizing a Trainium2 kernel written in Bass

```python
import jax, jax.numpy as jnp
from jax import lax
import numpy as np

D_MODEL = 1024
BATCH = 32
SEQ = 2048
DEPTH = 2

GRID_W = 64
CTX_LEN = 256
NORM_EPS = 1e-6
ROPE_BASE = 10000.0
Q_BLOCK = 128

MLA_HEADS = 6
MLA_Q_RANK = 256
MLA_KV_RANK = 128
MLA_NOPE = 64
MLA_ROPE = 32
MLA_V = 64
MLA_WIDTH = MLA_HEADS * MLA_V
MLA_SCALE = (MLA_NOPE + MLA_ROPE) ** -0.5

SWA_HEADS = 6
SWA_KV_HEADS = 2
SWA_REP = SWA_HEADS // SWA_KV_HEADS
SWA_HEAD_DIM = 64
SWA_WINDOW = 128
SWA_WIDTH = SWA_HEADS * SWA_HEAD_DIM
SWA_SCALE = SWA_HEAD_DIM ** -0.5

CONV_CH = 256
CONV_K = 3

MIX_WIDTH = MLA_WIDTH + SWA_WIDTH + CONV_CH
IN_SPLITS = (MLA_Q_RANK, MLA_KV_RANK, MLA_ROPE, SWA_WIDTH, SWA_KV_HEADS * SWA_HEAD_DIM, SWA_KV_HEADS * SWA_HEAD_DIM, CONV_CH, CONV_CH, CONV_CH)
IN_WIDTH = sum(IN_SPLITS)

N_EXPERTS = 16
EXPERT_FF = 512
EC_CAPACITY_FACTOR = 2
N_MOD = 6

kernel_name = 'hybrid_mla_swa_conv_ec_moe_dit'


def rmsnorm(x, g):
    xf = x.astype(jnp.float32)
    y = xf * lax.rsqrt(jnp.mean(xf * xf, axis=-1, keepdims=True) + NORM_EPS)
    return (y * g.astype(jnp.float32)).astype(x.dtype)


def axial_rope(n_tokens, rot_dim, dtype):
    rows = n_tokens // GRID_W
    row = jnp.repeat(jnp.arange(rows, dtype=jnp.float32), GRID_W)
    col = jnp.tile(jnp.arange(GRID_W, dtype=jnp.float32), rows)
    n_freq = rot_dim // 4
    inv = ROPE_BASE ** (-jnp.arange(n_freq, dtype=jnp.float32) / n_freq)
    ang = jnp.concatenate([row[:, None] * inv, col[:, None] * inv], axis=-1)
    return jnp.cos(ang).astype(dtype), jnp.sin(ang).astype(dtype)


def apply_rope(x, cos, sin):
    shape = (cos.shape[0],) + (1,) * (x.ndim - 3) + (cos.shape[1],)
    cos = cos.reshape(shape)
    sin = sin.reshape(shape)
    x1, x2 = jnp.split(x, 2, axis=-1)
    return jnp.concatenate([x1 * cos - x2 * sin, x2 * cos + x1 * sin], axis=-1)


def split_columns(p):
    offs = [int(o) for o in np.cumsum(IN_SPLITS)[:-1]]
    return jnp.split(p, offs, axis=-1)


def to_blocks(t):
    b, n = t.shape[0], t.shape[1]
    return jnp.moveaxis(t.reshape((b, n // Q_BLOCK, Q_BLOCK) + t.shape[2:]), 1, 0)


def from_blocks(o):
    nb, b, qb = o.shape[0], o.shape[1], o.shape[2]
    return jnp.moveaxis(o, 0, 1).reshape(b, nb * qb, -1)


def mla_q(p_q, q_norm_g, w_uq):
    b, t = p_q.shape[0], p_q.shape[1]
    q = (rmsnorm(p_q, q_norm_g) @ w_uq).reshape(b, t, MLA_HEADS, MLA_NOPE + MLA_ROPE)
    return q[..., :MLA_NOPE], q[..., MLA_NOPE:]


def mla_kv(p_kv, kv_norm_g, w_ukv):
    b, t = p_kv.shape[0], p_kv.shape[1]
    kv = (rmsnorm(p_kv, kv_norm_g) @ w_ukv).reshape(b, t, MLA_HEADS, MLA_NOPE + MLA_V)
    return kv[..., :MLA_NOPE], kv[..., MLA_NOPE:]


def mla_block(qn, qr, kn, kr, v):
    s = (jnp.einsum('bqhd,bkhd->bhqk', qn, kn) + jnp.einsum('bqhr,bkr->bhqk', qr, kr)).astype(jnp.float32) * MLA_SCALE
    p = jax.nn.softmax(s, axis=-1).astype(v.dtype)
    return jnp.einsum('bhqk,bkhd->bqhd', p, v)


def mla_latent(qn, qr, kn, kr, v):
    out = lax.map(lambda a: mla_block(a[0], a[1], kn, kr, v), (to_blocks(qn), to_blocks(qr)))
    return from_blocks(out)


def sink_softmax(scores, sink):
    s_sink = jnp.broadcast_to(sink.astype(jnp.float32)[None, :, :, None, None], scores.shape[:-1] + (1,))
    p = jax.nn.softmax(jnp.concatenate([scores, s_sink], axis=-1), axis=-1)
    return p[..., :-1]


def swa_latent(q, k, v, k_ctx, v_ctx, sink):
    s_len = q.shape[1]
    n_ctx = k_ctx.shape[1]
    nb = s_len // Q_BLOCK
    band = Q_BLOCK + 2 * SWA_WINDOW
    pad = ((0, 0), (SWA_WINDOW, SWA_WINDOW), (0, 0), (0, 0))
    kp = jnp.pad(k, pad)
    vp = jnp.pad(v, pad)
    q_off = jnp.arange(Q_BLOCK)
    k_off = jnp.arange(band)

    def block(args):
        i, q_b = args
        start = i * Q_BLOCK
        k_b = lax.dynamic_slice_in_dim(kp, start, band, axis=1)
        v_b = lax.dynamic_slice_in_dim(vp, start, band, axis=1)
        q_pos = start + q_off
        k_pos = start - SWA_WINDOW + k_off
        valid = (jnp.abs(q_pos[:, None] - k_pos[None, :]) <= SWA_WINDOW) & ((k_pos >= 0) & (k_pos < s_len))[None, :]
        s_loc = jnp.einsum('bqgrd,bkgd->bgrqk', q_b, k_b).astype(jnp.float32) * SWA_SCALE
        s_loc = jnp.where(valid, s_loc, -jnp.inf)
        s_ctx = jnp.einsum('bqgrd,bcgd->bgrqc', q_b, k_ctx).astype(jnp.float32) * SWA_SCALE
        p = sink_softmax(jnp.concatenate([s_ctx, s_loc], axis=-1), sink).astype(v.dtype)
        return (jnp.einsum('bgrqc,bcgd->bqgrd', p[..., :n_ctx], v_ctx)
                + jnp.einsum('bgrqk,bkgd->bqgrd', p[..., n_ctx:], v_b))

    out = lax.map(block, (jnp.arange(nb), to_blocks(q)))
    return from_blocks(out)


def swa_context(q, k, v, sink):
    s = jnp.einsum('bqgrd,bcgd->bgrqc', q, k).astype(jnp.float32) * SWA_SCALE
    p = sink_softmax(s, sink).astype(v.dtype)
    o = jnp.einsum('bgrqc,bcgd->bqgrd', p, v)
    return o.reshape(o.shape[0], o.shape[1], SWA_WIDTH)


def short_conv(b_gate, c_gate, u, w):
    z = c_gate * u
    y = lax.conv_general_dilated(z, w[:, None, :].astype(z.dtype), window_strides=(1,),
                                 padding=((CONV_K // 2, CONV_K // 2),),
                                 dimension_numbers=('NWC', 'WIO', 'NWC'),
                                 feature_group_count=CONV_CH)
    return b_gate * y


def ec_ffn(h, router_w, w_gate, w_up, w_down):
    n_tok, d = h.shape[1], h.shape[2]
    cap = EC_CAPACITY_FACTOR * n_tok // N_EXPERTS
    aff = jax.nn.softmax((h @ router_w).astype(jnp.float32), axis=-1)
    top_aff, idx = lax.top_k(jnp.swapaxes(aff, 1, 2), cap)
    xg = jax.vmap(lambda hb, ib: hb[ib])(h, idx)
    hid = jax.nn.silu(jnp.einsum('becd,edf->becf', xg, w_gate)) * jnp.einsum('becd,edf->becf', xg, w_up)
    y = jnp.einsum('becf,efd->becd', hid, w_down) * top_aff[..., None].astype(h.dtype)
    return jax.vmap(lambda yb, ib: jnp.zeros((n_tok, d), yb.dtype).at[ib.reshape(-1)].add(yb.reshape(-1, d)))(y, idx)


def hybrid_layer(xl, xc, c, c_ctx, ada_w, ada_b, norm1_g, w_in, q_norm_g, w_uq, kv_norm_g, w_ukv,
                 sink, conv_w, w_o, norm2_g, router_w, w_gate, w_up, w_down, rope_mla, rope_swa, update_ctx):
    b, s_len = xl.shape[0], xl.shape[1]
    n_ctx = xc.shape[1]
    sh1, sc1, g1, sh2, sc2, g2 = jnp.split((jax.nn.silu(c) @ ada_w + ada_b)[:, None, :], N_MOD, axis=-1)
    csh1, csc1, cg1, csh2, csc2, cg2 = jnp.split(jax.nn.silu(c_ctx) @ ada_w + ada_b, N_MOD, axis=-1)

    hl = rmsnorm(xl, norm1_g) * (1 + sc1) + sh1
    hc = rmsnorm(xc, norm1_g) * (1 + csc1) + csh1
    l_cq, l_ckv, l_kr, l_q, l_k, l_v, l_b, l_c, l_u = split_columns(hl @ w_in)
    c_cq, c_ckv, c_kr, c_q, c_k, c_v, c_b, c_c, c_u = split_columns(hc @ w_in)

    qn_l, qr_l = mla_q(l_cq, q_norm_g, w_uq)
    qr_l = apply_rope(qr_l, *rope_mla)
    kn_l, va_l = mla_kv(l_ckv, kv_norm_g, w_ukv)
    kr_l = apply_rope(l_kr, *rope_mla)
    kn_c, va_c = mla_kv(c_ckv, kv_norm_g, w_ukv)
    a_l = mla_latent(qn_l, qr_l, jnp.concatenate([kn_c, kn_l], axis=1),
                     jnp.concatenate([c_kr, kr_l], axis=1), jnp.concatenate([va_c, va_l], axis=1))

    sink_gr = sink.reshape(SWA_KV_HEADS, SWA_REP)
    q_l = apply_rope(l_q.reshape(b, s_len, SWA_KV_HEADS, SWA_REP, SWA_HEAD_DIM), *rope_swa)
    k_l = apply_rope(l_k.reshape(b, s_len, SWA_KV_HEADS, SWA_HEAD_DIM), *rope_swa)
    v_l = l_v.reshape(b, s_len, SWA_KV_HEADS, SWA_HEAD_DIM)
    k_c = c_k.reshape(b, n_ctx, SWA_KV_HEADS, SWA_HEAD_DIM)
    v_c = c_v.reshape(b, n_ctx, SWA_KV_HEADS, SWA_HEAD_DIM)
    b_l = swa_latent(q_l, k_l, v_l, k_c, v_c, sink_gr)

    cv_l = short_conv(l_b, l_c, l_u, conv_w)

    xl = xl + g1 * (jnp.concatenate([a_l, b_l, cv_l], axis=-1) @ w_o)
    xl = xl + g2 * ec_ffn(rmsnorm(xl, norm2_g) * (1 + sc2) + sh2, router_w, w_gate, w_up, w_down)

    if update_ctx:
        qn_c, qr_c = mla_q(c_cq, q_norm_g, w_uq)
        a_c = mla_block(qn_c, qr_c, kn_c, c_kr, va_c).reshape(b, n_ctx, MLA_WIDTH)
        b_c = swa_context(c_q.reshape(b, n_ctx, SWA_KV_HEADS, SWA_REP, SWA_HEAD_DIM), k_c, v_c, sink_gr)
        cv_c = short_conv(c_b, c_c, c_u, conv_w)
        xc = xc + cg1 * (jnp.concatenate([a_c, b_c, cv_c], axis=-1) @ w_o)
        xc = xc + cg2 * ec_ffn(rmsnorm(xc, norm2_g) * (1 + csc2) + csh2, router_w, w_gate, w_up, w_down)
    return xl, xc


def setup_inputs(seed: int = 0) -> dict:
    key = jax.random.key(seed)
    ks = jax.random.split(key, 21)
    f32 = jnp.float32
    D = D_MODEL

    def nrm(k, shape, scale):
        return jax.random.normal(k, shape, f32) * scale

    def gain(k, shape):
        return 1.0 + 0.02 * jax.random.normal(k, shape, f32)

    return {
        'x': nrm(ks[0], (BATCH, SEQ, D), 1.0),
        'c': nrm(ks[1], (BATCH, D), 1.0),
        'ctx': nrm(ks[2], (BATCH, CTX_LEN, D), 1.0),
        'c_ctx': nrm(ks[3], (D,), 1.0),
        'ada_w': nrm(ks[4], (DEPTH, D, N_MOD * D), 0.5 * D ** -0.5),
        'ada_b': nrm(ks[5], (DEPTH, N_MOD * D), 0.02),
        'norm1_g': gain(ks[6], (DEPTH, D)),
        'w_in': nrm(ks[7], (DEPTH, D, IN_WIDTH), D ** -0.5),
        'mla_q_norm_g': gain(ks[8], (DEPTH, MLA_Q_RANK)),
        'mla_w_uq': nrm(ks[9], (DEPTH, MLA_Q_RANK, MLA_HEADS * (MLA_NOPE + MLA_ROPE)), MLA_Q_RANK ** -0.5),
        'mla_kv_norm_g': gain(ks[10], (DEPTH, MLA_KV_RANK)),
        'mla_w_ukv': nrm(ks[11], (DEPTH, MLA_KV_RANK, MLA_HEADS * (MLA_NOPE + MLA_V)), MLA_KV_RANK ** -0.5),
        'swa_sink': nrm(ks[12], (DEPTH, SWA_HEADS), 0.5),
        'conv_w': nrm(ks[13], (DEPTH, CONV_K, CONV_CH), CONV_K ** -0.5),
        'w_o': nrm(ks[14], (DEPTH, MIX_WIDTH, D), MIX_WIDTH ** -0.5),
        'norm2_g': gain(ks[15], (DEPTH, D)),
        'router_w': nrm(ks[16], (DEPTH, D, N_EXPERTS), D ** -0.5),
        'exp_w_gate': nrm(ks[17], (DEPTH, N_EXPERTS, D, EXPERT_FF), D ** -0.5),
        'exp_w_up': nrm(ks[18], (DEPTH, N_EXPERTS, D, EXPERT_FF), D ** -0.5),
        'exp_w_down': nrm(ks[19], (DEPTH, N_EXPERTS, EXPERT_FF, D), EXPERT_FF ** -0.5),
        'final_norm_g': gain(ks[20], (D,)),
    }


def reference(x, c, ctx, c_ctx, ada_w, ada_b, norm1_g, w_in, mla_q_norm_g, mla_w_uq, mla_kv_norm_g,
              mla_w_ukv, swa_sink, conv_w, w_o, norm2_g, router_w, exp_w_gate, exp_w_up, exp_w_down,
              final_norm_g):
    n_lat = x.shape[1]
    rope_mla = axial_rope(n_lat, MLA_ROPE, x.dtype)
    rope_swa = axial_rope(n_lat, SWA_HEAD_DIM, x.dtype)
    xl, xc = x, ctx
    for li in range(DEPTH):
        xl, xc = hybrid_layer(xl, xc, c, c_ctx, ada_w[li], ada_b[li], norm1_g[li], w_in[li],
                              mla_q_norm_g[li], mla_w_uq[li], mla_kv_norm_g[li], mla_w_ukv[li],
                              swa_sink[li], conv_w[li], w_o[li], norm2_g[li], router_w[li],
                              exp_w_gate[li], exp_w_up[li], exp_w_down[li], rope_mla, rope_swa,
                              li < DEPTH - 1)
    return rmsnorm(xl, final_norm_g)
```

```python
import math
from contextlib import ExitStack
import numpy as np
import concourse.bass as bass
import concourse.mybir as mybir
from concourse.bass_utils import run_bass_kernel_spmd

F32 = mybir.dt.float32
BF = mybir.dt.bfloat16
AF = mybir.ActivationFunctionType
ALU = mybir.AluOpType
AX = mybir.AxisListType

D = 1024
SEQ = 2048
CTX = 256
NT = 18
NTOK = NT * 128
EPS = 1e-6
MLA_SCALE = 96 ** -0.5
SWA_SCALE = 64 ** -0.5
NE = 16
NBIS = 22
N_CORES = 8

EP = 60000
EPD = 3900
NDMA = 8


_POS = {"memset": ("ap", "constant"), "matmul": ("out", "lhsT", "rhs"), "transpose": ("out", "in_", "identity")}


def _R(meth, *a, **k):
    for nm, v in zip(_POS.get(meth, ()), a):
        k[nm] = v
    return (meth, k)


class Prog:
    def __init__(self):
        self.streams = {e: [] for e in ("pe", "act", "dve", "pool", "sp")}
        self.count = {}
        self.lastw = {}
        self.readers = {}
        self.seen = {e: {} for e in self.streams}
        self.waited = {}
        self.rr = 0
        self.rrq = {}

    def _collect(self, eng, r, w, is_dma):
        deps = {}

        def add(t, same_ok):
            if t is None:
                return
            g, i = t
            if g == eng and not same_ok:
                return
            if deps.get(g, 0) < i:
                deps[g] = i

        for x in r:
            add(self.lastw.get(x), is_dma or eng != "pe")
        for x in w:
            add(self.lastw.get(x), is_dma)
            for g, i in self.readers.get(x, {}).items():
                add((g, i), is_dma)
        return deps

    def _waits(self, eng, deps):
        waits = []
        for g, i in deps.items():
            if self.seen[eng].get(g, 0) >= i:
                continue
            self.seen[eng][g] = i
            waits.append((g, i))
            self.waited.setdefault(g, set()).add(i)
        return waits

    def _mark(self, t, r, w):
        for x in r:
            d = self.readers.setdefault(x, {})
            if d.get(t[0], 0) < t[1]:
                d[t[0]] = t[1]
        for x in w:
            self.lastw[x] = t
            self.readers[x] = {}

    limit = None
    nrec = 0

    def _over(self):
        self.nrec += 1
        return self.limit is not None and self.nrec > self.limit

    def op(self, eng, fn, r=(), w=()):
        if self._over():
            return
        deps = self._collect(eng, r, w, False)
        waits = self._waits(eng, deps)
        n = self.count.get(eng, 0) + 1
        self.count[eng] = n
        self.streams[eng].append(("c", fn, waits, (eng, n)))
        self._mark((eng, n), r, w)

    def dma(self, q, fn, r=(), w=()):
        if self._over():
            return
        k = self.rrq.get(q, 0)
        self.rrq[q] = (k + 1) % NDMA
        g = "q%s%d" % (q, k)
        deps = self._collect(q, r, w, True)
        n = self.count.get(g, 0) + 1
        if n > 1:
            deps[g] = max(deps.get(g, 0), n - 1)
        waits = self._waits(q, deps)
        self.count[g] = n
        self.streams[q].append(("d", fn, waits, (g, n)))
        self._mark((g, n), r, w)

    def barrier(self):
        for e in self.streams:
            deps = {g: c for g, c in self.count.items() if g != e and c > 0}
            waits = self._waits(e, deps)
            if waits:
                self.streams[e].append(("w", None, waits, None))

    def emit(self, nc, stack):
        rank = {}
        nsem = {}
        for g, c in self.count.items():
            if g.startswith("q"):
                nsem[g] = (c + EPD - 1) // EPD
            else:
                ws = sorted(self.waited.get(g, ()))
                rank[g] = {i: k + 1 for k, i in enumerate(ws)}
                nsem[g] = max(1, (len(ws) + EP - 1) // EP)
        sems = {g: [stack.enter_context(nc.semaphore("s_%s_%d" % (g, k))) for k in range(n)]
                for g, n in nsem.items()}

        def semval(g, i):
            if g.startswith("q"):
                ep = (i - 1) // EPD
                return sems[g][ep], (i - ep * EPD) * 16
            v = rank[g][i]
            ep = (v - 1) // EP
            return sems[g][ep], v - ep * EP

        def replay(name, e):
            for kind, fn, waits, t in self.streams[name]:
                for g, i in waits:
                    s, v = semval(g, i)
                    e.wait_ge(s, v)
                if kind == "w":
                    continue
                ins = getattr(e, fn[0])(**fn[1])
                if kind == "d":
                    s, _ = semval(*t)
                    ins.then_inc(s, 16)
                elif t[1] in rank.get(t[0], {}):
                    s, _ = semval(*t)
                    ins.then_inc(s, 1)

        block = stack.enter_context(nc.Block())

        @block.tensor
        def _(e):
            replay("pe", e)

        @block.scalar
        def _(e):
            replay("act", e)

        @block.vector
        def _(e):
            replay("dve", e)

        @block.gpsimd
        def _(e):
            replay("pool", e)

        @block.sync
        def _(e):
            replay("sp", e)


LAYOUT = {}


class Arena:
    def __init__(self, t):
        self.t = t
        self.off = 0

    def alloc(self, nelem, dt, name=None):
        nb4 = nelem if dt == F32 else (nelem + 1) // 2
        if name is not None:
            LAYOUT[name] = (self.off, nelem, "f32" if dt == F32 else "bf16")
        v = self.t[:, self.off:self.off + nb4]
        self.off += nb4
        LAYOUT["_peak"] = max(LAYOUT.get("_peak", 0), self.off)
        assert self.off <= self.t.shape[1], ("arena overflow", self.off)
        if dt == F32:
            return v
        return v.bitcast(dt)[:, 0:nelem]


class _Stop(Exception):
    pass


NOROPE = False


def build(NS=4, NL=2, dumps=None, STOP=99, LIMIT=None):
    dumps = dumps or {}
    nc = bass.Bass("TRN2", target_bir_lowering=False)
    P = Prog()
    P.limit = LIMIT
    stack = ExitStack()

    def din(name, shape):
        return nc.dram_tensor(name, list(shape), F32, kind="ExternalInput").ap()

    x_d = din("x", (NS, SEQ, D))
    ctx_d = din("ctx", (NS, CTX, D))
    cc_d = din("cc", (NS + 1, D))
    ada_w = din("ada_w", (2, D, 6 * D))
    ada_b = din("ada_b", (2, 6 * D))
    n1g = din("norm1_g", (2, D))
    w_in = din("w_in", (2, D, 1824))
    qg_d = din("mla_q_norm_g", (2, 256))
    w_uq = din("mla_w_uq", (2, 256, 576))
    kvg_d = din("mla_kv_norm_g", (2, 128))
    w_ukv = din("mla_w_ukv", (2, 128, 768))
    sink_d = din("swa_sink", (2, 6))
    convw_d = din("conv_w", (2, 3, 256))
    w_o = din("w_o", (2, D, D))
    n2g = din("norm2_g", (2, D))
    rw_d = din("router_w", (2, D, NE))
    wg_d = din("exp_w_gate", (2, NE, D, 512))
    wu_d = din("exp_w_up", (2, NE, D, 512))
    wd_d = din("exp_w_down", (2, NE, 512, D))
    fg_d = din("final_norm_g", (D,))
    k_ident = din("k_ident", (128, 128))
    k_masks = din("k_masks", (128, 384))
    k_iota = din("k_iota", (128, 256))
    k_ropem = din("k_ropem", (128, 16 * 64))
    k_ropes = din("k_ropes", (128, 16 * 128))
    out_d = nc.dram_tensor("out", [NS, SEQ, D], F32, kind="ExternalOutput").ap()
    xmid_d = nc.dram_tensor("xmid", [NTOK, D], F32, kind="Internal").ap()
    xl1_d = nc.dram_tensor("xl1", [NTOK, D], F32, kind="Internal").ap()
    dump_d = {k: nc.dram_tensor("dbg_" + k, list(shp), F32, kind="ExternalOutput").ap()
              for k, shp in dumps.items()}

    def sb(name, shape, dt):
        return stack.enter_context(nc.sbuf_tensor(name, list(shape), dt))

    ident_f = sb("ident_f", (128, 128), F32)
    ident_b = sb("ident_b", (128, 128), BF)
    ones_f = sb("ones_f", (128, 128), F32)
    ones_b = sb("ones_b", (128, 128), BF)
    masks = sb("masks", (128, 384), BF)
    iota_f = sb("iota_f", (128, 256), F32)
    ropem = sb("ropem", (128, 1024), F32)
    ropes = sb("ropes", (128, 2048), F32)
    modT = sb("modT", (128, 2 * 48 * 5), F32)
    gT = sb("gT", (128, 2 * 2 * 8), F32)
    qgT = sb("qgT", (128, 4), F32)
    kvgT = sb("kvgT", (128, 2), F32)
    cwT = sb("cwT", (128, 2 * 2 * 3), F32)
    esink = sb("esink", (128, 12), F32)
    abT = sb("abT", (128, 2 * 48), F32)
    fgb = sb("fgb", (128, D), F32)
    kvec = sb("kvec", (128, 32), F32)
    AR = sb("arena", (128, 47300), F32)
    ar = Arena(AR)
    psum = [stack.enter_context(nc.psum_tensor("ps%d" % i, [128, 512], F32)) for i in range(8)]

    def psb(i):
        return psum[i][:, :].bitcast(BF)

    def act(fn, r, w):
        P.op("act", fn, r, w)

    def dve(fn, r, w):
        P.op("dve", fn, r, w)

    def pool(fn, r, w):
        P.op("pool", fn, r, w)

    def mm(out, lhsT, rhs, start, stop, r, w):
        P.op("pe", _R("matmul", out, lhsT, rhs, start=start, stop=stop, skip_group_check=True), r, w)

    def tr(out, in_, ident, r, w):
        P.op("pe", _R("transpose", out, in_, ident), r, w)

    def dma(q, out, in_, r, w, slow=False):
        if slow:
            P.dma(q, _R("dma_start", out=out, in_=in_, allow_slow_non_contiguous=True), r, w)
        else:
            P.dma(q, _R("dma_start", out=out, in_=in_), r, w)

    def rsq(ap, r, w):
        dve(_R("tensor_scalar", out=ap, in0=ap, scalar1=EPS, scalar2=None, op0=ALU.add), r, w)
        act(_R("activation", out=ap, in_=ap, func=AF.Ln), w, w)
        act(_R("activation", out=ap, in_=ap, func=AF.Exp, scale=-0.5), w, w)

    def dump(name, ap, idx=None):
        if name in dump_d:
            d = dump_d[name] if idx is None else dump_d[name][idx]
            P.barrier()
            dma("sp", d, ap, [], ["dump_" + name])


    dma("sp", ident_f[:, :], k_ident, [], ["ident_f"])
    dma("pool", ident_b[:, :], k_ident, [], ["ident_b"])
    dma("pool", masks[:, :], k_masks, [], ["masks"])
    dma("sp", iota_f[:, :], k_iota, [], ["iota_f"])
    dma("sp", ropem[:, :], k_ropem, [], ["ropem"])
    dma("sp", ropes[:, :], k_ropes, [], ["ropes"])
    dve(_R("memset", ones_f[:, :], 1.0), [], ["ones_f"])
    dve(_R("memset", ones_b[:, :], 1.0), [], ["ones_b"])
    dve(_R("memset", kvec[:, 0:16], 32.0), [], ["kvec"])
    dve(_R("memset", kvec[:, 16:32], 256.0), [], ["kvec"])
    ctxmgr = nc.allow_non_contiguous_dma(reason="tiny parameter vectors")
    ctxmgr.__enter__()
    for l in range(2):
        dma("sp", gT[:, (l * 2) * 8:(l * 2 + 1) * 8], n1g[l].rearrange("(k p) -> p k", p=128), [], ["gT"], slow=True)
        dma("sp", gT[:, (l * 2 + 1) * 8:(l * 2 + 2) * 8], n2g[l].rearrange("(k p) -> p k", p=128), [], ["gT"], slow=True)
        dma("sp", qgT[:, l * 2:l * 2 + 2], qg_d[l].rearrange("(k p) -> p k", p=128), [], ["qgT"], slow=True)
        dma("sp", kvgT[:, l:l + 1], kvg_d[l].rearrange("(k p) -> p k", p=128), [], ["kvgT"], slow=True)
        for c2 in range(2):
            dma("sp", cwT[:, (l * 2 + c2) * 3:(l * 2 + c2) * 3 + 3],
                convw_d[l][:, c2 * 128:(c2 + 1) * 128].rearrange("k p -> p k"), [], ["cwT"], slow=True)
        dma("sp", abT[:, l * 48:(l + 1) * 48], ada_b[l].rearrange("(j p) -> p j", p=128), [], ["abT"], slow=True)
        dma("sp", esink[:, l * 6:(l + 1) * 6], sink_d[l].partition_broadcast(128), [], ["esink"])
    ctxmgr.__exit__(None, None, None)
    dma("sp", fgb[:, :], fg_d.partition_broadcast(128), [], ["fgb"])
    act(_R("activation", out=esink[:, :], in_=esink[:, :], func=AF.Exp), ["esink"], ["esink"])

    ar.off = 0
    cc_sb = ar.alloc(D, F32, "cc_sb")
    cs_bf = ar.alloc(D, BF, "cs_bf")
    sT = ar.alloc(64, BF, "sT")
    awb = [ar.alloc(6 * D, BF), ar.alloc(6 * D, BF)]
    NB = NS + 1
    if STOP >= 1:
        dma("sp", cc_sb[0:NB, :], cc_d, [], ["cc_sb"])
        act(_R("activation", out=cs_bf[0:NB, :], in_=cc_sb[0:NB, :], func=AF.Silu), ["cc_sb"], ["cs_bf"])
        for k in range(8):
            tr(psb(0)[:, k * 8:k * 8 + NB], cs_bf[0:NB, k * 128:(k + 1) * 128], ident_b[0:NB, 0:NB],
               ["cs_bf", "ident_b"], ["ps0"])
        dve(_R("tensor_copy", out=sT[:, :].rearrange("p (k b) -> p k b", b=8)[:, :, 0:NB],
               in_=psb(0)[:, 0:64].rearrange("p (k b) -> p k b", b=8)[:, :, 0:NB]), ["ps0"], ["sT"])
        for l in range(NL):
            for k in range(8):
                b = awb[k % 2]
                dma("pool", b[:, :], ada_w[l, k * 128:(k + 1) * 128, :], [], ["awb%d" % (k % 2)])
                for j in range(48):
                    mm(psum[1][:, j * NB:(j + 1) * NB], b[:, j * 128:(j + 1) * 128], sT[:, k * 8:k * 8 + NB],
                       (k == 0 and j == 0), (k == 7 and j == 47), ["awb%d" % (k % 2), "sT"], ["ps1"])
            mview = modT[:, l * 240:l * 240 + 48 * NB].rearrange("p (j b) -> p j b", b=NB) if NB == 5 else \
                modT[:, l * 240:l * 240 + 48 * NB].rearrange("p (j b) -> p j b", b=NB)
            dve(_R("tensor_tensor",
                out=mview, in0=psum[1][:, 0:48 * NB].rearrange("p (j b) -> p j b", b=NB),
                in1=abT[:, l * 48:(l + 1) * 48].unsqueeze(2).broadcast_to([128, 48, NB]), op=ALU.add),
                ["ps1", "abT"], ["modT"])

    def modcol(l, m, b):
        return modT[:, l * 240:l * 240 + 48 * NB].rearrange("p (j b) -> p j b", b=NB)[:, m * 8:(m + 1) * 8, b]

    P.barrier()

    chunks = [(0, 2)] + [(2 + 4 * j, 4) for j in range(4)]
    LAYOUT["n1"] = P.nrec
    if STOP <= 1:
        NS = 0

    for s in range(NS):
        for l in range(NL):
            src_lat = x_d[s] if l == 0 else xl1_d[256:NTOK, :]
            src_ctx = ctx_d[s] if l == 0 else xl1_d[0:256, :]

            def src_tiles(t0, nt):
                if t0 == 0:
                    return src_ctx.rearrange("(n p) d -> p n d", p=128)
                return src_lat[(t0 - 2) * 128:(t0 - 2 + nt) * 128, :].rearrange("(n p) d -> p n d", p=128)

            ar.off = 0
            AB = ar.alloc(4 * 2 * 8, F32)
            KT_mla = ar.alloc(6 * NTOK, BF, "KT_mla")
            V_mla = ar.alloc(NT * 3 * 160, BF, "V_mla")
            KT_swa = ar.alloc(2 * NTOK, BF, "KT_swa")
            V_swa = ar.alloc(NT * 2 * 160, BF, "V_swa")
            convT = ar.alloc(2 * NTOK, BF, "convT")
            xc = ar.alloc(4 * D, F32, "xc")
            hT = ar.alloc(8 * 512, BF, "hT")
            xn = [ar.alloc(D, BF), ar.alloc(D, BF)]
            junk = ar.alloc(D, BF, "junk")
            tmpf = ar.alloc(D, F32, "tmpf")
            ss = ar.alloc(8, F32, "ss")
            gtmp = ar.alloc(512, F32, "gtmp")
            rx = ar.alloc(512, F32, "rx")
            ph_off = ar.off
            KT_mla3 = KT_mla.rearrange("p (h t) -> p h t", h=6)
            KT_swa3 = KT_swa.rearrange("p (g t) -> p g t", g=2)
            V_mla4 = V_mla.rearrange("p (i q c) -> p i q c", i=NT, q=3)
            V_swa4 = V_swa.rearrange("p (i g c) -> p i g c", i=NT, g=2)
            convT3 = convT.rearrange("p (c t) -> p c t", c=2)
            hT3 = hT.rearrange("p (k t) -> p k t", k=8)
            xc3 = xc.rearrange("p (n d) -> p n d", n=4)

            def ABv(which, seg):
                o = (which * 2 + seg) * 8
                return AB[:, o:o + 8]

            for seg in range(2):
                b = NS if seg == 0 else s
                for nrm in range(2):
                    dve(_R("scalar_tensor_tensor",
                        out=ABv(2 * nrm, seg), in0=modcol(l, 3 * nrm + 1, b), scalar=1.0,
                        in1=gT[:, (l * 2 + nrm) * 8:(l * 2 + nrm + 1) * 8], op0=ALU.add, op1=ALU.mult),
                        ["modT", "gT"], ["AB"])
                    dve(_R("tensor_copy",
                        out=ABv(2 * nrm + 1, seg), in_=modcol(l, 3 * nrm, b)), ["modT"], ["AB"])

            pool(_R("memset", V_mla[:, :], 0.0), [], ["V_mla"])
            pool(_R("memset", V_swa[:, :], 0.0), [], ["V_swa"])
            pool(_R("memset", V_mla4[:, :, :, 64:65], 1.0), [], ["V_mla"])
            pool(_R("memset", V_swa4[:, :, :, 64:65], 1.0), [], ["V_swa"])

            def gbuild(dst, col8, pbank):
                for k in range(8):
                    dve(_R("tensor_scalar", out=gtmp[:, (k % 4) * 128:(k % 4 + 1) * 128], in0=ident_f[:, :],
                                                       scalar1=col8[:, k:k + 1], scalar2=None, op0=ALU.mult),
                        ["ident_f", "modT"], ["gtmp%d" % (k % 4)])
                    mm(psum[pbank][:, (k % 4) * 128:(k % 4 + 1) * 128], ones_f[:, :],
                       gtmp[:, (k % 4) * 128:(k % 4 + 1) * 128], k % 4 == 0, k % 4 == 3,
                       ["ones_f", "gtmp%d" % (k % 4)], ["ps%d" % pbank])
                    if k % 4 == 3:
                        act(_R("copy", out=dst[:, (k - 3) * 128:(k + 1) * 128], in_=psum[pbank][:, :]),
                            ["ps%d" % pbank], ["gb"])

            def load_chunk(t0, nt):
                dma("sp", xc3[:, 0:nt, :], src_tiles(t0, nt), ["xsrc"], ["xc"])

            def norm_T(t0, nt, which, dst3, dcol0, pb0, hname="hT", keep=None):
                seg = 0 if t0 == 0 else 1
                A = ABv(2 * which, seg)
                B = ABv(2 * which + 1, seg)
                dve(_R("memset", ss[:, :], 0.0), [], ["ss"])
                for j in range(nt):
                    act(_R("activation", out=junk[:, :], in_=xc3[:, j, :], func=AF.Square, scale=1.0 / 32.0,
                                                    accum_out=ss[:, j:j + 1]), ["xc", "ss"], ["junk", "ss%d" % j])
                rsq(ss[:, 0:nt], ["ss"] + ["ss%d" % j for j in range(nt)], ["ss"])
                for j in range(nt):
                    xb = xn[j % 2]
                    xbn = "xn%d" % (j % 2)
                    if keep is not None:
                        xb, xbn = keep(j)
                    pb = pb0 + (j % 2)
                    act(_R("activation", out=xb[:, :], in_=xc3[:, j, :], func=AF.Copy,
                                                           scale=ss[:, j:j + 1]), ["xc", "ss"], [xbn])
                    for k in range(8):
                        tr(psb(pb)[:, k * 128:(k + 1) * 128], xb[:, k * 128:(k + 1) * 128], ident_b[:, :],
                           [xbn, "ident_b"], ["ps%d" % pb])
                    dve(_R("tensor_tensor",
                        out=tmpf.rearrange("p (k t) -> p k t", k=8),
                        in0=psb(pb).rearrange("p (k t) -> p k t", k=8),
                        in1=A.unsqueeze(2).broadcast_to([128, 8, 128]), op=ALU.mult),
                        ["ps%d" % pb, "AB"], ["tmpf"])
                    dve(_R("tensor_tensor",
                        out=dst3[:, :, dcol0 + j * 128:dcol0 + (j + 1) * 128],
                        in0=tmpf.rearrange("p (k t) -> p k t", k=8),
                        in1=B.unsqueeze(2).broadcast_to([128, 8, 128]), op=ALU.add),
                        ["tmpf", "AB"], [hname])

            def rope(out3, in3, ti, kind, H, u, v, rr, ww):
                if NOROPE:
                    act(_R("copy", out=out3, in_=in3), rr, ww)
                    return
                rot = 32 if kind == "m" else 64
                half = rot // 2
                tab = ropem if kind == "m" else ropes
                cosd = tab[:, ti * 2 * rot:ti * 2 * rot + rot].unsqueeze(1).broadcast_to([128, H, rot])
                sind = tab[:, ti * 2 * rot + rot:(ti + 1) * 2 * rot].unsqueeze(1).broadcast_to([128, H, rot])
                n = H * rot
                u3 = u[:, 0:n].rearrange("p (h d) -> p h d", h=H)
                v3 = v[:, 0:n].rearrange("p (h d) -> p h d", h=H)
                x3 = rx[:, 0:n].rearrange("p (h d) -> p h d", h=H)
                act(_R("copy", out=x3, in_=in3), rr, ["rope_x"])
                pool(_R("tensor_tensor", out=u3, in0=x3, in1=cosd, op=ALU.mult), ["rope_x"] + rr, ["rope_u"])
                pool(_R("tensor_tensor", out=v3, in0=x3, in1=sind, op=ALU.mult), ["rope_x"] + rr, ["rope_v"])
                pool(_R("tensor_tensor", out=out3[:, :, 0:half], in0=u3[:, :, 0:half], in1=v3[:, :, half:rot],
                        op=ALU.subtract), ["rope_u", "rope_v"], ww)
                pool(_R("tensor_tensor", out=out3[:, :, half:rot], in0=u3[:, :, half:rot], in1=v3[:, :, 0:half],
                        op=ALU.add), ["rope_u", "rope_v"], ww)

            ar.off = ph_off
            wA = ar.alloc(8 * 1184, BF, "wA")
            wA3 = wA.rearrange("p (k c) -> p k c", k=8)
            wkv_f = ar.alloc(768, F32, "wkv_f")
            wkv = ar.alloc(768, BF, "wkv")
            zT = ar.alloc(2 * 2308, BF, "zT")
            zT3 = zT.rearrange("p (c t) -> p c t", c=2)
            BT = ar.alloc(2 * NTOK, BF, "BT")
            BT3 = BT.rearrange("p (c t) -> p c t", c=2)
            ckvnb = [ar.alloc(128, BF) for _ in range(2)]
            ckvnTb = [ar.alloc(128, BF) for _ in range(2)]
            krb = [ar.alloc(32, BF) for _ in range(2)]
            ktokb = [ar.alloc(6 * 96, BF) for _ in range(2)]
            kswb = [ar.alloc(128, BF) for _ in range(2)]
            kdupb = [ar.alloc(256, BF) for _ in range(2)]
            sskb = [ar.alloc(2, F32) for _ in range(2)]
            hT_b = ar.alloc(8 * 512, BF, "hT_b")
            hTdb = [hT3, hT_b.rearrange("p (k t) -> p k t", k=8)]
            ru = ar.alloc(512, F32, "ru")
            rv = ar.alloc(512, F32, "rv")
            tmpC = ar.alloc(512, F32, "tmpC")
            ssk = ar.alloc(2, F32, "ssk")

            w_l = w_in[l].rearrange("(k p) c -> p k c", p=128)
            dma("pool", wA3[:, :, 0:160], w_l[:, :, 256:416], [], ["wA"])
            dma("pool", wA3[:, :, 160:416], w_l[:, :, 800:1056], [], ["wA"])
            dma("pool", wA3[:, :, 416:1184], w_l[:, :, 1056:1824], [], ["wA"])
            with nc.allow_non_contiguous_dma(reason="head-split weight layout"):
                for two in range(2):
                    dma("sp", wkv_f[:, two * 384:(two + 1) * 384].rearrange("p (h d) -> p h d", h=6),
                        w_ukv[l].rearrange("r (h two d) -> r two h d", h=6, two=2)[:, two], [], ["wkv_f"], slow=True)
            dve(_R("tensor_scalar", out=wkv[:, :], in0=wkv_f[:, :], scalar1=kvgT[:, l:l + 1], scalar2=None,
                                          op0=ALU.mult), ["wkv_f", "kvgT"], ["wkv"])
            pool(_R("memset", zT[:, :], 0.0), [], ["zT"])

            def zcol(t):
                return 1 + t if t < 256 else 3 + t

            def kvM0(j, i, hTb, hname):
                p = i % 2
                hsl = slice(j * 128, (j + 1) * 128)
                pa = 2 + p
                pan = "ps%d" % pa
                for k in range(8):
                    mm(psum[pa][:, 0:416], hTb[:, k, hsl], wA3[:, k, 0:416], k == 0, k == 7, [hname, "wA"], [pan])
                dve(_R("memset", sskb[p][:, :], 0.0), [], ["ssk%d" % p])
                act(_R("activation", out=junk[:, 0:128], in_=psum[pa][:, 0:128], func=AF.Square,
                       scale=128 ** -0.5, accum_out=sskb[p][:, 0:1]), [pan, "ssk%d" % p], ["junk", "ssk%d" % p])
                rsq(sskb[p][:, 0:1], ["ssk%d" % p], ["ssk%d" % p])
                act(_R("activation", out=ckvnb[p][:, :], in_=psum[pa][:, 0:128], func=AF.Copy,
                       scale=sskb[p][:, 0:1]), [pan, "ssk%d" % p], ["ckvn%d" % p])
                if i >= 2:
                    ti = i - 2
                    rope(krb[p].rearrange("p (h d) -> p h d", h=1),
                         psum[pa][:, 128:160].rearrange("p (h d) -> p h d", h=1), ti, "m", 1, ru, rv,
                         [pan, "ropem"], ["kr%d" % p])
                    rope(kswb[p].rearrange("p (h d) -> p h d", h=2),
                         psum[pa][:, 160:288].rearrange("p (h d) -> p h d", h=2), ti, "s", 2, ru, rv,
                         [pan, "ropes"], ["ksw%d" % p])
                else:
                    act(_R("copy", out=krb[p][:, :], in_=psum[pa][:, 128:160]), [pan], ["kr%d" % p])
                    act(_R("copy", out=kswb[p][:, :], in_=psum[pa][:, 160:288]), [pan], ["ksw%d" % p])
                pool(_R("tensor_copy", out=kdupb[p].rearrange("p (g c d) -> p g c d", g=2, c=2),
                        in_=kswb[p].rearrange("p (g d) -> p g d", g=2).unsqueeze(2).broadcast_to([128, 2, 2, 64])),
                     ["ksw%d" % p], ["kdup%d" % p])
                for c0 in (0, 96):
                    act(_R("copy", out=V_swa4[:, i, :, c0:c0 + 64],
                           in_=psum[pa][:, 288:416].rearrange("p (g d) -> p g d", g=2)), [pan], ["V_swa"])

            def kvM1(j, i, hTb, hname):
                p = i % 2
                cols = slice(i * 128, (i + 1) * 128)
                tr(psb(4)[:, 0:128], ckvnb[p][:, :], ident_b[:, :], ["ckvn%d" % p, "ident_b"], ["ps4"])
                for g in range(2):
                    tr(psb(4)[:, 128 + g * 128:256 + g * 128], kdupb[p][:, g * 128:(g + 1) * 128], ident_b[:, :],
                       ["kdup%d" % p, "ident_b"], ["ps4"])
                act(_R("copy", out=ckvnTb[p][:, :], in_=psb(4)[:, 0:128]), ["ps4"], ["ckvnT%d" % p])
                act(_R("copy", out=KT_swa3[:, :, cols], in_=psb(4)[:, 128:384].rearrange("p (g t) -> p g t", g=2)),
                    ["ps4"], ["KT_swa"])

            def kvM2(j, i, hTb, hname):
                p = i % 2
                mm(psum[5][:, 0:384], ckvnTb[p][:, :], wkv[:, 0:384], True, True, ["ckvnT%d" % p, "wkv"], ["ps5"])
                mm(psum[6][:, 0:384], ckvnTb[p][:, :], wkv[:, 384:768], True, True, ["ckvnT%d" % p, "wkv"], ["ps6"])
                pv4 = psum[6][:, 0:384].rearrange("p (q two d) -> p q two d", q=3, two=2)
                act(_R("copy", out=V_mla4[:, i, :, 0:64], in_=pv4[:, :, 0, :]), ["ps6"], ["V_mla"])
                act(_R("copy", out=V_mla4[:, i, :, 96:160], in_=pv4[:, :, 1, :]), ["ps6"], ["V_mla"])
                kt3 = ktokb[p].rearrange("p (h d) -> p h d", h=6)
                dve(_R("tensor_copy", out=kt3[:, :, 0:64], in_=psum[5][:, 0:384].rearrange("p (h d) -> p h d", h=6)),
                    ["ps5"], ["ktok%d" % p])
                pool(_R("tensor_copy", out=kt3[:, :, 64:96], in_=krb[p].unsqueeze(1).broadcast_to([128, 6, 32])),
                     ["kr%d" % p], ["ktok%d" % p])

            def kvM3(j, i, hTb, hname):
                p = i % 2
                cols = slice(i * 128, (i + 1) * 128)
                kt3 = ktokb[p].rearrange("p (h d) -> p h d", h=6)
                for h in range(6):
                    tr(psb(7)[0:96, h * 128:(h + 1) * 128], kt3[:, h, :], ident_b[:, :], ["ktok%d" % p, "ident_b"], ["ps7"])
                dve(_R("tensor_copy", out=KT_mla3[0:96, :, cols],
                       in_=psb(7)[0:96, 0:768].rearrange("p (h t) -> p h t", h=6)), ["ps7"], ["KT_mla"])

            def conv_chunk(t0, nt, hTb, hname):
                n = nt * 128
                g0 = t0 * 128
                for c2 in range(2):
                    for which, pbk in ((1, 5), (2, 6), (0, 7)):
                        cb = 416 + (which * 2 + c2) * 128
                        for k in range(8):
                            mm(psum[pbk][:, 0:n], wA3[:, k, cb:cb + 128], hTb[:, k, 0:n], k == 0, k == 7,
                               [hname, "wA"], ["ps%d" % pbk])
                    act(_R("copy", out=tmpC[:, 0:n], in_=psum[5][:, 0:n]), ["ps5"], ["tmpC"])
                    dve(_R("tensor_tensor", out=zT3[:, c2, zcol(g0):zcol(g0) + n], in0=tmpC[:, 0:n], in1=psum[6][:, 0:n],
                           op=ALU.mult), ["tmpC", "ps6"], ["zT"])
                    act(_R("copy", out=BT3[:, c2, g0:g0 + n], in_=psum[7][:, 0:n]), ["ps7"], ["BT"])

            kv_stages = [kvM0, kvM1, kvM2, kvM3]
            tiles_a = []
            for ci, (t0, nt) in enumerate(chunks):
                for j in range(nt):
                    tiles_a.append((ci, t0, nt, j, t0 + j))
            for step in range(len(tiles_a) + len(kv_stages) - 1):
                if step < len(tiles_a):
                    ci, t0, nt, j, i = tiles_a[step]
                    if j == 0:
                        load_chunk(t0, nt)
                        norm_T(t0, nt, 0, hTdb[ci % 2], 0, 0, hname="hT%d" % (ci % 2))
                for sidx in reversed(range(len(kv_stages))):
                    t = step - sidx
                    if 0 <= t < len(tiles_a):
                        ci, t0, nt, j, i = tiles_a[t]
                        kv_stages[sidx](j, i, hTdb[ci % 2], "hT%d" % (ci % 2))
                        if sidx == 0 and j == nt - 1:
                            conv_chunk(t0, nt, hTdb[ci % 2], "hT%d" % (ci % 2))
            for c2 in range(2):
                cw = cwT[:, (l * 2 + c2) * 3:(l * 2 + c2) * 3 + 3]
                for g0, n in [(0, 256)] + [(256 + 512 * j, 512) for j in range(4)]:
                    zc = zcol(g0)
                    dve(_R("tensor_scalar",
                        out=tmpf[:, 0:n], in0=zT3[:, c2, zc - 1:zc - 1 + n], scalar1=cw[:, 0:1], scalar2=None,
                        op0=ALU.mult), ["zT", "cwT"], ["tmpf"])
                    dve(_R("scalar_tensor_tensor",
                        out=tmpf[:, 0:n], in0=zT3[:, c2, zc:zc + n], scalar=cw[:, 1:2], in1=tmpf[:, 0:n],
                        op0=ALU.mult, op1=ALU.add), ["zT", "cwT", "tmpf"], ["tmpf"])
                    dve(_R("scalar_tensor_tensor",
                        out=tmpf[:, 0:n], in0=zT3[:, c2, zc + 1:zc + 1 + n], scalar=cw[:, 2:3], in1=tmpf[:, 0:n],
                        op0=ALU.mult, op1=ALU.add), ["zT", "cwT", "tmpf"], ["tmpf"])
                    dve(_R("tensor_tensor",
                        out=convT3[:, c2, g0:g0 + n], in0=tmpf[:, 0:n], in1=BT3[:, c2, g0:g0 + n], op=ALU.mult),
                        ["tmpf", "BT"], ["convT"])
            P.barrier()
            LAYOUT["n2"] = P.nrec
            if STOP <= 2:
                continue

            ar.off = ph_off
            wB = ar.alloc(8 * 640, BF, "wB")
            wB3 = wB.rearrange("p (k c) -> p k c", k=8)
            wuq_f = ar.alloc(2 * 576, F32, "wuq_f")
            wuq = ar.alloc(2 * 576, BF, "wuq")
            wuq3 = wuq.rearrange("p (r c) -> p r c", r=2)
            wo = ar.alloc(8 * D, BF, "wo")
            wo3 = wo.rearrange("p (f d) -> p f d", f=8)
            Gb = ar.alloc(D, F32, "Gb")
            QT_mla = ar.alloc(6 * 512, BF, "QT_mla")
            QT_mla3 = QT_mla.rearrange("p (h t) -> p h t", h=6)
            QT_swa = ar.alloc(3 * 512, BF, "QT_swa")
            QT_swa3 = QT_swa.rearrange("p (h t) -> p h t", h=3)
            mixT = ar.alloc(6 * 512, BF, "mixT")
            mixT3 = mixT.rearrange("p (f t) -> p f t", f=6)
            PT = [ar.alloc(512, BF) for _ in range(6)]
            cqnb = [ar.alloc(256, BF) for _ in range(2)]
            cqnTb = [ar.alloc(256, BF) for _ in range(2)]
            qtokb = [ar.alloc(6 * 96, BF) for _ in range(2)]
            qswb = [ar.alloc(384, BF) for _ in range(2)]
            ru = ar.alloc(512, F32, "ru")
            rv = ar.alloc(512, F32, "rv")
            rs = ar.alloc(512, F32, "rs")
            bcs = ar.alloc(512, F32, "bcs")
            ssqb = [ar.alloc(2, F32) for _ in range(2)]

            dma("pool", wB3[:, :, 0:256], w_l[:, :, 0:256], [], ["wB"])
            dma("pool", wB3[:, :, 256:640], w_l[:, :, 416:800], [], ["wB"])
            dma("sp", wuq_f.rearrange("p (r c) -> p r c", r=2), w_uq[l].rearrange("(r p) c -> p r c", p=128),
                [], ["wuq_f"])
            for r_ in range(2):
                dve(_R("tensor_scalar", out=wuq3[:, r_, :], in0=wuq_f[:, r_ * 576:(r_ + 1) * 576],
                                                     scalar1=qgT[:, l * 2 + r_:l * 2 + r_ + 1], scalar2=None,
                                                     op0=ALU.mult), ["wuq_f", "qgT"], ["wuq"])
            dma("pool", wo3[:, :, :], w_o[l].rearrange("(f p) d -> p f d", p=128), [], ["wo"])

            SBK = [0, 1, 2, 3, 7]
            NPT = len(PT)
            att_state = {"pti": 0, "pending": None}

            def attention(nq, heads, qlhs, klhs, vwin, kblocks, scale, sink_l, mix_chunk0):
                LOOK = 4

                def finalize(h, po):
                    half = h % 2
                    r0 = 64 if half == 0 else 32
                    rows = slice(0, 64) if half == 0 else slice(64, 128)
                    if sink_l is not None:
                        dve(_R("tensor_scalar", out=rs[r0:r0 + 1, 0:nq], in0=psum[po][r0:r0 + 1, 0:nq],
                               scalar1=esink[r0:r0 + 1, sink_l * 6 + h:sink_l * 6 + h + 1], scalar2=None, op0=ALU.add),
                            ["ps%d" % po, "esink"], ["rs"])
                        dve(_R("reciprocal", out=rs[r0:r0 + 1, 0:nq], in_=rs[r0:r0 + 1, 0:nq]), ["rs"], ["rs"])
                    else:
                        dve(_R("reciprocal", out=rs[r0:r0 + 1, 0:nq], in_=psum[po][r0:r0 + 1, 0:nq]),
                            ["ps%d" % po], ["rs"])
                    mm(psum[6][:, 0:nq], ones_f[r0:r0 + 1, :], rs[r0:r0 + 1, 0:nq], True, True,
                       ["rs", "ones_f"], ["ps6"])
                    act(_R("copy", out=bcs[:, 0:nq], in_=psum[6][:, 0:nq]), ["ps6"], ["bcs"])
                    dve(_R("tensor_tensor", out=mixT3[rows, mix_chunk0 + h // 2, 0:nq], in0=psum[po][rows, 0:nq],
                           in1=bcs[rows, 0:nq], op=ALU.mult), ["ps%d" % po, "bcs"], ["mixT"])

                for h in heads:
                    po = 4 + (h % 2)
                    blocks = kblocks(h)
                    ptinfo = {}

                    def issue_S(bi, h=h, blocks=blocks, ptinfo=ptinfo):
                        kb, c0, c1, mks = blocks[bi]
                        psn = SBK[att_state["pti"] % len(SBK)]
                        mm(psum[psn][:, c0:c1], klhs(h, kb), qlhs(h, c0, c1), True, True,
                           ["KT_mla", "KT_swa", "QT"], ["ps%d" % psn])
                        pt = PT[att_state["pti"] % NPT]
                        ptn = "PT%d" % (att_state["pti"] % NPT)
                        att_state["pti"] += 1
                        ptinfo[bi] = (pt, ptn)
                        act(_R("activation", out=pt[:, c0:c1], in_=psum[psn][:, c0:c1], func=AF.Exp, scale=scale),
                            ["ps%d" % psn], [ptn])
                        for (m0, mk) in mks:
                            pool(_R("tensor_tensor", out=pt[:, m0:m0 + 128], in0=pt[:, m0:m0 + 128],
                                    in1=masks[:, mk * 128:(mk + 1) * 128], op=ALU.mult), [ptn, "masks"], [ptn])

                    for bi in range(min(LOOK, len(blocks))):
                        issue_S(bi)
                    for bi, (kb, c0, c1, mks) in enumerate(blocks):
                        if bi + LOOK < len(blocks):
                            issue_S(bi + LOOK)
                        if bi == 1 and att_state["pending"] is not None:
                            att_state["pending"]()
                            att_state["pending"] = None
                        pt, ptn = ptinfo[bi]
                        mm(psum[po][:, c0:c1], vwin(h, kb), pt[:, c0:c1], bi == 0, bi == len(blocks) - 1,
                           [ptn, "V_mla", "V_swa"], ["ps%d" % po])
                    if att_state["pending"] is not None:
                        att_state["pending"]()
                    att_state["pending"] = (lambda h=h, po=po: finalize(h, po))

            def attention_flush():
                if att_state["pending"] is not None:
                    att_state["pending"]()
                    att_state["pending"] = None

            first_lat = True
            for (t0, nt) in chunks:
                if t0 == 0 and l == 1:
                    continue
                seg = 0 if t0 == 0 else 1
                nq = nt * 128
                if t0 == 0 or first_lat:
                    gbuild(Gb, modcol(l, 2, NS if t0 == 0 else s), 7)
                    if t0 != 0:
                        first_lat = False
                load_chunk(t0, nt)
                norm_T(t0, nt, 0, hT3, 0, 0)
                def qM0(j, i):
                    p = i % 2
                    hsl = slice(j * 128, (j + 1) * 128)
                    pq, ps_ = (2, 3) if p == 0 else (0, 1)
                    for k in range(8):
                        mm(psum[pq][:, 0:256], hT3[:, k, hsl], wB3[:, k, 0:256], k == 0, k == 7, ["hT", "wB"], ["ps%d" % pq])
                    for k in range(8):
                        mm(psum[ps_][:, 0:384], hT3[:, k, hsl], wB3[:, k, 256:640], k == 0, k == 7, ["hT", "wB"], ["ps%d" % ps_])
                    dve(_R("memset", ssqb[p][:, :], 0.0), [], ["ssq%d" % p])
                    act(_R("activation", out=junk[:, 0:256], in_=psum[pq][:, 0:256], func=AF.Square,
                           scale=1.0 / 16.0, accum_out=ssqb[p][:, 0:1]), ["ps%d" % pq, "ssq%d" % p], ["junk", "ssq%d" % p])
                    rsq(ssqb[p][:, 0:1], ["ssq%d" % p], ["ssq%d" % p])
                    act(_R("activation", out=cqnb[p][:, :], in_=psum[pq][:, 0:256], func=AF.Copy, scale=ssqb[p][:, 0:1]),
                        ["ps%d" % pq, "ssq%d" % p], ["cqn%d" % p])
                    if i >= 2:
                        rope(qswb[p].rearrange("p (h d) -> p h d", h=6),
                             psum[ps_][:, 0:384].rearrange("p (h d) -> p h d", h=6), i - 2, "s", 6, ru, rv,
                             ["ps%d" % ps_, "ropes"], ["qsw%d" % p])
                    else:
                        act(_R("copy", out=qswb[p][:, :], in_=psum[ps_][:, 0:384]), ["ps%d" % ps_], ["qsw%d" % p])

                def qM1(j, i):
                    p = i % 2
                    hsl = slice(j * 128, (j + 1) * 128)
                    for r_ in range(2):
                        tr(psb(4)[:, r_ * 128:(r_ + 1) * 128], cqnb[p][:, r_ * 128:(r_ + 1) * 128], ident_b[:, :],
                           ["cqn%d" % p, "ident_b"], ["ps4"])
                    for pr in range(3):
                        tr(psb(4)[:, 256 + pr * 128:256 + (pr + 1) * 128], qswb[p][:, pr * 128:(pr + 1) * 128], ident_b[:, :],
                           ["qsw%d" % p, "ident_b"], ["ps4"])
                    act(_R("copy", out=cqnTb[p][:, :], in_=psb(4)[:, 0:256]), ["ps4"], ["cqnT%d" % p])
                    act(_R("copy", out=QT_swa3[:, :, hsl], in_=psb(4)[:, 256:640].rearrange("p (h t) -> p h t", h=3)),
                        ["ps4"], ["QT"])

                def qM2(j, i):
                    p = i % 2
                    for hh in range(2):
                        for r_ in range(2):
                            mm(psum[5 + hh][:, 0:288], cqnTb[p][:, r_ * 128:(r_ + 1) * 128],
                               wuq3[:, r_, hh * 288:(hh + 1) * 288], r_ == 0, r_ == 1, ["cqnT%d" % p, "wuq"], ["ps%d" % (5 + hh)])
                    qt3 = qtokb[p].rearrange("p (h d) -> p h d", h=6)
                    for hh in range(2):
                        pq3 = psum[5 + hh][:, 0:288].rearrange("p (h d) -> p h d", h=3)
                        if i >= 2:
                            act(_R("copy", out=qt3[:, hh * 3:(hh + 1) * 3, 0:64], in_=pq3[:, :, 0:64]),
                                ["ps%d" % (5 + hh)], ["qtok%d" % p])
                            rope(qt3[:, hh * 3:(hh + 1) * 3, 64:96], pq3[:, :, 64:96], i - 2, "m", 3, ru, rv,
                                 ["ps%d" % (5 + hh), "ropem"], ["qtok%d" % p])
                        else:
                            act(_R("copy", out=qt3[:, hh * 3:(hh + 1) * 3, :], in_=pq3), ["ps%d" % (5 + hh)], ["qtok%d" % p])

                def qM3(j, i):
                    p = i % 2
                    hsl = slice(j * 128, (j + 1) * 128)
                    qt3 = qtokb[p].rearrange("p (h d) -> p h d", h=6)
                    for h in range(6):
                        tr(psb(7)[0:96, h * 128:(h + 1) * 128], qt3[:, h, :], ident_b[:, :], ["qtok%d" % p, "ident_b"], ["ps7"])
                    dve(_R("tensor_copy", out=QT_mla3[0:96, :, hsl],
                           in_=psb(7)[0:96, 0:768].rearrange("p (h t) -> p h t", h=6)), ["ps7"], ["QT"])

                q_stages = [qM0, qM1, qM2, qM3]
                for step in range(nt + len(q_stages) - 1):
                    for sidx in reversed(range(len(q_stages))):
                        j = step - sidx
                        if 0 <= j < nt:
                            q_stages[sidx](j, t0 + j)

                if t0 == 0:
                    mla_blocks = lambda h: [(kb, 0, nq, []) for kb in range(2)]
                else:
                    mla_blocks = lambda h: [(kb, 0, nq, []) for kb in range(NT)]
                attention(nq, range(6),
                          lambda h, c0, c1: QT_mla3[0:96, h, c0:c1],
                          lambda h, kb: KT_mla3[0:96, h, kb * 128:(kb + 1) * 128],
                          lambda h, kb: V_mla4[:, kb, h // 2, (0 if h % 2 == 0 else 32):(128 if h % 2 == 0 else 160)],
                          mla_blocks, MLA_SCALE, None, 0)
                if t0 == 0:
                    swa_blocks = lambda h: [(kb, 0, nq, []) for kb in range(2)]
                else:
                    qb0 = t0 - 2

                    def swa_blocks(h, qb0=qb0):
                        bl = [(kb, 0, nq, []) for kb in range(2)]
                        for lb in range(max(0, qb0 - 1), min(15, qb0 + 4) + 1):
                            qlo = max(lb - 1, qb0)
                            qhi = min(lb + 1, qb0 + 3)
                            mks = []
                            for qb in range(qlo, qhi + 1):
                                if qb == lb + 1:
                                    mks.append(((qb - qb0) * 128, 0))
                                elif qb == lb - 1:
                                    mks.append(((qb - qb0) * 128, 1))
                            bl.append((2 + lb, (qlo - qb0) * 128, (qhi - qb0 + 1) * 128, mks))
                        return bl
                attention(nq, range(6),
                          lambda h, c0, c1: QT_swa3[(h % 2) * 64:(h % 2) * 64 + 64, h // 2, c0:c1],
                          lambda h, kb: KT_swa3[(h % 2) * 64:(h % 2) * 64 + 64, h // 3, kb * 128:(kb + 1) * 128],
                          lambda h, kb: V_swa4[:, kb, h // 3, (0 if h % 2 == 0 else 32):(128 if h % 2 == 0 else 160)],
                          swa_blocks, SWA_SCALE, l, 3)
                attention_flush()
                for j in range(nt):
                    i = t0 + j
                    for dh in range(2):
                        pbk = dh
                        for f in range(8):
                            lhs = mixT3[:, f, j * 128:(j + 1) * 128] if f < 6 else convT3[:, f - 6, i * 128:(i + 1) * 128]
                            mm(psum[pbk][:, :], lhs, wo3[:, f, dh * 512:(dh + 1) * 512], f == 0, f == 7,
                               ["mixT", "convT", "wo"], ["ps%d" % pbk])
                        dve(_R("tensor_tensor",
                            out=tmpf[:, dh * 512:(dh + 1) * 512], in0=psum[pbk][:, :], in1=Gb[:, dh * 512:(dh + 1) * 512],
                            op=ALU.mult), ["ps%d" % pbk, "gb"], ["tmpf"])
                        pool(_R("tensor_tensor",
                            out=xc3[:, j, dh * 512:(dh + 1) * 512], in0=xc3[:, j, dh * 512:(dh + 1) * 512],
                            in1=tmpf[:, dh * 512:(dh + 1) * 512], op=ALU.add), ["tmpf", "xc"], ["xc"])
                dma("sp", xmid_d[t0 * 128:(t0 + nt) * 128, :].rearrange("(n p) d -> p n d", p=128), xc3[:, 0:nt, :],
                    ["xc"], ["xmid"])
            P.barrier()
            if STOP <= 3:
                continue

            has_ctx = (l == 0)
            tlo = 0 if has_ctx else 2
            ar.off = 0
            AB2 = ar.alloc(4 * 2 * 8, F32)
            X = ar.alloc(NT * D, F32, "X")
            X3 = X.rearrange("p (n d) -> p n d", n=NT)
            G2 = [ar.alloc(D, F32), ar.alloc(D, F32)]
            wsel = ar.alloc(NT * NE, F32, "wsel")
            wsel3 = wsel.rearrange("p (t e) -> p t e", t=NT)
            maskf = ar.alloc(NT * NE, F32, "maskf")
            maskf3 = maskf.rearrange("p (t e) -> p t e", t=NT)
            rank = ar.alloc(16 * NE, F32, "rank")
            rank3 = rank.rearrange("p (t e) -> p t e", t=16)
            xn_all = ar.alloc(16 * D, BF, "xn_all")
            h2c = ar.alloc(8 * 256, BF, "h2c")
            h2c3 = h2c.rearrange("p (k t) -> p k t", k=8)
            c2_off = ar.off
            h2T = ar.alloc(8 * NTOK, BF, "h2T")
            h2T3 = h2T.rearrange("p (k t) -> p k t", k=8)
            maskb = ar.alloc(NT * NE, BF, "maskb")
            maskb3 = maskb.rearrange("p (t e) -> p t e", t=NT)
            xn = [ar.alloc(D, BF), ar.alloc(D, BF)]
            junk = ar.alloc(D, BF, "junk")
            tmpf = ar.alloc(D, F32, "tmpf")
            ss = ar.alloc(8, F32, "ss")
            gtmp = ar.alloc(512, F32, "gtmp")
            rwb = ar.alloc(8 * NE, BF, "rwb")
            rwb3 = rwb.rearrange("p (k e) -> p k e", k=8)
            aff = ar.alloc(NT * NE, F32, "aff")
            aff3 = aff.rearrange("p (t e) -> p t e", t=NT)
            cmpb = ar.alloc(NT * NE, BF, "cmpb")
            cmp3 = cmpb.rearrange("p (t e) -> p t e", t=NT)
            mx = ar.alloc(NT, F32, "mx")
            lo = ar.alloc(32, F32, "lo")
            c32 = ar.alloc(32, F32, "c32")
            xc3 = X3

            dma("sp", X3[:, tlo:NT, :], xmid_d[tlo * 128:NTOK, :].rearrange("(n p) d -> p n d", p=128), ["xmid"], ["xc"])
            dma("pool", rwb3[:, :, :], rw_d[l].rearrange("(k p) e -> p k e", p=128), [], ["rwb"])
            if has_ctx:
                gbuild(G2[0], modcol(l, 5, NS), 7)
            gbuild(G2[1], modcol(l, 5, s), 7)
            for (t0, nt) in chunks:
                if t0 == 0 and not has_ctx:
                    continue
                xc3 = X3[:, t0:t0 + nt, :]
                keep = None
                if t0 >= 2:
                    keep = (lambda j, t0=t0: (xn_all[:, (t0 - 2 + j) * D:(t0 - 1 + j) * D], "xnall"))
                norm_T(t0, nt, 1, h2T3, t0 * 128, 0, keep=keep)
            xc3 = X3
            first = True
            for i in range(tlo, NT):
                for k in range(8):
                    mm(psum[2][:, i * NE:(i + 1) * NE], h2T3[:, k, i * 128:(i + 1) * 128], rwb3[:, k, :],
                       first, (i == NT - 1 and k == 7), ["hT", "rwb"], ["ps2"])
                    first = False
            T0 = tlo
            nT = NT - tlo
            lg3 = psum[2][:, T0 * NE:NT * NE].rearrange("p (t e) -> p t e", e=NE)
            a3 = aff3[:, T0:NT, :]
            dve(_R("tensor_reduce", out=mx[:, T0:NT], in_=lg3, axis=AX.X, op=ALU.max), ["ps2"], ["mx"])
            dve(_R("tensor_tensor", out=a3, in0=lg3, in1=mx[:, T0:NT].unsqueeze(2).broadcast_to([128, nT, NE]),
                                          op=ALU.subtract), ["ps2", "mx"], ["aff"])
            act(_R("activation", out=a3, in_=a3, func=AF.Exp), ["aff"], ["aff"])
            dve(_R("tensor_reduce", out=mx[:, T0:NT], in_=a3, axis=AX.X, op=ALU.add), ["aff"], ["mx"])
            dve(_R("reciprocal", out=mx[:, T0:NT], in_=mx[:, T0:NT]), ["mx"], ["mx"])
            dve(_R("tensor_tensor", out=a3, in0=a3, in1=mx[:, T0:NT].unsqueeze(2).broadcast_to([128, nT, NE]),
                                          op=ALU.mult), ["aff", "mx"], ["aff"])
            dve(_R("memset", lo[:, :], 0.0), [], ["lo"])
            segs = ([(0, 2, 0)] if has_ctx else []) + [(2, 16, 16)]
            for it in range(NBIS):
                hstep = 2.0 ** -(it + 1)
                for (ta, tn, lc) in segs:
                    dve(_R("scalar_tensor_tensor",
                        out=cmp3[:, ta:ta + tn, :], in0=aff3[:, ta:ta + tn, :], scalar=-hstep,
                        in1=lo[:, lc:lc + 16].unsqueeze(1).broadcast_to([128, tn, NE]), op0=ALU.add, op1=ALU.is_ge),
                        ["aff", "lo"], ["cmp"])
                mm(psum[3][:, T0 * NE:NT * NE], ones_b[:, :], cmpb[:, T0 * NE:NT * NE], True, True, ["cmp", "ones_b"], ["ps3"])
                for (ta, tn, lc) in segs:
                    dve(_R("tensor_reduce",
                        out=c32[:, lc:lc + 16],
                        in_=psum[3][:, ta * NE:(ta + tn) * NE].rearrange("p (t e) -> p e t", e=NE),
                        axis=AX.X, op=ALU.add), ["ps3"], ["c32"])
                lsl = slice(0 if has_ctx else 16, 32)
                dve(_R("tensor_tensor", out=c32[:, lsl], in0=c32[:, lsl], in1=kvec[:, lsl], op=ALU.is_ge),
                    ["c32", "kvec"], ["c32"])
                dve(_R("scalar_tensor_tensor",
                    out=lo[:, lsl], in0=c32[:, lsl], scalar=hstep, in1=lo[:, lsl], op0=ALU.mult, op1=ALU.add),
                    ["c32", "lo"], ["lo"])
            for (ta, tn, lc) in segs:
                dve(_R("tensor_tensor", out=maskf3[:, ta:ta + tn, :], in0=aff3[:, ta:ta + tn, :],
                       in1=lo[:, lc:lc + 16].unsqueeze(1).broadcast_to([128, tn, NE]), op=ALU.is_ge), ["aff", "lo"], ["maskf"])
            dve(_R("tensor_tensor", out=wsel3[:, T0:NT, :], in0=maskf3[:, T0:NT, :], in1=a3, op=ALU.mult),
                ["maskf", "aff"], ["wsel"])
            dve(_R("tensor_copy", out=maskb3[:, 2:NT, :], in_=maskf3[:, 2:NT, :]), ["maskf"], ["maskb"])
            firstr = True
            for li in range(16):
                mm(psum[3][:, li * NE:(li + 1) * NE], masks[:, 256:384], maskb3[:, 2 + li, :], firstr, False,
                   ["maskb", "masks"], ["ps3"])
                firstr = False
                for l2 in range(li):
                    mm(psum[3][:, li * NE:(li + 1) * NE], ones_b[:, :], maskb3[:, 2 + l2, :], False,
                       (li == 15 and l2 == li - 1), ["maskb", "ones_b"], ["ps3"])
            dve(_R("tensor_copy", out=rank[:, :], in_=psum[3][:, 0:16 * NE]), ["ps3"], ["rank"])
            if has_ctx:
                pool(_R("tensor_copy", out=h2c3, in_=h2T3[:, :, 0:256]), ["hT"], ["h2c"])
            dump("aff", aff[:, :])
            dump("wsel", wsel[:, :])

            P.barrier()
            ar.off = c2_off
            wgb = ar.alloc(8 * 512, BF, "wgb")
            wub = ar.alloc(8 * 512, BF, "wub")
            wdf = ar.alloc(D, F32, "wdf")
            wdb = ar.alloc(4 * D, BF, "wdb")
            wdc = ar.alloc(4 * D, BF, "wdc")
            Sel = ar.alloc(16 * 256, BF, "Sel")
            Sel3 = Sel.rearrange("p (t s) -> p t s", t=16)
            SelWT = ar.alloc(2 * 2048, BF, "SelWT")
            SelWT3 = SelWT.rearrange("p (c t) -> p c t", c=2)
            selw = [ar.alloc(256, BF) for _ in range(2)]
            xgT = ar.alloc(8 * 256, BF, "xgT")
            xgT3 = xgT.rearrange("p (k t) -> p k t", k=8)
            ysb = ar.alloc(2 * D, BF, "ysb")
            ysb3 = ysb.rearrange("p (c d) -> p c d", c=2)
            hid = ar.alloc(4 * 256, BF, "hid")
            hid3 = hid.rearrange("p (f t) -> p f t", f=4)
            sgb = ar.alloc(256, F32, "sgb")
            mx = ar.alloc(NT, F32, "mx2")
            junk = SelWT[:, 0:D]
            wg3 = wgb.rearrange("p (k f) -> p k f", k=8)
            wu3 = wub.rearrange("p (k f) -> p k f", k=8)
            wd3 = wdb.rearrange("p (f d) -> p f d", f=4)
            wdc3 = wdc.rearrange("p (f d) -> p f d", f=4)
            A2l = ABv(2, 1)
            B2l = ABv(3, 1)

            def ffn_hidden(rhs_of_k, n, extra=None):
                for fc in range(4):
                    pg = 2 * (fc % 2)
                    for k in range(8):
                        mm(psum[pg][:, 0:n], wg3[:, k, fc * 128:(fc + 1) * 128], rhs_of_k(k), k == 0, k == 7,
                           ["xgT", "h2c", "wg"], ["ps%d" % pg])
                    for k in range(8):
                        mm(psum[pg + 1][:, 0:n], wu3[:, k, fc * 128:(fc + 1) * 128], rhs_of_k(k), k == 0, k == 7,
                           ["xgT", "h2c", "wu"], ["ps%d" % (pg + 1)])
                    act(_R("activation", out=sgb[:, 0:n], in_=psum[pg][:, 0:n], func=AF.Silu), ["ps%d" % pg], ["sg0"])
                    dve(_R("tensor_tensor", out=hid3[:, fc, 0:n], in0=sgb[:, 0:n], in1=psum[pg + 1][:, 0:n], op=ALU.mult),
                        ["sg0", "ps%d" % (pg + 1)], ["hid"])
                    if extra is not None:
                        extra(fc)

            srot = [0]

            def load_weights(ex):
                dma("pool", wg3, wg_d[l, ex].rearrange("(k p) f -> p k f", p=128), [], ["wg"])
                dma("pool", wu3, wu_d[l, ex].rearrange("(k p) f -> p k f", p=128), [], ["wu"])
                for fc in range(4):
                    dma("sp", wdf[:, :], wd_d[l, ex, fc * 128:(fc + 1) * 128, :], [], ["wdf"])
                    pool(_R("tensor_tensor", out=wdb[:, fc * D:(fc + 1) * D], in0=wdf[:, :], in1=G2[1][:, :], op=ALU.mult),
                         ["wdf", "gb"], ["wd"])
                    if has_ctx:
                        pool(_R("tensor_tensor", out=wdc[:, fc * D:(fc + 1) * D], in0=wdf[:, :], in1=G2[0][:, :],
                                op=ALU.mult), ["wdf", "gb"], ["wdc"])

            def build_sel(ex, lis):
                for li in lis:
                    dve(_R("tensor_scalar", out=Sel3[:, li, :], in0=iota_f[:, :], scalar1=rank3[:, li, ex:ex + 1],
                           scalar2=maskf3[:, 2 + li, ex:ex + 1], op0=ALU.is_equal, op1=ALU.mult),
                        ["iota_f", "rank", "maskf"], ["Sel"])

            def ctx_dense(ex):
                ffn_hidden(lambda k: h2c3[:, k, 0:256], 256)
                for j in range(2):
                    for dh in range(2):
                        py = 4 + ((j * 2 + dh) % 2)
                        for fc in range(4):
                            mm(psum[py][:, :], hid3[:, fc, j * 128:(j + 1) * 128], wdc3[:, fc, dh * 512:(dh + 1) * 512],
                               fc == 0, fc == 3, ["hid", "wdc"], ["ps%d" % py])
                        dve(_R("scalar_tensor_tensor", out=X3[:, j, dh * 512:(dh + 1) * 512], in0=psum[py][:, :],
                               scalar=wsel3[:, j, ex:ex + 1], in1=X3[:, j, dh * 512:(dh + 1) * 512],
                               op0=ALU.mult, op1=ALU.add), ["ps%d" % py, "wsel", "xc"], ["xc"])

            def gather_k(ex, k):
                pb = k % 2
                for li in range(16):
                    mm(psum[pb][:, 0:256], xn_all[:, li * D + k * 128:li * D + (k + 1) * 128], Sel3[:, li, :],
                       li == 0, li == 15, ["xnall", "Sel"], ["ps%d" % pb])
                act(_R("activation", out=xgT3[:, k, :], in_=psum[pb][:, 0:256], func=AF.Identity,
                       scale=A2l[:, k:k + 1], bias=B2l[:, k:k + 1]), ["ps%d" % pb, "AB"], ["xgT"])

            def down_proj(ex):
                for sc in range(2):
                    for dh in range(2):
                        py = 4 + ((sc * 2 + dh) % 2)
                        for fc in range(4):
                            mm(psum[py][:, :], hid3[:, fc, sc * 128:(sc + 1) * 128], wd3[:, fc, dh * 512:(dh + 1) * 512],
                               fc == 0, fc == 3, ["hid", "wd"], ["ps%d" % py])
                        act(_R("copy", out=ysb3[:, sc, dh * 512:(dh + 1) * 512], in_=psum[py][:, :]), ["ps%d" % py], ["ysb"])

            def selw_tr(ex, li):
                sw = selw[li % 2]
                swn = "selw%d" % (li % 2)
                dve(_R("tensor_scalar", out=sw[:, :], in0=iota_f[:, :], scalar1=rank3[:, li, ex:ex + 1],
                       scalar2=wsel3[:, 2 + li, ex:ex + 1], op0=ALU.is_equal, op1=ALU.mult),
                    ["iota_f", "rank", "wsel"], [swn])
                for sc in range(2):
                    tr(psb(6)[:, ((li % 4) * 2 + sc) * 128:((li % 4) * 2 + sc + 1) * 128], sw[:, sc * 128:(sc + 1) * 128],
                       ident_b[:, :], [swn, "ident_b"], ["ps6"])
                if li % 4 == 3:
                    for sc in range(2):
                        act(_R("copy", out=SelWT3[:, sc, (li - 3) * 128:(li + 1) * 128].rearrange("p (i t) -> p i t", i=4),
                               in_=psb(6)[:, :].rearrange("p (i c t) -> p i c t", i=4, c=2)[:, :, sc, :]),
                            ["ps6"], ["SelWT"])

            def scatter_tile(ex, li):
                for dh in range(2):
                    py = (7, 3)[srot[0] % 2]
                    srot[0] += 1
                    for sc in range(2):
                        mm(psum[py][:, :], SelWT3[:, sc, li * 128:(li + 1) * 128], ysb3[:, sc, dh * 512:(dh + 1) * 512],
                           sc == 0, sc == 1, ["SelWT", "ysb"], ["ps%d" % py])
                    dve(_R("tensor_tensor", out=X3[:, 2 + li, dh * 512:(dh + 1) * 512], in0=psum[py][:, :],
                           in1=X3[:, 2 + li, dh * 512:(dh + 1) * 512], op=ALU.add), ["ps%d" % py, "xc"], ["xc"])

            load_weights(0)
            build_sel(0, range(16))
            for ex in range(NE):
                for k in range(8):
                    gather_k(ex, k)
                    if ex > 0:
                        scatter_tile(ex - 1, 2 * k)
                        scatter_tile(ex - 1, 2 * k + 1)
                if has_ctx:
                    ctx_dense(ex)
                ffn_hidden(lambda k: xgT3[:, k, :], 256,
                           extra=(lambda fc, ex=ex: build_sel(ex + 1, range(4 * fc, 4 * fc + 4))) if ex + 1 < NE else None)
                down_proj(ex)
                if ex + 1 < NE:
                    load_weights(ex + 1)
                for li in range(16):
                    selw_tr(ex, li)
            for li in range(16):
                scatter_tile(NE - 1, li)
            if l == 0:
                dma("sp", xl1_d.rearrange("(n p) d -> p n d", p=128), X3[:, :, :], ["xc"], ["xsrc"])
            else:
                dve(_R("memset", mx[:, :], 0.0), [], ["mx"])
                for i in range(2, NT):
                    act(_R("activation", out=junk[:, :], in_=X3[:, i, :], func=AF.Square, scale=1.0 / 32.0,
                                                    accum_out=mx[:, i:i + 1]), ["xc", "mx"], ["junk", "mxa%d" % i])
                rsq(mx[:, 2:NT], ["mx"] + ["mxa%d" % i for i in range(2, NT)], ["mx"])
                for i in range(2, NT):
                    act(_R("activation", out=X3[:, i, :], in_=X3[:, i, :], func=AF.Copy, scale=mx[:, i:i + 1]),
                        ["xc", "mx"], ["xc"])
                    pool(_R("tensor_tensor", out=X3[:, i, :], in0=X3[:, i, :], in1=fgb[:, :], op=ALU.mult),
                         ["xc", "fgb"], ["xc"])
                dma("sp", out_d[s].rearrange("(n p) d -> p n d", p=128), X3[:, 2:NT, :], ["xc"], ["out"])
            P.barrier()

    P.barrier()
    P.emit(nc, stack)
    stack.close()
    return nc


def _consts():
    ident = np.eye(128, dtype=np.float32)
    k = np.arange(128)[:, None]
    q = np.arange(128)[None, :]
    masks = np.concatenate([(k >= q), (k <= q), (k < q)], axis=1).astype(np.float32)
    rows = SEQ // 64
    row = np.repeat(np.arange(rows, dtype=np.float32), 64)
    col = np.tile(np.arange(64, dtype=np.float32), rows)

    def table(rot):
        nf = rot // 4
        inv = (np.float32(10000.0) ** (-np.arange(nf, dtype=np.float32) / np.float32(nf))).astype(np.float32)
        ang = np.concatenate([row[:, None] * inv, col[:, None] * inv], axis=-1).astype(np.float32)
        cs, sn = np.cos(ang), np.sin(ang)
        t = np.concatenate([cs, cs, sn, sn], axis=-1).astype(np.float32)
        return np.ascontiguousarray(t.reshape(16, 128, 2 * rot).transpose(1, 0, 2).reshape(128, 32 * rot))

    iota = np.ascontiguousarray(np.broadcast_to(np.arange(256, dtype=np.float32)[None, :], (128, 256)))
    return ident, masks, table(32), table(64), iota


def kernel(**inputs):
    NS = 4
    ident, masks, ropem, ropes, iota = _consts()
    nc = build(NS=NS, NL=2)
    f = lambda a: np.ascontiguousarray(np.asarray(a, dtype=np.float32))
    shared = {k: f(inputs[k]) for k in ("ada_w", "ada_b", "norm1_g", "w_in", "mla_q_norm_g", "mla_w_uq",
                                        "mla_kv_norm_g", "mla_w_ukv", "swa_sink", "conv_w", "w_o", "norm2_g",
                                        "router_w", "exp_w_gate", "exp_w_up", "exp_w_down", "final_norm_g")}
    shared.update(k_ident=ident, k_masks=masks, k_ropem=ropem, k_ropes=ropes, k_iota=iota)
    x = f(inputs["x"])
    ctx = f(inputs["ctx"])
    c = f(inputs["c"])
    c_ctx = f(inputs["c_ctx"])
    in_maps = []
    for core in range(N_CORES):
        sl = slice(core * NS, (core + 1) * NS)
        m = dict(shared)
        m["x"] = np.ascontiguousarray(x[sl])
        m["ctx"] = np.ascontiguousarray(ctx[sl])
        m["cc"] = np.ascontiguousarray(np.concatenate([c[sl], c_ctx[None, :]], axis=0))
        in_maps.append(m)
    res = run_bass_kernel_spmd(nc, in_maps, core_ids=list(range(N_CORES)))
    return np.concatenate([r["out"] for r in res.results], axis=0)
```

```python
import math
from contextlib import ExitStack
import numpy as np
import concourse.bass as bass
import concourse.mybir as mybir
from concourse.bass_utils import run_bass_kernel_spmd

F32 = mybir.dt.float32
BF = mybir.dt.bfloat16
AF = mybir.ActivationFunctionType
ALU = mybir.AluOpType
AX = mybir.AxisListType

D = 1024
SEQ = 2048
CTX = 256
NT = 18
NTOK = NT * 128
EPS = 1e-6
MLA_SCALE = 96 ** -0.5
SWA_SCALE = 64 ** -0.5
NE = 16
NBIS = 22
N_CORES = 8

EP = 60000
EPD = 3900
NDMA = 8


_POS = {"memset": ("ap", "constant"), "matmul": ("out", "lhsT", "rhs"), "transpose": ("out", "in_", "identity")}


def _R(meth, *a, **k):
    for nm, v in zip(_POS.get(meth, ()), a):
        k[nm] = v
    return (meth, k)


class Prog:
    def __init__(self):
        self.streams = {e: [] for e in ("pe", "act", "dve", "pool", "sp")}
        self.count = {}
        self.lastw = {}
        self.readers = {}
        self.seen = {e: {} for e in self.streams}
        self.waited = {}
        self.rr = 0
        self.rrq = {}

    def _collect(self, eng, r, w, is_dma):
        deps = {}

        def add(t, same_ok):
            if t is None:
                return
            g, i = t
            if g == eng and not same_ok:
                return
            if deps.get(g, 0) < i:
                deps[g] = i

        for x in r:
            add(self.lastw.get(x), is_dma or eng != "pe")
        for x in w:
            add(self.lastw.get(x), is_dma)
            for g, i in self.readers.get(x, {}).items():
                add((g, i), is_dma)
        return deps

    def _waits(self, eng, deps):
        waits = []
        for g, i in deps.items():
            if self.seen[eng].get(g, 0) >= i:
                continue
            self.seen[eng][g] = i
            waits.append((g, i))
            self.waited.setdefault(g, set()).add(i)
        return waits

    def _mark(self, t, r, w):
        for x in r:
            d = self.readers.setdefault(x, {})
            if d.get(t[0], 0) < t[1]:
                d[t[0]] = t[1]
        for x in w:
            self.lastw[x] = t
            self.readers[x] = {}

    limit = None
    nrec = 0

    def _over(self):
        self.nrec += 1
        return self.limit is not None and self.nrec > self.limit

    def op(self, eng, fn, r=(), w=()):
        if self._over():
            return
        deps = self._collect(eng, r, w, False)
        waits = self._waits(eng, deps)
        n = self.count.get(eng, 0) + 1
        self.count[eng] = n
        self.streams[eng].append(("c", fn, waits, (eng, n)))
        self._mark((eng, n), r, w)

    def dma(self, q, fn, r=(), w=()):
        if self._over():
            return
        k = self.rrq.get(q, 0)
        self.rrq[q] = (k + 1) % NDMA
        g = "q%s%d" % (q, k)
        deps = self._collect(q, r, w, True)
        n = self.count.get(g, 0) + 1
        if n > 1:
            deps[g] = max(deps.get(g, 0), n - 1)
        waits = self._waits(q, deps)
        self.count[g] = n
        self.streams[q].append(("d", fn, waits, (g, n)))
        self._mark((g, n), r, w)

    def barrier(self):
        for e in self.streams:
            deps = {g: c for g, c in self.count.items() if g != e and c > 0}
            waits = self._waits(e, deps)
            if waits:
                self.streams[e].append(("w", None, waits, None))

    def emit(self, nc, stack):
        rank = {}
        nsem = {}
        for g, c in self.count.items():
            if g.startswith("q"):
                nsem[g] = (c + EPD - 1) // EPD
            else:
                ws = sorted(self.waited.get(g, ()))
                rank[g] = {i: k + 1 for k, i in enumerate(ws)}
                nsem[g] = max(1, (len(ws) + EP - 1) // EP)
        sems = {g: [stack.enter_context(nc.semaphore("s_%s_%d" % (g, k))) for k in range(n)]
                for g, n in nsem.items()}

        def semval(g, i):
            if g.startswith("q"):
                ep = (i - 1) // EPD
                return sems[g][ep], (i - ep * EPD) * 16
            v = rank[g][i]
            ep = (v - 1) // EP
            return sems[g][ep], v - ep * EP

        def replay(name, e):
            for kind, fn, waits, t in self.streams[name]:
                for g, i in waits:
                    s, v = semval(g, i)
                    e.wait_ge(s, v)
                if kind == "w":
                    continue
                ins = getattr(e, fn[0])(**fn[1])
                if kind == "d":
                    s, _ = semval(*t)
                    ins.then_inc(s, 16)
                elif t[1] in rank.get(t[0], {}):
                    s, _ = semval(*t)
                    ins.then_inc(s, 1)

        block = stack.enter_context(nc.Block())

        @block.tensor
        def _(e):
            replay("pe", e)

        @block.scalar
        def _(e):
            replay("act", e)

        @block.vector
        def _(e):
            replay("dve", e)

        @block.gpsimd
        def _(e):
            replay("pool", e)

        @block.sync
        def _(e):
            replay("sp", e)


LAYOUT = {}


class Arena:
    def __init__(self, t):
        self.t = t
        self.off = 0

    def alloc(self, nelem, dt, name=None):
        nb4 = nelem if dt == F32 else (nelem + 1) // 2
        if name is not None:
            LAYOUT[name] = (self.off, nelem, "f32" if dt == F32 else "bf16")
        v = self.t[:, self.off:self.off + nb4]
        self.off += nb4
        LAYOUT["_peak"] = max(LAYOUT.get("_peak", 0), self.off)
        assert self.off <= self.t.shape[1], ("arena overflow", self.off)
        if dt == F32:
            return v
        return v.bitcast(dt)[:, 0:nelem]


class _Stop(Exception):
    pass


NOROPE = False


def build(NS=4, NL=2, dumps=None, STOP=99, LIMIT=None):
    dumps = dumps or {}
    nc = bass.Bass("TRN2", target_bir_lowering=False)
    P = Prog()
    P.limit = LIMIT
    stack = ExitStack()

    def din(name, shape):
        return nc.dram_tensor(name, list(shape), F32, kind="ExternalInput").ap()

    x_d = din("x", (NS, SEQ, D))
    ctx_d = din("ctx", (NS, CTX, D))
    cc_d = din("cc", (NS + 1, D))
    ada_w = din("ada_w", (2, D, 6 * D))
    ada_b = din("ada_b", (2, 6 * D))
    n1g = din("norm1_g", (2, D))
    w_in = din("w_in", (2, D, 1824))
    qg_d = din("mla_q_norm_g", (2, 256))
    w_uq = din("mla_w_uq", (2, 256, 576))
    kvg_d = din("mla_kv_norm_g", (2, 128))
    w_ukv = din("mla_w_ukv", (2, 128, 768))
    sink_d = din("swa_sink", (2, 6))
    convw_d = din("conv_w", (2, 3, 256))
    w_o = din("w_o", (2, D, D))
    n2g = din("norm2_g", (2, D))
    rw_d = din("router_w", (2, D, NE))
    wg_d = din("exp_w_gate", (2, NE, D, 512))
    wu_d = din("exp_w_up", (2, NE, D, 512))
    wd_d = din("exp_w_down", (2, NE, 512, D))
    fg_d = din("final_norm_g", (D,))
    k_ident = din("k_ident", (128, 128))
    k_masks = din("k_masks", (128, 384))
    k_iota = din("k_iota", (128, 256))
    k_ropem = din("k_ropem", (128, 16 * 64))
    k_ropes = din("k_ropes", (128, 16 * 128))
    out_d = nc.dram_tensor("out", [NS, SEQ, D], F32, kind="ExternalOutput").ap()
    xmid_d = nc.dram_tensor("xmid", [NTOK, D], F32, kind="Internal").ap()
    xl1_d = nc.dram_tensor("xl1", [NTOK, D], F32, kind="Internal").ap()
    dump_d = {k: nc.dram_tensor("dbg_" + k, list(shp), F32, kind="ExternalOutput").ap()
              for k, shp in dumps.items()}

    def sb(name, shape, dt):
        return stack.enter_context(nc.sbuf_tensor(name, list(shape), dt))

    ident_f = sb("ident_f", (128, 128), F32)
    ident_b = sb("ident_b", (128, 128), BF)
    ones_f = sb("ones_f", (128, 128), F32)
    ones_b = sb("ones_b", (128, 128), BF)
    masks = sb("masks", (128, 384), BF)
    iota_f = sb("iota_f", (128, 256), F32)
    ropem = sb("ropem", (128, 1024), F32)
    ropes = sb("ropes", (128, 2048), F32)
    modT = sb("modT", (128, 2 * 48 * 5), F32)
    gT = sb("gT", (128, 2 * 2 * 8), F32)
    qgT = sb("qgT", (128, 4), F32)
    kvgT = sb("kvgT", (128, 2), F32)
    cwT = sb("cwT", (128, 2 * 2 * 3), F32)
    esink = sb("esink", (128, 12), F32)
    abT = sb("abT", (128, 2 * 48), F32)
    fgb = sb("fgb", (128, D), F32)
    kvec = sb("kvec", (128, 32), F32)
    AR = sb("arena", (128, 47300), F32)
    ar = Arena(AR)
    psum = [stack.enter_context(nc.psum_tensor("ps%d" % i, [128, 512], F32)) for i in range(8)]

    def psb(i):
        return psum[i][:, :].bitcast(BF)

    def act(fn, r, w):
        P.op("act", fn, r, w)

    def dve(fn, r, w):
        P.op("dve", fn, r, w)

    def pool(fn, r, w):
        P.op("pool", fn, r, w)

    def mm(out, lhsT, rhs, start, stop, r, w):
        P.op("pe", _R("matmul", out, lhsT, rhs, start=start, stop=stop, skip_group_check=True), r, w)

    def tr(out, in_, ident, r, w):
        P.op("pe", _R("transpose", out, in_, ident), r, w)

    def dma(q, out, in_, r, w, slow=False):
        if slow:
            P.dma(q, _R("dma_start", out=out, in_=in_, allow_slow_non_contiguous=True), r, w)
        else:
            P.dma(q, _R("dma_start", out=out, in_=in_), r, w)

    def rsq(ap, r, w):
        dve(_R("tensor_scalar", out=ap, in0=ap, scalar1=EPS, scalar2=None, op0=ALU.add), r, w)
        act(_R("activation", out=ap, in_=ap, func=AF.Ln), w, w)
        act(_R("activation", out=ap, in_=ap, func=AF.Exp, scale=-0.5), w, w)

    def dump(name, ap, idx=None):
        if name in dump_d:
            d = dump_d[name] if idx is None else dump_d[name][idx]
            P.barrier()
            dma("sp", d, ap, [], ["dump_" + name])


    dma("sp", ident_f[:, :], k_ident, [], ["ident_f"])
    dma("pool", ident_b[:, :], k_ident, [], ["ident_b"])
    dma("pool", masks[:, :], k_masks, [], ["masks"])
    dma("sp", iota_f[:, :], k_iota, [], ["iota_f"])
    dma("sp", ropem[:, :], k_ropem, [], ["ropem"])
    dma("sp", ropes[:, :], k_ropes, [], ["ropes"])
    dve(_R("memset", ones_f[:, :], 1.0), [], ["ones_f"])
    dve(_R("memset", ones_b[:, :], 1.0), [], ["ones_b"])
    dve(_R("memset", kvec[:, 0:16], 32.0), [], ["kvec"])
    dve(_R("memset", kvec[:, 16:32], 256.0), [], ["kvec"])
    ctxmgr = nc.allow_non_contiguous_dma(reason="tiny parameter vectors")
    ctxmgr.__enter__()
    for l in range(2):
        dma("sp", gT[:, (l * 2) * 8:(l * 2 + 1) * 8], n1g[l].rearrange("(k p) -> p k", p=128), [], ["gT"], slow=True)
        dma("sp", gT[:, (l * 2 + 1) * 8:(l * 2 + 2) * 8], n2g[l].rearrange("(k p) -> p k", p=128), [], ["gT"], slow=True)
        dma("sp", qgT[:, l * 2:l * 2 + 2], qg_d[l].rearrange("(k p) -> p k", p=128), [], ["qgT"], slow=True)
        dma("sp", kvgT[:, l:l + 1], kvg_d[l].rearrange("(k p) -> p k", p=128), [], ["kvgT"], slow=True)
        for c2 in range(2):
            dma("sp", cwT[:, (l * 2 + c2) * 3:(l * 2 + c2) * 3 + 3],
                convw_d[l][:, c2 * 128:(c2 + 1) * 128].rearrange("k p -> p k"), [], ["cwT"], slow=True)
        dma("sp", abT[:, l * 48:(l + 1) * 48], ada_b[l].rearrange("(j p) -> p j", p=128), [], ["abT"], slow=True)
        dma("sp", esink[:, l * 6:(l + 1) * 6], sink_d[l].partition_broadcast(128), [], ["esink"])
    ctxmgr.__exit__(None, None, None)
    dma("sp", fgb[:, :], fg_d.partition_broadcast(128), [], ["fgb"])
    act(_R("activation", out=esink[:, :], in_=esink[:, :], func=AF.Exp), ["esink"], ["esink"])

    ar.off = 0
    cc_sb = ar.alloc(D, F32, "cc_sb")
    cs_bf = ar.alloc(D, BF, "cs_bf")
    sT = ar.alloc(64, BF, "sT")
    awb = [ar.alloc(6 * D, BF), ar.alloc(6 * D, BF)]
    NB = NS + 1
    if STOP >= 1:
        dma("sp", cc_sb[0:NB, :], cc_d, [], ["cc_sb"])
        act(_R("activation", out=cs_bf[0:NB, :], in_=cc_sb[0:NB, :], func=AF.Silu), ["cc_sb"], ["cs_bf"])
        for k in range(8):
            tr(psb(0)[:, k * 8:k * 8 + NB], cs_bf[0:NB, k * 128:(k + 1) * 128], ident_b[0:NB, 0:NB],
               ["cs_bf", "ident_b"], ["ps0"])
        dve(_R("tensor_copy", out=sT[:, :].rearrange("p (k b) -> p k b", b=8)[:, :, 0:NB],
               in_=psb(0)[:, 0:64].rearrange("p (k b) -> p k b", b=8)[:, :, 0:NB]), ["ps0"], ["sT"])
        for l in range(NL):
            for k in range(8):
                b = awb[k % 2]
                dma("pool", b[:, :], ada_w[l, k * 128:(k + 1) * 128, :], [], ["awb%d" % (k % 2)])
                for j in range(48):
                    mm(psum[1][:, j * NB:(j + 1) * NB], b[:, j * 128:(j + 1) * 128], sT[:, k * 8:k * 8 + NB],
                       (k == 0 and j == 0), (k == 7 and j == 47), ["awb%d" % (k % 2), "sT"], ["ps1"])
            mview = modT[:, l * 240:l * 240 + 48 * NB].rearrange("p (j b) -> p j b", b=NB) if NB == 5 else \
                modT[:, l * 240:l * 240 + 48 * NB].rearrange("p (j b) -> p j b", b=NB)
            dve(_R("tensor_tensor",
                out=mview, in0=psum[1][:, 0:48 * NB].rearrange("p (j b) -> p j b", b=NB),
                in1=abT[:, l * 48:(l + 1) * 48].unsqueeze(2).broadcast_to([128, 48, NB]), op=ALU.add),
                ["ps1", "abT"], ["modT"])

    def modcol(l, m, b):
        return modT[:, l * 240:l * 240 + 48 * NB].rearrange("p (j b) -> p j b", b=NB)[:, m * 8:(m + 1) * 8, b]

    P.barrier()

    chunks = [(0, 2)] + [(2 + 4 * j, 4) for j in range(4)]
    LAYOUT["n1"] = P.nrec
    if STOP <= 1:
        NS = 0

    for s in range(NS):
        for l in range(NL):
            src_lat = x_d[s] if l == 0 else xl1_d[256:NTOK, :]
            src_ctx = ctx_d[s] if l == 0 else xl1_d[0:256, :]

            def src_tiles(t0, nt):
                if t0 == 0:
                    return src_ctx.rearrange("(n p) d -> p n d", p=128)
                return src_lat[(t0 - 2) * 128:(t0 - 2 + nt) * 128, :].rearrange("(n p) d -> p n d", p=128)

            ar.off = 0
            AB = ar.alloc(4 * 2 * 8, F32)
            KT_mla = ar.alloc(6 * NTOK, BF, "KT_mla")
            V_mla = ar.alloc(NT * 3 * 160, BF, "V_mla")
            KT_swa = ar.alloc(2 * NTOK, BF, "KT_swa")
            V_swa = ar.alloc(NT * 2 * 160, BF, "V_swa")
            convT = ar.alloc(2 * NTOK, BF, "convT")
            xc = ar.alloc(4 * D, F32, "xc")
            hT = ar.alloc(8 * 512, BF, "hT")
            xn = [ar.alloc(D, BF), ar.alloc(D, BF)]
            junk = ar.alloc(D, BF, "junk")
            tmpf = ar.alloc(D, F32, "tmpf")
            ss = ar.alloc(8, F32, "ss")
            gtmp = ar.alloc(512, F32, "gtmp")
            rx = ar.alloc(512, F32, "rx")
            ph_off = ar.off
            KT_mla3 = KT_mla.rearrange("p (h t) -> p h t", h=6)
            KT_swa3 = KT_swa.rearrange("p (g t) -> p g t", g=2)
            V_mla4 = V_mla.rearrange("p (i q c) -> p i q c", i=NT, q=3)
            V_swa4 = V_swa.rearrange("p (i g c) -> p i g c", i=NT, g=2)
            convT3 = convT.rearrange("p (c t) -> p c t", c=2)
            hT3 = hT.rearrange("p (k t) -> p k t", k=8)
            xc3 = xc.rearrange("p (n d) -> p n d", n=4)

            def ABv(which, seg):
                o = (which * 2 + seg) * 8
                return AB[:, o:o + 8]

            for seg in range(2):
                b = NS if seg == 0 else s
                for nrm in range(2):
                    dve(_R("scalar_tensor_tensor",
                        out=ABv(2 * nrm, seg), in0=modcol(l, 3 * nrm + 1, b), scalar=1.0,
                        in1=gT[:, (l * 2 + nrm) * 8:(l * 2 + nrm + 1) * 8], op0=ALU.add, op1=ALU.mult),
                        ["modT", "gT"], ["AB"])
                    dve(_R("tensor_copy",
                        out=ABv(2 * nrm + 1, seg), in_=modcol(l, 3 * nrm, b)), ["modT"], ["AB"])

            pool(_R("memset", V_mla[:, :], 0.0), [], ["V_mla"])
            pool(_R("memset", V_swa[:, :], 0.0), [], ["V_swa"])
            pool(_R("memset", V_mla4[:, :, :, 64:65], 1.0), [], ["V_mla"])
            pool(_R("memset", V_swa4[:, :, :, 64:65], 1.0), [], ["V_swa"])

            def gbuild(dst, col8, pbank):
                for k in range(8):
                    dve(_R("tensor_scalar", out=gtmp[:, (k % 4) * 128:(k % 4 + 1) * 128], in0=ident_f[:, :],
                                                       scalar1=col8[:, k:k + 1], scalar2=None, op0=ALU.mult),
                        ["ident_f", "modT"], ["gtmp%d" % (k % 4)])
                    mm(psum[pbank][:, (k % 4) * 128:(k % 4 + 1) * 128], ones_f[:, :],
                       gtmp[:, (k % 4) * 128:(k % 4 + 1) * 128], k % 4 == 0, k % 4 == 3,
                       ["ones_f", "gtmp%d" % (k % 4)], ["ps%d" % pbank])
                    if k % 4 == 3:
                        act(_R("copy", out=dst[:, (k - 3) * 128:(k + 1) * 128], in_=psum[pbank][:, :]),
                            ["ps%d" % pbank], ["gb"])

            def load_chunk(t0, nt):
                dma("sp", xc3[:, 0:nt, :], src_tiles(t0, nt), ["xsrc"], ["xc"])

            def norm_T(t0, nt, which, dst3, dcol0, pb0, hname="hT", keep=None, xname="xc"):
                seg = 0 if t0 == 0 else 1
                A = ABv(2 * which, seg)
                B = ABv(2 * which + 1, seg)
                dve(_R("memset", ss[:, :], 0.0), [], ["ss"])
                for j in range(nt):
                    act(_R("activation", out=junk[:, :], in_=xc3[:, j, :], func=AF.Square, scale=1.0 / 32.0,
                                                    accum_out=ss[:, j:j + 1]), [xname, "ss"], ["junk", "ss%d" % j])
                rsq(ss[:, 0:nt], ["ss"] + ["ss%d" % j for j in range(nt)], ["ss"])
                for j in range(nt):
                    xb = xn[j % 2]
                    xbn = "xn%d" % (j % 2)
                    if keep is not None:
                        xb, xbn = keep(j)
                    pb = pb0 + (j % 2)
                    act(_R("activation", out=xb[:, :], in_=xc3[:, j, :], func=AF.Copy,
                                                           scale=ss[:, j:j + 1]), [xname, "ss"], [xbn])
                    for k in range(8):
                        tr(psb(pb)[:, k * 128:(k + 1) * 128], xb[:, k * 128:(k + 1) * 128], ident_b[:, :],
                           [xbn, "ident_b"], ["ps%d" % pb])
                    dve(_R("tensor_tensor",
                        out=tmpf.rearrange("p (k t) -> p k t", k=8),
                        in0=psb(pb).rearrange("p (k t) -> p k t", k=8),
                        in1=A.unsqueeze(2).broadcast_to([128, 8, 128]), op=ALU.mult),
                        ["ps%d" % pb, "AB"], ["tmpf"])
                    dve(_R("tensor_tensor",
                        out=dst3[:, :, dcol0 + j * 128:dcol0 + (j + 1) * 128],
                        in0=tmpf.rearrange("p (k t) -> p k t", k=8),
                        in1=B.unsqueeze(2).broadcast_to([128, 8, 128]), op=ALU.add),
                        ["tmpf", "AB"], [hname])

            def rope(out3, in3, ti, kind, H, u, v, rr, ww):
                if NOROPE:
                    act(_R("copy", out=out3, in_=in3), rr, ww)
                    return
                rot = 32 if kind == "m" else 64
                half = rot // 2
                tab = ropem if kind == "m" else ropes
                cosd = tab[:, ti * 2 * rot:ti * 2 * rot + rot].unsqueeze(1).broadcast_to([128, H, rot])
                sind = tab[:, ti * 2 * rot + rot:(ti + 1) * 2 * rot].unsqueeze(1).broadcast_to([128, H, rot])
                n = H * rot
                u3 = u[:, 0:n].rearrange("p (h d) -> p h d", h=H)
                v3 = v[:, 0:n].rearrange("p (h d) -> p h d", h=H)
                x3 = rx[:, 0:n].rearrange("p (h d) -> p h d", h=H)
                act(_R("copy", out=x3, in_=in3), rr, ["rope_x"])
                pool(_R("tensor_tensor", out=u3, in0=x3, in1=cosd, op=ALU.mult), ["rope_x"] + rr, ["rope_u"])
                pool(_R("tensor_tensor", out=v3, in0=x3, in1=sind, op=ALU.mult), ["rope_x"] + rr, ["rope_v"])
                pool(_R("tensor_tensor", out=out3[:, :, 0:half], in0=u3[:, :, 0:half], in1=v3[:, :, half:rot],
                        op=ALU.subtract), ["rope_u", "rope_v"], ww)
                pool(_R("tensor_tensor", out=out3[:, :, half:rot], in0=u3[:, :, half:rot], in1=v3[:, :, 0:half],
                        op=ALU.add), ["rope_u", "rope_v"], ww)

            ar.off = ph_off
            wA = ar.alloc(8 * 1184, BF, "wA")
            wA3 = wA.rearrange("p (k c) -> p k c", k=8)
            wkv_f = ar.alloc(768, F32, "wkv_f")
            wkv = ar.alloc(768, BF, "wkv")
            zT = ar.alloc(2 * 2308, BF, "zT")
            zT3 = zT.rearrange("p (c t) -> p c t", c=2)
            BT = ar.alloc(2 * NTOK, BF, "BT")
            BT3 = BT.rearrange("p (c t) -> p c t", c=2)
            ckvnb = [ar.alloc(128, BF) for _ in range(2)]
            ckvnTb = [ar.alloc(128, BF) for _ in range(2)]
            krb = [ar.alloc(32, BF) for _ in range(2)]
            ktokb = [ar.alloc(6 * 96, BF) for _ in range(2)]
            kswb = [ar.alloc(128, BF) for _ in range(2)]
            kdupb = [ar.alloc(256, BF) for _ in range(2)]
            sskb = [ar.alloc(2, F32) for _ in range(2)]
            hT_b = ar.alloc(8 * 512, BF, "hT_b")
            hTdb = [hT3, hT_b.rearrange("p (k t) -> p k t", k=8)]
            ru = ar.alloc(512, F32, "ru")
            rv = ar.alloc(512, F32, "rv")
            tmpC = ar.alloc(512, F32, "tmpC")
            ssk = ar.alloc(2, F32, "ssk")

            w_l = w_in[l].rearrange("(k p) c -> p k c", p=128)
            dma("pool", wA3[:, :, 0:160], w_l[:, :, 256:416], [], ["wA"])
            dma("pool", wA3[:, :, 160:416], w_l[:, :, 800:1056], [], ["wA"])
            dma("pool", wA3[:, :, 416:1184], w_l[:, :, 1056:1824], [], ["wAc"])
            with nc.allow_non_contiguous_dma(reason="head-split weight layout"):
                for two in range(2):
                    dma("sp", wkv_f[:, two * 384:(two + 1) * 384].rearrange("p (h d) -> p h d", h=6),
                        w_ukv[l].rearrange("r (h two d) -> r two h d", h=6, two=2)[:, two], [], ["wkv_f"], slow=True)
            dve(_R("tensor_scalar", out=wkv[:, :], in0=wkv_f[:, :], scalar1=kvgT[:, l:l + 1], scalar2=None,
                                          op0=ALU.mult), ["wkv_f", "kvgT"], ["wkv"])
            pool(_R("memset", zT[:, :], 0.0), [], ["zT"])

            def zcol(t):
                return 1 + t if t < 256 else 3 + t

            def kvM0(j, i, hTb, hname):
                p = i % 2
                hsl = slice(j * 128, (j + 1) * 128)
                pa = 2 + p
                pan = "ps%d" % pa
                for k in range(8):
                    mm(psum[pa][:, 0:416], hTb[:, k, hsl], wA3[:, k, 0:416], k == 0, k == 7, [hname, "wA"], [pan])
                dve(_R("memset", sskb[p][:, :], 0.0), [], ["ssk%d" % p])
                act(_R("activation", out=junk[:, 0:128], in_=psum[pa][:, 0:128], func=AF.Square,
                       scale=128 ** -0.5, accum_out=sskb[p][:, 0:1]), [pan, "ssk%d" % p], ["junk", "ssk%d" % p])
                rsq(sskb[p][:, 0:1], ["ssk%d" % p], ["ssk%d" % p])
                act(_R("activation", out=ckvnb[p][:, :], in_=psum[pa][:, 0:128], func=AF.Copy,
                       scale=sskb[p][:, 0:1]), [pan, "ssk%d" % p], ["ckvn%d" % p])
                if i >= 2:
                    ti = i - 2
                    rope(krb[p].rearrange("p (h d) -> p h d", h=1),
                         psum[pa][:, 128:160].rearrange("p (h d) -> p h d", h=1), ti, "m", 1, ru, rv,
                         [pan, "ropem"], ["kr%d" % p])
                    rope(kswb[p].rearrange("p (h d) -> p h d", h=2),
                         psum[pa][:, 160:288].rearrange("p (h d) -> p h d", h=2), ti, "s", 2, ru, rv,
                         [pan, "ropes"], ["ksw%d" % p])
                else:
                    act(_R("copy", out=krb[p][:, :], in_=psum[pa][:, 128:160]), [pan], ["kr%d" % p])
                    act(_R("copy", out=kswb[p][:, :], in_=psum[pa][:, 160:288]), [pan], ["ksw%d" % p])
                pool(_R("tensor_copy", out=kdupb[p].rearrange("p (g c d) -> p g c d", g=2, c=2),
                        in_=kswb[p].rearrange("p (g d) -> p g d", g=2).unsqueeze(2).broadcast_to([128, 2, 2, 64])),
                     ["ksw%d" % p], ["kdup%d" % p])
                for c0 in (0, 96):
                    act(_R("copy", out=V_swa4[:, i, :, c0:c0 + 64],
                           in_=psum[pa][:, 288:416].rearrange("p (g d) -> p g d", g=2)), [pan], ["V_swa"])

            def kvM1(j, i, hTb, hname):
                p = i % 2
                cols = slice(i * 128, (i + 1) * 128)
                tr(psb(4)[:, 0:128], ckvnb[p][:, :], ident_b[:, :], ["ckvn%d" % p, "ident_b"], ["ps4"])
                for g in range(2):
                    tr(psb(4)[:, 128 + g * 128:256 + g * 128], kdupb[p][:, g * 128:(g + 1) * 128], ident_b[:, :],
                       ["kdup%d" % p, "ident_b"], ["ps4"])
                act(_R("copy", out=ckvnTb[p][:, :], in_=psb(4)[:, 0:128]), ["ps4"], ["ckvnT%d" % p])
                act(_R("copy", out=KT_swa3[:, :, cols], in_=psb(4)[:, 128:384].rearrange("p (g t) -> p g t", g=2)),
                    ["ps4"], ["KT_swa"])

            def kvM2(j, i, hTb, hname):
                p = i % 2
                mm(psum[5][:, 0:384], ckvnTb[p][:, :], wkv[:, 0:384], True, True, ["ckvnT%d" % p, "wkv"], ["ps5"])
                mm(psum[6][:, 0:384], ckvnTb[p][:, :], wkv[:, 384:768], True, True, ["ckvnT%d" % p, "wkv"], ["ps6"])
                pv4 = psum[6][:, 0:384].rearrange("p (q two d) -> p q two d", q=3, two=2)
                act(_R("copy", out=V_mla4[:, i, :, 0:64], in_=pv4[:, :, 0, :]), ["ps6"], ["V_mla"])
                act(_R("copy", out=V_mla4[:, i, :, 96:160], in_=pv4[:, :, 1, :]), ["ps6"], ["V_mla"])
                kt3 = ktokb[p].rearrange("p (h d) -> p h d", h=6)
                dve(_R("tensor_copy", out=kt3[:, :, 0:64], in_=psum[5][:, 0:384].rearrange("p (h d) -> p h d", h=6)),
                    ["ps5"], ["ktok%d" % p])
                pool(_R("tensor_copy", out=kt3[:, :, 64:96], in_=krb[p].unsqueeze(1).broadcast_to([128, 6, 32])),
                     ["kr%d" % p], ["ktok%d" % p])

            def kvM3(j, i, hTb, hname):
                p = i % 2
                cols = slice(i * 128, (i + 1) * 128)
                kt3 = ktokb[p].rearrange("p (h d) -> p h d", h=6)
                for h in range(6):
                    tr(psb(7)[0:96, h * 128:(h + 1) * 128], kt3[:, h, :], ident_b[:, :], ["ktok%d" % p, "ident_b"], ["ps7"])
                dve(_R("tensor_copy", out=KT_mla3[0:96, :, cols],
                       in_=psb(7)[0:96, 0:768].rearrange("p (h t) -> p h t", h=6)), ["ps7"], ["KT_mla"])

            def conv_chunk(t0, nt, hTb, hname):
                n = nt * 128
                g0 = t0 * 128
                for c2 in range(2):
                    for which, pbk in ((1, 5), (2, 6), (0, 7)):
                        cb = 416 + (which * 2 + c2) * 128
                        for k in range(8):
                            mm(psum[pbk][:, 0:n], wA3[:, k, cb:cb + 128], hTb[:, k, 0:n], k == 0, k == 7,
                               [hname, "wAc"], ["ps%d" % pbk])
                    act(_R("copy", out=tmpC[:, 0:n], in_=psum[5][:, 0:n]), ["ps5"], ["tmpC"])
                    dve(_R("tensor_tensor", out=zT3[:, c2, zcol(g0):zcol(g0) + n], in0=tmpC[:, 0:n], in1=psum[6][:, 0:n],
                           op=ALU.mult), ["tmpC", "ps6"], ["zT"])
                    act(_R("copy", out=BT3[:, c2, g0:g0 + n], in_=psum[7][:, 0:n]), ["ps7"], ["BT"])

            kv_stages = [kvM0, kvM1, kvM2, kvM3]
            tiles_a = []
            for ci, (t0, nt) in enumerate(chunks):
                for j in range(nt):
                    tiles_a.append((ci, t0, nt, j, t0 + j))
            for step in range(len(tiles_a) + len(kv_stages) - 1):
                if step < len(tiles_a):
                    ci, t0, nt, j, i = tiles_a[step]
                    if j == 0:
                        load_chunk(t0, nt)
                        norm_T(t0, nt, 0, hTdb[ci % 2], 0, 0, hname="hT%d" % (ci % 2))
                for sidx in reversed(range(len(kv_stages))):
                    t = step - sidx
                    if 0 <= t < len(tiles_a):
                        ci, t0, nt, j, i = tiles_a[t]
                        kv_stages[sidx](j, i, hTdb[ci % 2], "hT%d" % (ci % 2))
                        if sidx == 0 and j == nt - 1:
                            conv_chunk(t0, nt, hTdb[ci % 2], "hT%d" % (ci % 2))
            for c2 in range(2):
                cw = cwT[:, (l * 2 + c2) * 3:(l * 2 + c2) * 3 + 3]
                for g0, n in [(0, 256)] + [(256 + 512 * j, 512) for j in range(4)]:
                    zc = zcol(g0)
                    dve(_R("tensor_scalar",
                        out=tmpf[:, 0:n], in0=zT3[:, c2, zc - 1:zc - 1 + n], scalar1=cw[:, 0:1], scalar2=None,
                        op0=ALU.mult), ["zT", "cwT"], ["tmpf"])
                    dve(_R("scalar_tensor_tensor",
                        out=tmpf[:, 0:n], in0=zT3[:, c2, zc:zc + n], scalar=cw[:, 1:2], in1=tmpf[:, 0:n],
                        op0=ALU.mult, op1=ALU.add), ["zT", "cwT", "tmpf"], ["tmpf"])
                    dve(_R("scalar_tensor_tensor",
                        out=tmpf[:, 0:n], in0=zT3[:, c2, zc + 1:zc + 1 + n], scalar=cw[:, 2:3], in1=tmpf[:, 0:n],
                        op0=ALU.mult, op1=ALU.add), ["zT", "cwT", "tmpf"], ["tmpf"])
                    dve(_R("tensor_tensor",
                        out=convT3[:, c2, g0:g0 + n], in0=tmpf[:, 0:n], in1=BT3[:, c2, g0:g0 + n], op=ALU.mult),
                        ["tmpf", "BT"], ["convT"])
            P.barrier()
            LAYOUT["n2"] = P.nrec
            if STOP <= 2:
                continue

            ar.off = ph_off
            wB = ar.alloc(8 * 640, BF, "wB")
            wB3 = wB.rearrange("p (k c) -> p k c", k=8)
            wuq_f = ar.alloc(2 * 576, F32, "wuq_f")
            wuq = ar.alloc(2 * 576, BF, "wuq")
            wuq3 = wuq.rearrange("p (r c) -> p r c", r=2)
            wo = ar.alloc(8 * D, BF, "wo")
            wo3 = wo.rearrange("p (f d) -> p f d", f=8)
            Gb = ar.alloc(D, F32, "Gb")
            QT_mla = ar.alloc(6 * 512, BF, "QT_mla")
            QT_mla3 = QT_mla.rearrange("p (h t) -> p h t", h=6)
            QT_swa = ar.alloc(3 * 512, BF, "QT_swa")
            QT_swa3 = QT_swa.rearrange("p (h t) -> p h t", h=3)
            mixT = ar.alloc(6 * 512, BF, "mixT")
            mixT3 = mixT.rearrange("p (f t) -> p f t", f=6)
            PT = [ar.alloc(512, BF) for _ in range(6)]
            cqnb = [ar.alloc(256, BF) for _ in range(2)]
            cqnTb = [ar.alloc(256, BF) for _ in range(2)]
            qtokb = [ar.alloc(6 * 96, BF) for _ in range(2)]
            qswb = [ar.alloc(384, BF) for _ in range(2)]
            ru = ar.alloc(512, F32, "ru")
            rv = ar.alloc(512, F32, "rv")
            rs = ar.alloc(512, F32, "rs")
            bcs = ar.alloc(512, F32, "bcs")
            ssqb = [ar.alloc(2, F32) for _ in range(2)]

            dma("pool", wB3[:, :, 0:256], w_l[:, :, 0:256], [], ["wB"])
            dma("pool", wB3[:, :, 256:640], w_l[:, :, 416:800], [], ["wB"])
            dma("sp", wuq_f.rearrange("p (r c) -> p r c", r=2), w_uq[l].rearrange("(r p) c -> p r c", p=128),
                [], ["wuq_f"])
            for r_ in range(2):
                dve(_R("tensor_scalar", out=wuq3[:, r_, :], in0=wuq_f[:, r_ * 576:(r_ + 1) * 576],
                                                     scalar1=qgT[:, l * 2 + r_:l * 2 + r_ + 1], scalar2=None,
                                                     op0=ALU.mult), ["wuq_f", "qgT"], ["wuq"])
            dma("pool", wo3[:, :, :], w_o[l].rearrange("(f p) d -> p f d", p=128), [], ["wo"])

            SBK = [0, 1, 2, 3, 7]
            NPT = len(PT)
            att_state = {"pti": 0, "pending": None}

            def attention(nq, heads, qlhs, klhs, vwin, kblocks, scale, sink_l, mix_chunk0):
                LOOK = 4

                def finalize(h, po):
                    half = h % 2
                    r0 = 64 if half == 0 else 32
                    rows = slice(0, 64) if half == 0 else slice(64, 128)
                    if sink_l is not None:
                        dve(_R("tensor_scalar", out=rs[r0:r0 + 1, 0:nq], in0=psum[po][r0:r0 + 1, 0:nq],
                               scalar1=esink[r0:r0 + 1, sink_l * 6 + h:sink_l * 6 + h + 1], scalar2=None, op0=ALU.add),
                            ["ps%d" % po, "esink"], ["rs"])
                        dve(_R("reciprocal", out=rs[r0:r0 + 1, 0:nq], in_=rs[r0:r0 + 1, 0:nq]), ["rs"], ["rs"])
                    else:
                        dve(_R("reciprocal", out=rs[r0:r0 + 1, 0:nq], in_=psum[po][r0:r0 + 1, 0:nq]),
                            ["ps%d" % po], ["rs"])
                    mm(psum[6][:, 0:nq], ones_f[r0:r0 + 1, :], rs[r0:r0 + 1, 0:nq], True, True,
                       ["rs", "ones_f"], ["ps6"])
                    act(_R("copy", out=bcs[:, 0:nq], in_=psum[6][:, 0:nq]), ["ps6"], ["bcs"])
                    dve(_R("tensor_tensor", out=mixT3[rows, mix_chunk0 + h // 2, 0:nq], in0=psum[po][rows, 0:nq],
                           in1=bcs[rows, 0:nq], op=ALU.mult), ["ps%d" % po, "bcs"], ["mixT"])

                for h in heads:
                    po = 4 + (h % 2)
                    blocks = kblocks(h)
                    ptinfo = {}

                    def issue_S(bi, h=h, blocks=blocks, ptinfo=ptinfo):
                        kb, c0, c1, mks = blocks[bi]
                        psn = SBK[att_state["pti"] % len(SBK)]
                        mm(psum[psn][:, c0:c1], klhs(h, kb), qlhs(h, c0, c1), True, True,
                           ["KT_mla", "KT_swa", "QT"], ["ps%d" % psn])
                        pt = PT[att_state["pti"] % NPT]
                        ptn = "PT%d" % (att_state["pti"] % NPT)
                        att_state["pti"] += 1
                        ptinfo[bi] = (pt, ptn)
                        act(_R("activation", out=pt[:, c0:c1], in_=psum[psn][:, c0:c1], func=AF.Exp, scale=scale),
                            ["ps%d" % psn], [ptn])
                        for (m0, mk) in mks:
                            pool(_R("tensor_tensor", out=pt[:, m0:m0 + 128], in0=pt[:, m0:m0 + 128],
                                    in1=masks[:, mk * 128:(mk + 1) * 128], op=ALU.mult), [ptn, "masks"], [ptn])

                    for bi in range(min(LOOK, len(blocks))):
                        issue_S(bi)
                    for bi, (kb, c0, c1, mks) in enumerate(blocks):
                        if bi + LOOK < len(blocks):
                            issue_S(bi + LOOK)
                        if bi == 1 and att_state["pending"] is not None:
                            att_state["pending"]()
                            att_state["pending"] = None
                        pt, ptn = ptinfo[bi]
                        mm(psum[po][:, c0:c1], vwin(h, kb), pt[:, c0:c1], bi == 0, bi == len(blocks) - 1,
                           [ptn, "V_mla", "V_swa"], ["ps%d" % po])
                    if att_state["pending"] is not None:
                        att_state["pending"]()
                    att_state["pending"] = (lambda h=h, po=po: finalize(h, po))

            def attention_flush():
                if att_state["pending"] is not None:
                    att_state["pending"]()
                    att_state["pending"] = None

            first_lat = True
            for (t0, nt) in chunks:
                if t0 == 0 and l == 1:
                    continue
                seg = 0 if t0 == 0 else 1
                nq = nt * 128
                if t0 == 0 or first_lat:
                    gbuild(Gb, modcol(l, 2, NS if t0 == 0 else s), 7)
                    if t0 != 0:
                        first_lat = False
                load_chunk(t0, nt)
                norm_T(t0, nt, 0, hT3, 0, 0)
                def qM0(j, i):
                    p = i % 2
                    hsl = slice(j * 128, (j + 1) * 128)
                    pq, ps_ = (2, 3) if p == 0 else (0, 1)
                    for k in range(8):
                        mm(psum[pq][:, 0:256], hT3[:, k, hsl], wB3[:, k, 0:256], k == 0, k == 7, ["hT", "wB"], ["ps%d" % pq])
                    for k in range(8):
                        mm(psum[ps_][:, 0:384], hT3[:, k, hsl], wB3[:, k, 256:640], k == 0, k == 7, ["hT", "wB"], ["ps%d" % ps_])
                    dve(_R("memset", ssqb[p][:, :], 0.0), [], ["ssq%d" % p])
                    act(_R("activation", out=junk[:, 0:256], in_=psum[pq][:, 0:256], func=AF.Square,
                           scale=1.0 / 16.0, accum_out=ssqb[p][:, 0:1]), ["ps%d" % pq, "ssq%d" % p], ["junk", "ssq%d" % p])
                    rsq(ssqb[p][:, 0:1], ["ssq%d" % p], ["ssq%d" % p])
                    act(_R("activation", out=cqnb[p][:, :], in_=psum[pq][:, 0:256], func=AF.Copy, scale=ssqb[p][:, 0:1]),
                        ["ps%d" % pq, "ssq%d" % p], ["cqn%d" % p])
                    if i >= 2:
                        rope(qswb[p].rearrange("p (h d) -> p h d", h=6),
                             psum[ps_][:, 0:384].rearrange("p (h d) -> p h d", h=6), i - 2, "s", 6, ru, rv,
                             ["ps%d" % ps_, "ropes"], ["qsw%d" % p])
                    else:
                        act(_R("copy", out=qswb[p][:, :], in_=psum[ps_][:, 0:384]), ["ps%d" % ps_], ["qsw%d" % p])

                def qM1(j, i):
                    p = i % 2
                    hsl = slice(j * 128, (j + 1) * 128)
                    for r_ in range(2):
                        tr(psb(4)[:, r_ * 128:(r_ + 1) * 128], cqnb[p][:, r_ * 128:(r_ + 1) * 128], ident_b[:, :],
                           ["cqn%d" % p, "ident_b"], ["ps4"])
                    for pr in range(3):
                        tr(psb(4)[:, 256 + pr * 128:256 + (pr + 1) * 128], qswb[p][:, pr * 128:(pr + 1) * 128], ident_b[:, :],
                           ["qsw%d" % p, "ident_b"], ["ps4"])
                    act(_R("copy", out=cqnTb[p][:, :], in_=psb(4)[:, 0:256]), ["ps4"], ["cqnT%d" % p])
                    act(_R("copy", out=QT_swa3[:, :, hsl], in_=psb(4)[:, 256:640].rearrange("p (h t) -> p h t", h=3)),
                        ["ps4"], ["QT"])

                def qM2(j, i):
                    p = i % 2
                    for hh in range(2):
                        for r_ in range(2):
                            mm(psum[5 + hh][:, 0:288], cqnTb[p][:, r_ * 128:(r_ + 1) * 128],
                               wuq3[:, r_, hh * 288:(hh + 1) * 288], r_ == 0, r_ == 1, ["cqnT%d" % p, "wuq"], ["ps%d" % (5 + hh)])
                    qt3 = qtokb[p].rearrange("p (h d) -> p h d", h=6)
                    for hh in range(2):
                        pq3 = psum[5 + hh][:, 0:288].rearrange("p (h d) -> p h d", h=3)
                        if i >= 2:
                            act(_R("copy", out=qt3[:, hh * 3:(hh + 1) * 3, 0:64], in_=pq3[:, :, 0:64]),
                                ["ps%d" % (5 + hh)], ["qtok%d" % p])
                            rope(qt3[:, hh * 3:(hh + 1) * 3, 64:96], pq3[:, :, 64:96], i - 2, "m", 3, ru, rv,
                                 ["ps%d" % (5 + hh), "ropem"], ["qtok%d" % p])
                        else:
                            act(_R("copy", out=qt3[:, hh * 3:(hh + 1) * 3, :], in_=pq3), ["ps%d" % (5 + hh)], ["qtok%d" % p])

                def qM3(j, i):
                    p = i % 2
                    hsl = slice(j * 128, (j + 1) * 128)
                    qt3 = qtokb[p].rearrange("p (h d) -> p h d", h=6)
                    for h in range(6):
                        tr(psb(7)[0:96, h * 128:(h + 1) * 128], qt3[:, h, :], ident_b[:, :], ["qtok%d" % p, "ident_b"], ["ps7"])
                    dve(_R("tensor_copy", out=QT_mla3[0:96, :, hsl],
                           in_=psb(7)[0:96, 0:768].rearrange("p (h t) -> p h t", h=6)), ["ps7"], ["QT"])

                q_stages = [qM0, qM1, qM2, qM3]
                for step in range(nt + len(q_stages) - 1):
                    for sidx in reversed(range(len(q_stages))):
                        j = step - sidx
                        if 0 <= j < nt:
                            q_stages[sidx](j, t0 + j)

                if t0 == 0:
                    mla_blocks = lambda h: [(kb, 0, nq, []) for kb in range(2)]
                else:
                    mla_blocks = lambda h: [(kb, 0, nq, []) for kb in range(NT)]
                attention(nq, range(6),
                          lambda h, c0, c1: QT_mla3[0:96, h, c0:c1],
                          lambda h, kb: KT_mla3[0:96, h, kb * 128:(kb + 1) * 128],
                          lambda h, kb: V_mla4[:, kb, h // 2, (0 if h % 2 == 0 else 32):(128 if h % 2 == 0 else 160)],
                          mla_blocks, MLA_SCALE, None, 0)
                if t0 == 0:
                    swa_blocks = lambda h: [(kb, 0, nq, []) for kb in range(2)]
                else:
                    qb0 = t0 - 2

                    def swa_blocks(h, qb0=qb0):
                        bl = [(kb, 0, nq, []) for kb in range(2)]
                        for lb in range(max(0, qb0 - 1), min(15, qb0 + 4) + 1):
                            qlo = max(lb - 1, qb0)
                            qhi = min(lb + 1, qb0 + 3)
                            mks = []
                            for qb in range(qlo, qhi + 1):
                                if qb == lb + 1:
                                    mks.append(((qb - qb0) * 128, 0))
                                elif qb == lb - 1:
                                    mks.append(((qb - qb0) * 128, 1))
                            bl.append((2 + lb, (qlo - qb0) * 128, (qhi - qb0 + 1) * 128, mks))
                        return bl
                attention(nq, range(6),
                          lambda h, c0, c1: QT_swa3[(h % 2) * 64:(h % 2) * 64 + 64, h // 2, c0:c1],
                          lambda h, kb: KT_swa3[(h % 2) * 64:(h % 2) * 64 + 64, h // 3, kb * 128:(kb + 1) * 128],
                          lambda h, kb: V_swa4[:, kb, h // 3, (0 if h % 2 == 0 else 32):(128 if h % 2 == 0 else 160)],
                          swa_blocks, SWA_SCALE, l, 3)
                attention_flush()
                for j in range(nt):
                    i = t0 + j
                    for dh in range(2):
                        pbk = dh
                        for f in range(8):
                            lhs = mixT3[:, f, j * 128:(j + 1) * 128] if f < 6 else convT3[:, f - 6, i * 128:(i + 1) * 128]
                            mm(psum[pbk][:, :], lhs, wo3[:, f, dh * 512:(dh + 1) * 512], f == 0, f == 7,
                               ["mixT", "convT", "wo"], ["ps%d" % pbk])
                        dve(_R("tensor_tensor",
                            out=tmpf[:, dh * 512:(dh + 1) * 512], in0=psum[pbk][:, :], in1=Gb[:, dh * 512:(dh + 1) * 512],
                            op=ALU.mult), ["ps%d" % pbk, "gb"], ["tmpf"])
                        pool(_R("tensor_tensor",
                            out=xc3[:, j, dh * 512:(dh + 1) * 512], in0=xc3[:, j, dh * 512:(dh + 1) * 512],
                            in1=tmpf[:, dh * 512:(dh + 1) * 512], op=ALU.add), ["tmpf", "xc"], ["xc"])
                dma("sp", xmid_d[t0 * 128:(t0 + nt) * 128, :].rearrange("(n p) d -> p n d", p=128), xc3[:, 0:nt, :],
                    ["xc"], ["xmid"])
            P.barrier()
            if STOP <= 3:
                continue

            has_ctx = (l == 0)
            tlo = 0 if has_ctx else 2
            ar.off = 0
            AB2 = ar.alloc(4 * 2 * 8, F32)
            X = ar.alloc(NT * D, F32, "X")
            X3 = X.rearrange("p (n d) -> p n d", n=NT)
            G2 = [ar.alloc(D, F32), ar.alloc(D, F32)]
            wsel = ar.alloc(NT * NE, F32, "wsel")
            wsel3 = wsel.rearrange("p (t e) -> p t e", t=NT)
            maskf = ar.alloc(NT * NE, F32, "maskf")
            maskf3 = maskf.rearrange("p (t e) -> p t e", t=NT)
            rank = ar.alloc(16 * NE, F32, "rank")
            rank3 = rank.rearrange("p (t e) -> p t e", t=16)
            xn_all = ar.alloc(16 * D, BF, "xn_all")
            h2c = ar.alloc(8 * 256, BF, "h2c")
            h2c3 = h2c.rearrange("p (k t) -> p k t", k=8)
            c2_off = ar.off
            h2T = ar.alloc(8 * NTOK, BF, "h2T")
            h2T3 = h2T.rearrange("p (k t) -> p k t", k=8)
            maskb = ar.alloc(NT * NE, BF, "maskb")
            maskb3 = maskb.rearrange("p (t e) -> p t e", t=NT)
            xn = [ar.alloc(D, BF), ar.alloc(D, BF)]
            junk = ar.alloc(D, BF, "junk")
            tmpf = ar.alloc(D, F32, "tmpf")
            ss = ar.alloc(8, F32, "ss")
            gtmp = ar.alloc(512, F32, "gtmp")
            rwb = ar.alloc(8 * NE, BF, "rwb")
            rwb3 = rwb.rearrange("p (k e) -> p k e", k=8)
            aff = ar.alloc(NT * NE, F32, "aff")
            aff3 = aff.rearrange("p (t e) -> p t e", t=NT)
            cmpb = ar.alloc(NT * NE, BF, "cmpb")
            cmp3 = cmpb.rearrange("p (t e) -> p t e", t=NT)
            mx = ar.alloc(NT, F32, "mx")
            lo = ar.alloc(32, F32, "lo")
            c32 = ar.alloc(32, F32, "c32")
            xc3 = X3

            for (t0, nt) in chunks:
                if t0 == 0 and not has_ctx:
                    continue
                dma("sp", X3[:, t0:t0 + nt, :], xmid_d[t0 * 128:(t0 + nt) * 128, :].rearrange("(n p) d -> p n d", p=128),
                    ["xmid"], ["xld%d" % t0])
            dma("pool", rwb3[:, :, :], rw_d[l].rearrange("(k p) e -> p k e", p=128), [], ["rwb"])
            if has_ctx:
                gbuild(G2[0], modcol(l, 5, NS), 7)
            gbuild(G2[1], modcol(l, 5, s), 7)
            for (t0, nt) in chunks:
                if t0 == 0 and not has_ctx:
                    continue
                xc3 = X3[:, t0:t0 + nt, :]
                keep = None
                if t0 >= 2:
                    keep = (lambda j, t0=t0: (xn_all[:, (t0 - 2 + j) * D:(t0 - 1 + j) * D], "xnall"))
                norm_T(t0, nt, 1, h2T3, t0 * 128, 0, keep=keep, xname="xld%d" % t0)
            xc3 = X3
            first = True
            for i in range(tlo, NT):
                for k in range(8):
                    mm(psum[2][:, i * NE:(i + 1) * NE], h2T3[:, k, i * 128:(i + 1) * 128], rwb3[:, k, :],
                       first, (i == NT - 1 and k == 7), ["hT", "rwb"], ["ps2"])
                    first = False
            T0 = tlo
            nT = NT - tlo
            lg3 = psum[2][:, T0 * NE:NT * NE].rearrange("p (t e) -> p t e", e=NE)
            a3 = aff3[:, T0:NT, :]
            dve(_R("tensor_reduce", out=mx[:, T0:NT], in_=lg3, axis=AX.X, op=ALU.max), ["ps2"], ["mx"])
            dve(_R("tensor_tensor", out=a3, in0=lg3, in1=mx[:, T0:NT].unsqueeze(2).broadcast_to([128, nT, NE]),
                                          op=ALU.subtract), ["ps2", "mx"], ["aff"])
            act(_R("activation", out=a3, in_=a3, func=AF.Exp), ["aff"], ["aff"])
            dve(_R("tensor_reduce", out=mx[:, T0:NT], in_=a3, axis=AX.X, op=ALU.add), ["aff"], ["mx"])
            dve(_R("reciprocal", out=mx[:, T0:NT], in_=mx[:, T0:NT]), ["mx"], ["mx"])
            dve(_R("tensor_tensor", out=a3, in0=a3, in1=mx[:, T0:NT].unsqueeze(2).broadcast_to([128, nT, NE]),
                                          op=ALU.mult), ["aff", "mx"], ["aff"])
            dve(_R("memset", lo[:, :], 0.0), [], ["lo"])
            segs = ([(0, 2, 0)] if has_ctx else []) + [(2, 16, 16)]
            for it in range(NBIS):
                hstep = 2.0 ** -(it + 1)
                for (ta, tn, lc) in segs:
                    dve(_R("scalar_tensor_tensor",
                        out=cmp3[:, ta:ta + tn, :], in0=aff3[:, ta:ta + tn, :], scalar=-hstep,
                        in1=lo[:, lc:lc + 16].unsqueeze(1).broadcast_to([128, tn, NE]), op0=ALU.add, op1=ALU.is_ge),
                        ["aff", "lo"], ["cmp"])
                mm(psum[3][:, T0 * NE:NT * NE], ones_b[:, :], cmpb[:, T0 * NE:NT * NE], True, True, ["cmp", "ones_b"], ["ps3"])
                for (ta, tn, lc) in segs:
                    dve(_R("tensor_reduce",
                        out=c32[:, lc:lc + 16],
                        in_=psum[3][:, ta * NE:(ta + tn) * NE].rearrange("p (t e) -> p e t", e=NE),
                        axis=AX.X, op=ALU.add), ["ps3"], ["c32"])
                lsl = slice(0 if has_ctx else 16, 32)
                dve(_R("tensor_tensor", out=c32[:, lsl], in0=c32[:, lsl], in1=kvec[:, lsl], op=ALU.is_ge),
                    ["c32", "kvec"], ["c32"])
                dve(_R("scalar_tensor_tensor",
                    out=lo[:, lsl], in0=c32[:, lsl], scalar=hstep, in1=lo[:, lsl], op0=ALU.mult, op1=ALU.add),
                    ["c32", "lo"], ["lo"])
            for (ta, tn, lc) in segs:
                dve(_R("tensor_tensor", out=maskf3[:, ta:ta + tn, :], in0=aff3[:, ta:ta + tn, :],
                       in1=lo[:, lc:lc + 16].unsqueeze(1).broadcast_to([128, tn, NE]), op=ALU.is_ge), ["aff", "lo"], ["maskf"])
            dve(_R("tensor_tensor", out=wsel3[:, T0:NT, :], in0=maskf3[:, T0:NT, :], in1=a3, op=ALU.mult),
                ["maskf", "aff"], ["wsel"])
            dve(_R("tensor_copy", out=maskb3[:, 2:NT, :], in_=maskf3[:, 2:NT, :]), ["maskf"], ["maskb"])
            firstr = True
            for li in range(16):
                mm(psum[3][:, li * NE:(li + 1) * NE], masks[:, 256:384], maskb3[:, 2 + li, :], firstr, False,
                   ["maskb", "masks"], ["ps3"])
                firstr = False
                for l2 in range(li):
                    mm(psum[3][:, li * NE:(li + 1) * NE], ones_b[:, :], maskb3[:, 2 + l2, :], False,
                       (li == 15 and l2 == li - 1), ["maskb", "ones_b"], ["ps3"])
            dve(_R("tensor_copy", out=rank[:, :], in_=psum[3][:, 0:16 * NE]), ["ps3"], ["rank"])
            if has_ctx:
                pool(_R("tensor_copy", out=h2c3, in_=h2T3[:, :, 0:256]), ["hT"], ["h2c"])
            dump("aff", aff[:, :])
            dump("wsel", wsel[:, :])

            P.barrier()
            ar.off = c2_off
            wgb = ar.alloc(8 * 512, BF, "wgb")
            wub = ar.alloc(8 * 512, BF, "wub")
            wdf = ar.alloc(D, F32, "wdf")
            wdb = ar.alloc(4 * D, BF, "wdb")
            wdc = ar.alloc(4 * D, BF, "wdc")
            Sel = ar.alloc(16 * 256, BF, "Sel")
            Sel3 = Sel.rearrange("p (t s) -> p t s", t=16)
            SelWT = ar.alloc(2 * 2048, BF, "SelWT")
            SelWT3 = SelWT.rearrange("p (c t) -> p c t", c=2)
            selw = [ar.alloc(256, BF) for _ in range(2)]
            xgT = ar.alloc(8 * 256, BF, "xgT")
            xgT3 = xgT.rearrange("p (k t) -> p k t", k=8)
            ysb = ar.alloc(2 * D, BF, "ysb")
            ysb3 = ysb.rearrange("p (c d) -> p c d", c=2)
            hid = ar.alloc(4 * 256, BF, "hid")
            hid3 = hid.rearrange("p (f t) -> p f t", f=4)
            sgb = ar.alloc(256, F32, "sgb")
            mx = ar.alloc(NT, F32, "mx2")
            junk = SelWT[:, 0:D]
            wg3 = wgb.rearrange("p (k f) -> p k f", k=8)
            wu3 = wub.rearrange("p (k f) -> p k f", k=8)
            wd3 = wdb.rearrange("p (f d) -> p f d", f=4)
            wdc3 = wdc.rearrange("p (f d) -> p f d", f=4)
            A2l = ABv(2, 1)
            B2l = ABv(3, 1)

            def ffn_hidden(rhs_of_k, n, extra=None):
                for fc in range(4):
                    pg = 2 * (fc % 2)
                    for k in range(8):
                        mm(psum[pg][:, 0:n], wg3[:, k, fc * 128:(fc + 1) * 128], rhs_of_k(k), k == 0, k == 7,
                           ["xgT", "h2c", "wg"], ["ps%d" % pg])
                    for k in range(8):
                        mm(psum[pg + 1][:, 0:n], wu3[:, k, fc * 128:(fc + 1) * 128], rhs_of_k(k), k == 0, k == 7,
                           ["xgT", "h2c", "wu"], ["ps%d" % (pg + 1)])
                    act(_R("activation", out=sgb[:, 0:n], in_=psum[pg][:, 0:n], func=AF.Silu), ["ps%d" % pg], ["sg0"])
                    dve(_R("tensor_tensor", out=hid3[:, fc, 0:n], in0=sgb[:, 0:n], in1=psum[pg + 1][:, 0:n], op=ALU.mult),
                        ["sg0", "ps%d" % (pg + 1)], ["hid"])
                    if extra is not None:
                        extra(fc)

            srot = [0]

            def load_weights(ex):
                dma("pool", wg3, wg_d[l, ex].rearrange("(k p) f -> p k f", p=128), [], ["wg"])
                dma("pool", wu3, wu_d[l, ex].rearrange("(k p) f -> p k f", p=128), [], ["wu"])
                for fc in range(4):
                    dma("sp", wdf[:, :], wd_d[l, ex, fc * 128:(fc + 1) * 128, :], [], ["wdf"])
                    pool(_R("tensor_tensor", out=wdb[:, fc * D:(fc + 1) * D], in0=wdf[:, :], in1=G2[1][:, :], op=ALU.mult),
                         ["wdf", "gb"], ["wd"])
                    if has_ctx:
                        pool(_R("tensor_tensor", out=wdc[:, fc * D:(fc + 1) * D], in0=wdf[:, :], in1=G2[0][:, :],
                                op=ALU.mult), ["wdf", "gb"], ["wdc"])

            def build_sel(ex, lis):
                for li in lis:
                    dve(_R("tensor_scalar", out=Sel3[:, li, :], in0=iota_f[:, :], scalar1=rank3[:, li, ex:ex + 1],
                           scalar2=maskf3[:, 2 + li, ex:ex + 1], op0=ALU.is_equal, op1=ALU.mult),
                        ["iota_f", "rank", "maskf"], ["Sel"])

            def ctx_dense(ex):
                ffn_hidden(lambda k: h2c3[:, k, 0:256], 256)
                for j in range(2):
                    for dh in range(2):
                        py = 4 + ((j * 2 + dh) % 2)
                        for fc in range(4):
                            mm(psum[py][:, :], hid3[:, fc, j * 128:(j + 1) * 128], wdc3[:, fc, dh * 512:(dh + 1) * 512],
                               fc == 0, fc == 3, ["hid", "wdc"], ["ps%d" % py])
                        dve(_R("scalar_tensor_tensor", out=X3[:, j, dh * 512:(dh + 1) * 512], in0=psum[py][:, :],
                               scalar=wsel3[:, j, ex:ex + 1], in1=X3[:, j, dh * 512:(dh + 1) * 512],
                               op0=ALU.mult, op1=ALU.add), ["ps%d" % py, "wsel", "xc"], ["xc"])

            def gather_k(ex, k):
                pb = k % 2
                for li in range(16):
                    mm(psum[pb][:, 0:256], xn_all[:, li * D + k * 128:li * D + (k + 1) * 128], Sel3[:, li, :],
                       li == 0, li == 15, ["xnall", "Sel"], ["ps%d" % pb])
                act(_R("activation", out=xgT3[:, k, :], in_=psum[pb][:, 0:256], func=AF.Identity,
                       scale=A2l[:, k:k + 1], bias=B2l[:, k:k + 1]), ["ps%d" % pb, "AB"], ["xgT"])

            def down_proj(ex):
                for sc in range(2):
                    for dh in range(2):
                        py = 4 + ((sc * 2 + dh) % 2)
                        for fc in range(4):
                            mm(psum[py][:, :], hid3[:, fc, sc * 128:(sc + 1) * 128], wd3[:, fc, dh * 512:(dh + 1) * 512],
                               fc == 0, fc == 3, ["hid", "wd"], ["ps%d" % py])
                        act(_R("copy", out=ysb3[:, sc, dh * 512:(dh + 1) * 512], in_=psum[py][:, :]), ["ps%d" % py], ["ysb"])

            def selw_tr(ex, li):
                sw = selw[li % 2]
                swn = "selw%d" % (li % 2)
                dve(_R("tensor_scalar", out=sw[:, :], in0=iota_f[:, :], scalar1=rank3[:, li, ex:ex + 1],
                       scalar2=wsel3[:, 2 + li, ex:ex + 1], op0=ALU.is_equal, op1=ALU.mult),
                    ["iota_f", "rank", "wsel"], [swn])
                for sc in range(2):
                    tr(psb(6)[:, ((li % 4) * 2 + sc) * 128:((li % 4) * 2 + sc + 1) * 128], sw[:, sc * 128:(sc + 1) * 128],
                       ident_b[:, :], [swn, "ident_b"], ["ps6"])
                if li % 4 == 3:
                    for sc in range(2):
                        act(_R("copy", out=SelWT3[:, sc, (li - 3) * 128:(li + 1) * 128].rearrange("p (i t) -> p i t", i=4),
                               in_=psb(6)[:, :].rearrange("p (i c t) -> p i c t", i=4, c=2)[:, :, sc, :]),
                            ["ps6"], ["SelWT"])

            def scatter_tile(ex, li):
                for dh in range(2):
                    py = (7, 3)[srot[0] % 2]
                    srot[0] += 1
                    for sc in range(2):
                        mm(psum[py][:, :], SelWT3[:, sc, li * 128:(li + 1) * 128], ysb3[:, sc, dh * 512:(dh + 1) * 512],
                           sc == 0, sc == 1, ["SelWT", "ysb"], ["ps%d" % py])
                    dve(_R("tensor_tensor", out=X3[:, 2 + li, dh * 512:(dh + 1) * 512], in0=psum[py][:, :],
                           in1=X3[:, 2 + li, dh * 512:(dh + 1) * 512], op=ALU.add), ["ps%d" % py, "xc"], ["xc"])

            load_weights(0)
            build_sel(0, range(16))
            for ex in range(NE):
                for k in range(8):
                    gather_k(ex, k)
                    if ex > 0:
                        scatter_tile(ex - 1, 2 * k)
                        scatter_tile(ex - 1, 2 * k + 1)
                if has_ctx:
                    ctx_dense(ex)
                ffn_hidden(lambda k: xgT3[:, k, :], 256,
                           extra=(lambda fc, ex=ex: build_sel(ex + 1, range(4 * fc, 4 * fc + 4))) if ex + 1 < NE else None)
                down_proj(ex)
                if ex + 1 < NE:
                    load_weights(ex + 1)
                for li in range(16):
                    selw_tr(ex, li)
            for li in range(16):
                scatter_tile(NE - 1, li)
            if l == 0:
                dma("sp", xl1_d.rearrange("(n p) d -> p n d", p=128), X3[:, :, :], ["xc"], ["xsrc"])
            else:
                dve(_R("memset", mx[:, :], 0.0), [], ["mx"])
                for i in range(2, NT):
                    act(_R("activation", out=junk[:, :], in_=X3[:, i, :], func=AF.Square, scale=1.0 / 32.0,
                                                    accum_out=mx[:, i:i + 1]), ["xc", "mx"], ["junk", "mxa%d" % i])
                rsq(mx[:, 2:NT], ["mx"] + ["mxa%d" % i for i in range(2, NT)], ["mx"])
                for i in range(2, NT):
                    act(_R("activation", out=X3[:, i, :], in_=X3[:, i, :], func=AF.Copy, scale=mx[:, i:i + 1]),
                        ["xc", "mx"], ["xc"])
                    pool(_R("tensor_tensor", out=X3[:, i, :], in0=X3[:, i, :], in1=fgb[:, :], op=ALU.mult),
                         ["xc", "fgb"], ["xc"])
                dma("sp", out_d[s].rearrange("(n p) d -> p n d", p=128), X3[:, 2:NT, :], ["xc"], ["out"])
            P.barrier()

    P.barrier()
    P.emit(nc, stack)
    stack.close()
    return nc


def _consts():
    ident = np.eye(128, dtype=np.float32)
    k = np.arange(128)[:, None]
    q = np.arange(128)[None, :]
    masks = np.concatenate([(k >= q), (k <= q), (k < q)], axis=1).astype(np.float32)
    rows = SEQ // 64
    row = np.repeat(np.arange(rows, dtype=np.float32), 64)
    col = np.tile(np.arange(64, dtype=np.float32), rows)

    def table(rot):
        nf = rot // 4
        inv = (np.float32(10000.0) ** (-np.arange(nf, dtype=np.float32) / np.float32(nf))).astype(np.float32)
        ang = np.concatenate([row[:, None] * inv, col[:, None] * inv], axis=-1).astype(np.float32)
        cs, sn = np.cos(ang), np.sin(ang)
        t = np.concatenate([cs, cs, sn, sn], axis=-1).astype(np.float32)
        return np.ascontiguousarray(t.reshape(16, 128, 2 * rot).transpose(1, 0, 2).reshape(128, 32 * rot))

    iota = np.ascontiguousarray(np.broadcast_to(np.arange(256, dtype=np.float32)[None, :], (128, 256)))
    return ident, masks, table(32), table(64), iota


def kernel(**inputs):
    NS = 4
    ident, masks, ropem, ropes, iota = _consts()
    nc = build(NS=NS, NL=2)
    f = lambda a: np.ascontiguousarray(np.asarray(a, dtype=np.float32))
    shared = {k: f(inputs[k]) for k in ("ada_w", "ada_b", "norm1_g", "w_in", "mla_q_norm_g", "mla_w_uq",
                                        "mla_kv_norm_g", "mla_w_ukv", "swa_sink", "conv_w", "w_o", "norm2_g",
                                        "router_w", "exp_w_gate", "exp_w_up", "exp_w_down", "final_norm_g")}
    shared.update(k_ident=ident, k_masks=masks, k_ropem=ropem, k_ropes=ropes, k_iota=iota)
    x = f(inputs["x"])
    ctx = f(inputs["ctx"])
    c = f(inputs["c"])
    c_ctx = f(inputs["c_ctx"])
    in_maps = []
    for core in range(N_CORES):
        sl = slice(core * NS, (core + 1) * NS)
        m = dict(shared)
        m["x"] = np.ascontiguousarray(x[sl])
        m["ctx"] = np.ascontiguousarray(ctx[sl])
        m["cc"] = np.ascontiguousarray(np.concatenate([c[sl], c_ctx[None, :]], axis=0))
        in_maps.append(m)
    res = run_bass_kernel_spmd(nc, in_maps, core_ids=list(range(N_CORES)))
    return np.concatenate([r["out"] for r in res.results], axis=0)
```

```python
import math
from contextlib import ExitStack
import numpy as np
import concourse.bass as bass
import concourse.mybir as mybir
from concourse.bass_utils import run_bass_kernel_spmd

F32 = mybir.dt.float32
BF = mybir.dt.bfloat16
AF = mybir.ActivationFunctionType
ALU = mybir.AluOpType
AX = mybir.AxisListType

D = 1024
SEQ = 2048
CTX = 256
NT = 18
NTOK = NT * 128
EPS = 1e-6
MLA_SCALE = 96 ** -0.5
SWA_SCALE = 64 ** -0.5
NE = 16
NBIS = 22
N_CORES = 8

EP = 60000
EPD = 3900
NDMA = 8


_POS = {"memset": ("ap", "constant"), "matmul": ("out", "lhsT", "rhs"), "transpose": ("out", "in_", "identity")}


def _R(meth, *a, **k):
    for nm, v in zip(_POS.get(meth, ()), a):
        k[nm] = v
    return (meth, k)


class Prog:
    def __init__(self):
        self.streams = {e: [] for e in ("pe", "act", "dve", "pool", "sp")}
        self.count = {}
        self.lastw = {}
        self.readers = {}
        self.seen = {e: {} for e in self.streams}
        self.waited = {}
        self.rr = 0
        self.rrq = {}

    def _collect(self, eng, r, w, is_dma):
        deps = {}

        def add(t, same_ok):
            if t is None:
                return
            g, i = t
            if g == eng and not same_ok:
                return
            if deps.get(g, 0) < i:
                deps[g] = i

        for x in r:
            add(self.lastw.get(x), is_dma or eng != "pe")
        for x in w:
            add(self.lastw.get(x), is_dma)
            for g, i in self.readers.get(x, {}).items():
                add((g, i), is_dma)
        return deps

    def _waits(self, eng, deps):
        waits = []
        for g, i in deps.items():
            if self.seen[eng].get(g, 0) >= i:
                continue
            self.seen[eng][g] = i
            waits.append((g, i))
            self.waited.setdefault(g, set()).add(i)
        return waits

    def _mark(self, t, r, w):
        for x in r:
            d = self.readers.setdefault(x, {})
            if d.get(t[0], 0) < t[1]:
                d[t[0]] = t[1]
        for x in w:
            self.lastw[x] = t
            self.readers[x] = {}

    limit = None
    nrec = 0

    def _over(self):
        self.nrec += 1
        return self.limit is not None and self.nrec > self.limit

    def op(self, eng, fn, r=(), w=()):
        if self._over():
            return
        deps = self._collect(eng, r, w, False)
        waits = self._waits(eng, deps)
        n = self.count.get(eng, 0) + 1
        self.count[eng] = n
        self.streams[eng].append(("c", fn, waits, (eng, n)))
        self._mark((eng, n), r, w)

    def dma(self, q, fn, r=(), w=()):
        if self._over():
            return
        k = self.rrq.get(q, 0)
        self.rrq[q] = (k + 1) % NDMA
        g = "q%s%d" % (q, k)
        deps = self._collect(q, r, w, True)
        n = self.count.get(g, 0) + 1
        if n > 1:
            deps[g] = max(deps.get(g, 0), n - 1)
        waits = self._waits(q, deps)
        self.count[g] = n
        self.streams[q].append(("d", fn, waits, (g, n)))
        self._mark((g, n), r, w)

    def barrier(self):
        for e in self.streams:
            deps = {g: c for g, c in self.count.items() if g != e and c > 0}
            waits = self._waits(e, deps)
            if waits:
                self.streams[e].append(("w", None, waits, None))

    def emit(self, nc, stack):
        rank = {}
        nsem = {}
        for g, c in self.count.items():
            if g.startswith("q"):
                nsem[g] = (c + EPD - 1) // EPD
            else:
                ws = sorted(self.waited.get(g, ()))
                rank[g] = {i: k + 1 for k, i in enumerate(ws)}
                nsem[g] = max(1, (len(ws) + EP - 1) // EP)
        sems = {g: [stack.enter_context(nc.semaphore("s_%s_%d" % (g, k))) for k in range(n)]
                for g, n in nsem.items()}

        def semval(g, i):
            if g.startswith("q"):
                ep = (i - 1) // EPD
                return sems[g][ep], (i - ep * EPD) * 16
            v = rank[g][i]
            ep = (v - 1) // EP
            return sems[g][ep], v - ep * EP

        def replay(name, e):
            for kind, fn, waits, t in self.streams[name]:
                for g, i in waits:
                    s, v = semval(g, i)
                    e.wait_ge(s, v)
                if kind == "w":
                    continue
                ins = getattr(e, fn[0])(**fn[1])
                if kind == "d":
                    s, _ = semval(*t)
                    ins.then_inc(s, 16)
                elif t[1] in rank.get(t[0], {}):
                    s, _ = semval(*t)
                    ins.then_inc(s, 1)

        block = stack.enter_context(nc.Block())

        @block.tensor
        def _(e):
            replay("pe", e)

        @block.scalar
        def _(e):
            replay("act", e)

        @block.vector
        def _(e):
            replay("dve", e)

        @block.gpsimd
        def _(e):
            replay("pool", e)

        @block.sync
        def _(e):
            replay("sp", e)


LAYOUT = {}


class Arena:
    def __init__(self, t):
        self.t = t
        self.off = 0

    def alloc(self, nelem, dt, name=None):
        nb4 = nelem if dt == F32 else (nelem + 1) // 2
        if name is not None:
            LAYOUT[name] = (self.off, nelem, "f32" if dt == F32 else "bf16")
        v = self.t[:, self.off:self.off + nb4]
        self.off += nb4
        LAYOUT["_peak"] = max(LAYOUT.get("_peak", 0), self.off)
        assert self.off <= self.t.shape[1], ("arena overflow", self.off)
        if dt == F32:
            return v
        return v.bitcast(dt)[:, 0:nelem]


class _Stop(Exception):
    pass


NOROPE = False


def build(NS=4, NL=2, dumps=None, STOP=99, LIMIT=None):
    dumps = dumps or {}
    nc = bass.Bass("TRN2", target_bir_lowering=False)
    P = Prog()
    P.limit = LIMIT
    stack = ExitStack()

    def din(name, shape):
        return nc.dram_tensor(name, list(shape), F32, kind="ExternalInput").ap()

    x_d = din("x", (NS, SEQ, D))
    ctx_d = din("ctx", (NS, CTX, D))
    cc_d = din("cc", (NS + 1, D))
    ada_w = din("ada_w", (2, D, 6 * D))
    ada_b = din("ada_b", (2, 6 * D))
    n1g = din("norm1_g", (2, D))
    w_in = din("w_in", (2, D, 1824))
    qg_d = din("mla_q_norm_g", (2, 256))
    w_uq = din("mla_w_uq", (2, 256, 576))
    kvg_d = din("mla_kv_norm_g", (2, 128))
    w_ukv = din("mla_w_ukv", (2, 128, 768))
    sink_d = din("swa_sink", (2, 6))
    convw_d = din("conv_w", (2, 3, 256))
    w_o = din("w_o", (2, D, D))
    n2g = din("norm2_g", (2, D))
    rw_d = din("router_w", (2, D, NE))
    wg_d = din("exp_w_gate", (2, NE, D, 512))
    wu_d = din("exp_w_up", (2, NE, D, 512))
    wd_d = din("exp_w_down", (2, NE, 512, D))
    fg_d = din("final_norm_g", (D,))
    k_ident = din("k_ident", (128, 128))
    k_masks = din("k_masks", (128, 384))
    k_iota = din("k_iota", (128, 256))
    k_ropem = din("k_ropem", (128, 16 * 64))
    k_ropes = din("k_ropes", (128, 16 * 128))
    out_d = nc.dram_tensor("out", [NS, SEQ, D], F32, kind="ExternalOutput").ap()
    xmid_d = nc.dram_tensor("xmid", [NTOK, D], F32, kind="Internal").ap()
    xl1_d = nc.dram_tensor("xl1", [NTOK, D], F32, kind="Internal").ap()
    dump_d = {k: nc.dram_tensor("dbg_" + k, list(shp), F32, kind="ExternalOutput").ap()
              for k, shp in dumps.items()}

    def sb(name, shape, dt):
        return stack.enter_context(nc.sbuf_tensor(name, list(shape), dt))

    ident_f = sb("ident_f", (128, 128), F32)
    ident_b = sb("ident_b", (128, 128), BF)
    ones_f = sb("ones_f", (128, 128), F32)
    ones_b = sb("ones_b", (128, 128), BF)
    masks = sb("masks", (128, 384), BF)
    iota_f = sb("iota_f", (128, 256), F32)
    ropem = sb("ropem", (128, 1024), F32)
    ropes = sb("ropes", (128, 2048), F32)
    modT = sb("modT", (128, 2 * 48 * 5), F32)
    gT = sb("gT", (128, 2 * 2 * 8), F32)
    qgT = sb("qgT", (128, 4), F32)
    kvgT = sb("kvgT", (128, 2), F32)
    cwT = sb("cwT", (128, 2 * 2 * 3), F32)
    esink = sb("esink", (128, 12), F32)
    abT = sb("abT", (128, 2 * 48), F32)
    fgb = sb("fgb", (128, D), F32)
    kvec = sb("kvec", (128, 32), F32)
    AR = sb("arena", (128, 47300), F32)
    ar = Arena(AR)
    psum = [stack.enter_context(nc.psum_tensor("ps%d" % i, [128, 512], F32)) for i in range(8)]

    def psb(i):
        return psum[i][:, :].bitcast(BF)

    def act(fn, r, w):
        P.op("act", fn, r, w)

    def dve(fn, r, w):
        P.op("dve", fn, r, w)

    def pool(fn, r, w):
        P.op("pool", fn, r, w)

    def mm(out, lhsT, rhs, start, stop, r, w):
        P.op("pe", _R("matmul", out, lhsT, rhs, start=start, stop=stop, skip_group_check=True), r, w)

    def tr(out, in_, ident, r, w):
        P.op("pe", _R("transpose", out, in_, ident), r, w)

    def dma(q, out, in_, r, w, slow=False):
        if slow:
            P.dma(q, _R("dma_start", out=out, in_=in_, allow_slow_non_contiguous=True), r, w)
        else:
            P.dma(q, _R("dma_start", out=out, in_=in_), r, w)

    def rsq(ap, r, w):
        dve(_R("tensor_scalar", out=ap, in0=ap, scalar1=EPS, scalar2=None, op0=ALU.add), r, w)
        act(_R("activation", out=ap, in_=ap, func=AF.Ln), w, w)
        act(_R("activation", out=ap, in_=ap, func=AF.Exp, scale=-0.5), w, w)

    def dump(name, ap, idx=None):
        if name in dump_d:
            d = dump_d[name] if idx is None else dump_d[name][idx]
            P.barrier()
            dma("sp", d, ap, [], ["dump_" + name])


    dma("sp", ident_f[:, :], k_ident, [], ["ident_f"])
    dma("pool", ident_b[:, :], k_ident, [], ["ident_b"])
    dma("pool", masks[:, :], k_masks, [], ["masks"])
    dma("sp", iota_f[:, :], k_iota, [], ["iota_f"])
    dma("sp", ropem[:, :], k_ropem, [], ["ropem"])
    dma("sp", ropes[:, :], k_ropes, [], ["ropes"])
    dve(_R("memset", ones_f[:, :], 1.0), [], ["ones_f"])
    dve(_R("memset", ones_b[:, :], 1.0), [], ["ones_b"])
    dve(_R("memset", kvec[:, 0:16], 32.0), [], ["kvec"])
    dve(_R("memset", kvec[:, 16:32], 256.0), [], ["kvec"])
    ctxmgr = nc.allow_non_contiguous_dma(reason="tiny parameter vectors")
    ctxmgr.__enter__()
    for l in range(2):
        dma("sp", gT[:, (l * 2) * 8:(l * 2 + 1) * 8], n1g[l].rearrange("(k p) -> p k", p=128), [], ["gT"], slow=True)
        dma("sp", gT[:, (l * 2 + 1) * 8:(l * 2 + 2) * 8], n2g[l].rearrange("(k p) -> p k", p=128), [], ["gT"], slow=True)
        dma("sp", qgT[:, l * 2:l * 2 + 2], qg_d[l].rearrange("(k p) -> p k", p=128), [], ["qgT"], slow=True)
        dma("sp", kvgT[:, l:l + 1], kvg_d[l].rearrange("(k p) -> p k", p=128), [], ["kvgT"], slow=True)
        for c2 in range(2):
            dma("sp", cwT[:, (l * 2 + c2) * 3:(l * 2 + c2) * 3 + 3],
                convw_d[l][:, c2 * 128:(c2 + 1) * 128].rearrange("k p -> p k"), [], ["cwT"], slow=True)
        dma("sp", abT[:, l * 48:(l + 1) * 48], ada_b[l].rearrange("(j p) -> p j", p=128), [], ["abT"], slow=True)
        dma("sp", esink[:, l * 6:(l + 1) * 6], sink_d[l].partition_broadcast(128), [], ["esink"])
    ctxmgr.__exit__(None, None, None)
    dma("sp", fgb[:, :], fg_d.partition_broadcast(128), [], ["fgb"])
    act(_R("activation", out=esink[:, :], in_=esink[:, :], func=AF.Exp), ["esink"], ["esink"])

    ar.off = 0
    cc_sb = ar.alloc(D, F32, "cc_sb")
    cs_bf = ar.alloc(D, BF, "cs_bf")
    sT = ar.alloc(64, BF, "sT")
    awb = [ar.alloc(6 * D, BF), ar.alloc(6 * D, BF)]
    NB = NS + 1
    if STOP >= 1:
        dma("sp", cc_sb[0:NB, :], cc_d, [], ["cc_sb"])
        act(_R("activation", out=cs_bf[0:NB, :], in_=cc_sb[0:NB, :], func=AF.Silu), ["cc_sb"], ["cs_bf"])
        for k in range(8):
            tr(psb(0)[:, k * 8:k * 8 + NB], cs_bf[0:NB, k * 128:(k + 1) * 128], ident_b[0:NB, 0:NB],
               ["cs_bf", "ident_b"], ["ps0"])
        dve(_R("tensor_copy", out=sT[:, :].rearrange("p (k b) -> p k b", b=8)[:, :, 0:NB],
               in_=psb(0)[:, 0:64].rearrange("p (k b) -> p k b", b=8)[:, :, 0:NB]), ["ps0"], ["sT"])
        for l in range(NL):
            for k in range(8):
                b = awb[k % 2]
                dma("pool", b[:, :], ada_w[l, k * 128:(k + 1) * 128, :], [], ["awb%d" % (k % 2)])
                for j in range(48):
                    mm(psum[1][:, j * NB:(j + 1) * NB], b[:, j * 128:(j + 1) * 128], sT[:, k * 8:k * 8 + NB],
                       (k == 0 and j == 0), (k == 7 and j == 47), ["awb%d" % (k % 2), "sT"], ["ps1"])
            mview = modT[:, l * 240:l * 240 + 48 * NB].rearrange("p (j b) -> p j b", b=NB) if NB == 5 else \
                modT[:, l * 240:l * 240 + 48 * NB].rearrange("p (j b) -> p j b", b=NB)
            dve(_R("tensor_tensor",
                out=mview, in0=psum[1][:, 0:48 * NB].rearrange("p (j b) -> p j b", b=NB),
                in1=abT[:, l * 48:(l + 1) * 48].unsqueeze(2).broadcast_to([128, 48, NB]), op=ALU.add),
                ["ps1", "abT"], ["modT"])

    def modcol(l, m, b):
        return modT[:, l * 240:l * 240 + 48 * NB].rearrange("p (j b) -> p j b", b=NB)[:, m * 8:(m + 1) * 8, b]

    P.barrier()

    chunks = [(0, 2)] + [(2 + 4 * j, 4) for j in range(4)]
    LAYOUT["n1"] = P.nrec
    if STOP <= 1:
        NS = 0

    for s in range(NS):
        for l in range(NL):
            src_lat = x_d[s] if l == 0 else xl1_d[256:NTOK, :]
            src_ctx = ctx_d[s] if l == 0 else xl1_d[0:256, :]

            def src_tiles(t0, nt):
                if t0 == 0:
                    return src_ctx.rearrange("(n p) d -> p n d", p=128)
                return src_lat[(t0 - 2) * 128:(t0 - 2 + nt) * 128, :].rearrange("(n p) d -> p n d", p=128)

            ar.off = 0
            AB = ar.alloc(4 * 2 * 8, F32)
            KT_mla = ar.alloc(6 * NTOK, BF, "KT_mla")
            V_mla = ar.alloc(NT * 3 * 160, BF, "V_mla")
            KT_swa = ar.alloc(2 * NTOK, BF, "KT_swa")
            V_swa = ar.alloc(NT * 2 * 160, BF, "V_swa")
            convT = ar.alloc(2 * NTOK, BF, "convT")
            xc = ar.alloc(4 * D, F32, "xc")
            hT = ar.alloc(8 * 512, BF, "hT")
            xn = [ar.alloc(D, BF), ar.alloc(D, BF)]
            junk = ar.alloc(D, BF, "junk")
            tmpf = ar.alloc(D, F32, "tmpf")
            ss = ar.alloc(8, F32, "ss")
            gtmp = ar.alloc(512, F32, "gtmp")
            rx = ar.alloc(512, F32, "rx")
            ph_off = ar.off
            KT_mla3 = KT_mla.rearrange("p (h t) -> p h t", h=6)
            KT_swa3 = KT_swa.rearrange("p (g t) -> p g t", g=2)
            V_mla4 = V_mla.rearrange("p (i q c) -> p i q c", i=NT, q=3)
            V_swa4 = V_swa.rearrange("p (i g c) -> p i g c", i=NT, g=2)
            convT3 = convT.rearrange("p (c t) -> p c t", c=2)
            hT3 = hT.rearrange("p (k t) -> p k t", k=8)
            xc3 = xc.rearrange("p (n d) -> p n d", n=4)

            def ABv(which, seg):
                o = (which * 2 + seg) * 8
                return AB[:, o:o + 8]

            for seg in range(2):
                b = NS if seg == 0 else s
                for nrm in range(2):
                    dve(_R("scalar_tensor_tensor",
                        out=ABv(2 * nrm, seg), in0=modcol(l, 3 * nrm + 1, b), scalar=1.0,
                        in1=gT[:, (l * 2 + nrm) * 8:(l * 2 + nrm + 1) * 8], op0=ALU.add, op1=ALU.mult),
                        ["modT", "gT"], ["AB"])
                    dve(_R("tensor_copy",
                        out=ABv(2 * nrm + 1, seg), in_=modcol(l, 3 * nrm, b)), ["modT"], ["AB"])


            def gbuild(dst, col8, pbank):
                for k in range(8):
                    dve(_R("tensor_scalar", out=gtmp[:, (k % 4) * 128:(k % 4 + 1) * 128], in0=ident_f[:, :],
                                                       scalar1=col8[:, k:k + 1], scalar2=None, op0=ALU.mult),
                        ["ident_f", "modT"], ["gtmp%d" % (k % 4)])
                    mm(psum[pbank][:, (k % 4) * 128:(k % 4 + 1) * 128], ones_f[:, :],
                       gtmp[:, (k % 4) * 128:(k % 4 + 1) * 128], k % 4 == 0, k % 4 == 3,
                       ["ones_f", "gtmp%d" % (k % 4)], ["ps%d" % pbank])
                    if k % 4 == 3:
                        act(_R("copy", out=dst[:, (k - 3) * 128:(k + 1) * 128], in_=psum[pbank][:, :]),
                            ["ps%d" % pbank], ["gb"])

            def load_chunk(t0, nt):
                dma("sp", xc3[:, 0:nt, :], src_tiles(t0, nt), ["xsrc"], ["xc"])

            def norm_T(t0, nt, which, dst3, dcol0, pb0, hname="hT", keep=None, xname="xc"):
                seg = 0 if t0 == 0 else 1
                A = ABv(2 * which, seg)
                B = ABv(2 * which + 1, seg)
                dve(_R("memset", ss[:, :], 0.0), [], ["ss"])
                for j in range(nt):
                    act(_R("activation", out=junk[:, :], in_=xc3[:, j, :], func=AF.Square, scale=1.0 / 32.0,
                                                    accum_out=ss[:, j:j + 1]), [xname, "ss"], ["junk", "ss%d" % j])
                rsq(ss[:, 0:nt], ["ss"] + ["ss%d" % j for j in range(nt)], ["ss"])
                for j in range(nt):
                    xb = xn[j % 2]
                    xbn = "xn%d" % (j % 2)
                    if keep is not None:
                        xb, xbn = keep(j)
                    pb = pb0 + (j % 2)
                    act(_R("activation", out=xb[:, :], in_=xc3[:, j, :], func=AF.Copy,
                                                           scale=ss[:, j:j + 1]), [xname, "ss"], [xbn])
                    for k in range(8):
                        tr(psb(pb)[:, k * 128:(k + 1) * 128], xb[:, k * 128:(k + 1) * 128], ident_b[:, :],
                           [xbn, "ident_b"], ["ps%d" % pb])
                    dve(_R("tensor_tensor",
                        out=tmpf.rearrange("p (k t) -> p k t", k=8),
                        in0=psb(pb).rearrange("p (k t) -> p k t", k=8),
                        in1=A.unsqueeze(2).broadcast_to([128, 8, 128]), op=ALU.mult),
                        ["ps%d" % pb, "AB"], ["tmpf"])
                    dve(_R("tensor_tensor",
                        out=dst3[:, :, dcol0 + j * 128:dcol0 + (j + 1) * 128],
                        in0=tmpf.rearrange("p (k t) -> p k t", k=8),
                        in1=B.unsqueeze(2).broadcast_to([128, 8, 128]), op=ALU.add),
                        ["tmpf", "AB"], [hname])

            def rope(out3, in3, ti, kind, H, u, v, rr, ww):
                if NOROPE:
                    act(_R("copy", out=out3, in_=in3), rr, ww)
                    return
                rot = 32 if kind == "m" else 64
                half = rot // 2
                tab = ropem if kind == "m" else ropes
                cosd = tab[:, ti * 2 * rot:ti * 2 * rot + rot].unsqueeze(1).broadcast_to([128, H, rot])
                sind = tab[:, ti * 2 * rot + rot:(ti + 1) * 2 * rot].unsqueeze(1).broadcast_to([128, H, rot])
                n = H * rot
                u3 = u[:, 0:n].rearrange("p (h d) -> p h d", h=H)
                v3 = v[:, 0:n].rearrange("p (h d) -> p h d", h=H)
                x3 = rx[:, 0:n].rearrange("p (h d) -> p h d", h=H)
                act(_R("copy", out=x3, in_=in3), rr, ["rope_x"])
                pool(_R("tensor_tensor", out=u3, in0=x3, in1=cosd, op=ALU.mult), ["rope_x"] + rr, ["rope_u"])
                pool(_R("tensor_tensor", out=v3, in0=x3, in1=sind, op=ALU.mult), ["rope_x"] + rr, ["rope_v"])
                pool(_R("tensor_tensor", out=out3[:, :, 0:half], in0=u3[:, :, 0:half], in1=v3[:, :, half:rot],
                        op=ALU.subtract), ["rope_u", "rope_v"], ww)
                pool(_R("tensor_tensor", out=out3[:, :, half:rot], in0=u3[:, :, half:rot], in1=v3[:, :, 0:half],
                        op=ALU.add), ["rope_u", "rope_v"], ww)

            ar.off = ph_off
            wA = ar.alloc(8 * 1184, BF, "wA")
            wA3 = wA.rearrange("p (k c) -> p k c", k=8)
            wkv_f = ar.alloc(768, F32, "wkv_f")
            wkv = ar.alloc(768, BF, "wkv")
            zT = ar.alloc(2 * 2308, BF, "zT")
            zT3 = zT.rearrange("p (c t) -> p c t", c=2)
            BT = ar.alloc(2 * NTOK, BF, "BT")
            BT3 = BT.rearrange("p (c t) -> p c t", c=2)
            ckvnb = [ar.alloc(128, BF) for _ in range(2)]
            ckvnTb = [ar.alloc(128, BF) for _ in range(2)]
            krb = [ar.alloc(32, BF) for _ in range(2)]
            ktokb = [ar.alloc(6 * 96, BF) for _ in range(2)]
            kswb = [ar.alloc(128, BF) for _ in range(2)]
            kdupb = [ar.alloc(256, BF) for _ in range(2)]
            sskb = [ar.alloc(2, F32) for _ in range(2)]
            hT_b = ar.alloc(8 * 512, BF, "hT_b")
            hTdb = [hT3, hT_b.rearrange("p (k t) -> p k t", k=8)]
            ru = ar.alloc(512, F32, "ru")
            rv = ar.alloc(512, F32, "rv")
            tmpC = ar.alloc(512, F32, "tmpC")
            ssk = ar.alloc(2, F32, "ssk")

            w_l = w_in[l].rearrange("(k p) c -> p k c", p=128)
            dma("pool", wA3[:, :, 0:160], w_l[:, :, 256:416], [], ["wA"])
            dma("pool", wA3[:, :, 160:416], w_l[:, :, 800:1056], [], ["wA"])
            dma("pool", wA3[:, :, 416:1184], w_l[:, :, 1056:1824], [], ["wAc"])
            with nc.allow_non_contiguous_dma(reason="head-split weight layout"):
                for two in range(2):
                    dma("sp", wkv_f[:, two * 384:(two + 1) * 384].rearrange("p (h d) -> p h d", h=6),
                        w_ukv[l].rearrange("r (h two d) -> r two h d", h=6, two=2)[:, two], [], ["wkv_f"], slow=True)
            dve(_R("tensor_scalar", out=wkv[:, :], in0=wkv_f[:, :], scalar1=kvgT[:, l:l + 1], scalar2=None,
                                          op0=ALU.mult), ["wkv_f", "kvgT"], ["wkv"])
            for (za, zb) in ((0, 1), (257, 259), (2307, 2308)):
                dve(_R("memset", zT3[:, :, za:zb], 0.0), [], ["zT"])
            dve(_R("memset", V_mla4[:, :, :, 64:96], 0.0), [], ["V_mla"])
            dve(_R("memset", V_swa4[:, :, :, 64:96], 0.0), [], ["V_swa"])
            dve(_R("memset", V_mla4[:, :, :, 64:65], 1.0), [], ["V_mla"])
            dve(_R("memset", V_swa4[:, :, :, 64:65], 1.0), [], ["V_swa"])

            def zcol(t):
                return 1 + t if t < 256 else 3 + t

            def kvM0(j, i, hTb, hname):
                p = i % 2
                hsl = slice(j * 128, (j + 1) * 128)
                pa = 2 + p
                pan = "ps%d" % pa
                for k in range(8):
                    mm(psum[pa][:, 0:416], hTb[:, k, hsl], wA3[:, k, 0:416], k == 0, k == 7, [hname, "wA"], [pan])
                dve(_R("memset", sskb[p][:, :], 0.0), [], ["ssk%d" % p])
                act(_R("activation", out=junk[:, 0:128], in_=psum[pa][:, 0:128], func=AF.Square,
                       scale=128 ** -0.5, accum_out=sskb[p][:, 0:1]), [pan, "ssk%d" % p], ["junk", "ssk%d" % p])
                rsq(sskb[p][:, 0:1], ["ssk%d" % p], ["ssk%d" % p])
                act(_R("activation", out=ckvnb[p][:, :], in_=psum[pa][:, 0:128], func=AF.Copy,
                       scale=sskb[p][:, 0:1]), [pan, "ssk%d" % p], ["ckvn%d" % p])
                if i >= 2:
                    ti = i - 2
                    rope(krb[p].rearrange("p (h d) -> p h d", h=1),
                         psum[pa][:, 128:160].rearrange("p (h d) -> p h d", h=1), ti, "m", 1, ru, rv,
                         [pan, "ropem"], ["kr%d" % p])
                    rope(kswb[p].rearrange("p (h d) -> p h d", h=2),
                         psum[pa][:, 160:288].rearrange("p (h d) -> p h d", h=2), ti, "s", 2, ru, rv,
                         [pan, "ropes"], ["ksw%d" % p])
                else:
                    act(_R("copy", out=krb[p][:, :], in_=psum[pa][:, 128:160]), [pan], ["kr%d" % p])
                    act(_R("copy", out=kswb[p][:, :], in_=psum[pa][:, 160:288]), [pan], ["ksw%d" % p])
                pool(_R("tensor_copy", out=kdupb[p].rearrange("p (g c d) -> p g c d", g=2, c=2),
                        in_=kswb[p].rearrange("p (g d) -> p g d", g=2).unsqueeze(2).broadcast_to([128, 2, 2, 64])),
                     ["ksw%d" % p], ["kdup%d" % p])
                for c0 in (0, 96):
                    act(_R("copy", out=V_swa4[:, i, :, c0:c0 + 64],
                           in_=psum[pa][:, 288:416].rearrange("p (g d) -> p g d", g=2)), [pan], ["V_swa"])

            def kvM1(j, i, hTb, hname):
                p = i % 2
                cols = slice(i * 128, (i + 1) * 128)
                tr(psb(4)[:, 0:128], ckvnb[p][:, :], ident_b[:, :], ["ckvn%d" % p, "ident_b"], ["ps4"])
                for g in range(2):
                    tr(psb(4)[:, 128 + g * 128:256 + g * 128], kdupb[p][:, g * 128:(g + 1) * 128], ident_b[:, :],
                       ["kdup%d" % p, "ident_b"], ["ps4"])
                act(_R("copy", out=ckvnTb[p][:, :], in_=psb(4)[:, 0:128]), ["ps4"], ["ckvnT%d" % p])
                act(_R("copy", out=KT_swa3[:, :, cols], in_=psb(4)[:, 128:384].rearrange("p (g t) -> p g t", g=2)),
                    ["ps4"], ["KT_swa"])

            def kvM2(j, i, hTb, hname):
                p = i % 2
                mm(psum[5][:, 0:384], ckvnTb[p][:, :], wkv[:, 0:384], True, True, ["ckvnT%d" % p, "wkv"], ["ps5"])
                mm(psum[6][:, 0:384], ckvnTb[p][:, :], wkv[:, 384:768], True, True, ["ckvnT%d" % p, "wkv"], ["ps6"])
                pv4 = psum[6][:, 0:384].rearrange("p (q two d) -> p q two d", q=3, two=2)
                act(_R("copy", out=V_mla4[:, i, :, 0:64], in_=pv4[:, :, 0, :]), ["ps6"], ["V_mla"])
                act(_R("copy", out=V_mla4[:, i, :, 96:160], in_=pv4[:, :, 1, :]), ["ps6"], ["V_mla"])
                kt3 = ktokb[p].rearrange("p (h d) -> p h d", h=6)
                dve(_R("tensor_copy", out=kt3[:, :, 0:64], in_=psum[5][:, 0:384].rearrange("p (h d) -> p h d", h=6)),
                    ["ps5"], ["ktok%d" % p])
                pool(_R("tensor_copy", out=kt3[:, :, 64:96], in_=krb[p].unsqueeze(1).broadcast_to([128, 6, 32])),
                     ["kr%d" % p], ["ktok%d" % p])

            def kvM3(j, i, hTb, hname):
                p = i % 2
                cols = slice(i * 128, (i + 1) * 128)
                kt3 = ktokb[p].rearrange("p (h d) -> p h d", h=6)
                for h in range(6):
                    tr(psb(7)[0:96, h * 128:(h + 1) * 128], kt3[:, h, :], ident_b[:, :], ["ktok%d" % p, "ident_b"], ["ps7"])
                dve(_R("tensor_copy", out=KT_mla3[0:96, :, cols],
                       in_=psb(7)[0:96, 0:768].rearrange("p (h t) -> p h t", h=6)), ["ps7"], ["KT_mla"])

            def conv_chunk(t0, nt, hTb, hname):
                n = nt * 128
                g0 = t0 * 128
                for c2 in range(2):
                    for which, pbk in ((1, 5), (2, 6), (0, 7)):
                        cb = 416 + (which * 2 + c2) * 128
                        for k in range(8):
                            mm(psum[pbk][:, 0:n], wA3[:, k, cb:cb + 128], hTb[:, k, 0:n], k == 0, k == 7,
                               [hname, "wAc"], ["ps%d" % pbk])
                    act(_R("copy", out=tmpC[:, 0:n], in_=psum[5][:, 0:n]), ["ps5"], ["tmpC"])
                    dve(_R("tensor_tensor", out=zT3[:, c2, zcol(g0):zcol(g0) + n], in0=tmpC[:, 0:n], in1=psum[6][:, 0:n],
                           op=ALU.mult), ["tmpC", "ps6"], ["zT"])
                    act(_R("copy", out=BT3[:, c2, g0:g0 + n], in_=psum[7][:, 0:n]), ["ps7"], ["BT"])

            kv_stages = [kvM0, kvM1, kvM2, kvM3]
            tiles_a = []
            for ci, (t0, nt) in enumerate(chunks):
                for j in range(nt):
                    tiles_a.append((ci, t0, nt, j, t0 + j))
            for step in range(len(tiles_a) + len(kv_stages) - 1):
                if step < len(tiles_a):
                    ci, t0, nt, j, i = tiles_a[step]
                    if j == 0:
                        load_chunk(t0, nt)
                        norm_T(t0, nt, 0, hTdb[ci % 2], 0, 0, hname="hT%d" % (ci % 2))
                for sidx in reversed(range(len(kv_stages))):
                    t = step - sidx
                    if 0 <= t < len(tiles_a):
                        ci, t0, nt, j, i = tiles_a[t]
                        kv_stages[sidx](j, i, hTdb[ci % 2], "hT%d" % (ci % 2))
                        if sidx == 0 and j == nt - 1:
                            conv_chunk(t0, nt, hTdb[ci % 2], "hT%d" % (ci % 2))
            for c2 in range(2):
                cw = cwT[:, (l * 2 + c2) * 3:(l * 2 + c2) * 3 + 3]
                for g0, n in [(0, 256)] + [(256 + 512 * j, 512) for j in range(4)]:
                    zc = zcol(g0)
                    dve(_R("tensor_scalar",
                        out=tmpf[:, 0:n], in0=zT3[:, c2, zc - 1:zc - 1 + n], scalar1=cw[:, 0:1], scalar2=None,
                        op0=ALU.mult), ["zT", "cwT"], ["tmpf"])
                    dve(_R("scalar_tensor_tensor",
                        out=tmpf[:, 0:n], in0=zT3[:, c2, zc:zc + n], scalar=cw[:, 1:2], in1=tmpf[:, 0:n],
                        op0=ALU.mult, op1=ALU.add), ["zT", "cwT", "tmpf"], ["tmpf"])
                    dve(_R("scalar_tensor_tensor",
                        out=tmpf[:, 0:n], in0=zT3[:, c2, zc + 1:zc + 1 + n], scalar=cw[:, 2:3], in1=tmpf[:, 0:n],
                        op0=ALU.mult, op1=ALU.add), ["zT", "cwT", "tmpf"], ["tmpf"])
                    dve(_R("tensor_tensor",
                        out=convT3[:, c2, g0:g0 + n], in0=tmpf[:, 0:n], in1=BT3[:, c2, g0:g0 + n], op=ALU.mult),
                        ["tmpf", "BT"], ["convT"])
            P.barrier()
            LAYOUT["n2"] = P.nrec
            if STOP <= 2:
                continue

            ar.off = ph_off
            wB = ar.alloc(8 * 640, BF, "wB")
            wB3 = wB.rearrange("p (k c) -> p k c", k=8)
            wuq_f = ar.alloc(2 * 576, F32, "wuq_f")
            wuq = ar.alloc(2 * 576, BF, "wuq")
            wuq3 = wuq.rearrange("p (r c) -> p r c", r=2)
            wo = ar.alloc(8 * D, BF, "wo")
            wo3 = wo.rearrange("p (f d) -> p f d", f=8)
            Gb = ar.alloc(D, F32, "Gb")
            QT_mla = ar.alloc(6 * 512, BF, "QT_mla")
            QT_mla3 = QT_mla.rearrange("p (h t) -> p h t", h=6)
            QT_swa = ar.alloc(3 * 512, BF, "QT_swa")
            QT_swa3 = QT_swa.rearrange("p (h t) -> p h t", h=3)
            mixT = ar.alloc(6 * 512, BF, "mixT")
            mixT3 = mixT.rearrange("p (f t) -> p f t", f=6)
            PT = [ar.alloc(512, BF) for _ in range(6)]
            cqnb = [ar.alloc(256, BF) for _ in range(2)]
            cqnTb = [ar.alloc(256, BF) for _ in range(2)]
            qtokb = [ar.alloc(6 * 96, BF) for _ in range(2)]
            qswb = [ar.alloc(384, BF) for _ in range(2)]
            ru = ar.alloc(512, F32, "ru")
            rv = ar.alloc(512, F32, "rv")
            rs = ar.alloc(512, F32, "rs")
            bcs = ar.alloc(512, F32, "bcs")
            ssqb = [ar.alloc(2, F32) for _ in range(2)]

            dma("pool", wB3[:, :, 0:256], w_l[:, :, 0:256], [], ["wB"])
            dma("pool", wB3[:, :, 256:640], w_l[:, :, 416:800], [], ["wB"])
            dma("sp", wuq_f.rearrange("p (r c) -> p r c", r=2), w_uq[l].rearrange("(r p) c -> p r c", p=128),
                [], ["wuq_f"])
            for r_ in range(2):
                dve(_R("tensor_scalar", out=wuq3[:, r_, :], in0=wuq_f[:, r_ * 576:(r_ + 1) * 576],
                                                     scalar1=qgT[:, l * 2 + r_:l * 2 + r_ + 1], scalar2=None,
                                                     op0=ALU.mult), ["wuq_f", "qgT"], ["wuq"])
            dma("pool", wo3[:, :, :], w_o[l].rearrange("(f p) d -> p f d", p=128), [], ["wo"])

            SBK = [0, 1, 2, 3, 7]
            NPT = len(PT)
            att_state = {"pti": 0, "pending": None}

            def attention(nq, heads, qlhs, klhs, vwin, kblocks, scale, sink_l, mix_chunk0):
                LOOK = 4

                def finalize(h, po):
                    half = h % 2
                    r0 = 64 if half == 0 else 32
                    rows = slice(0, 64) if half == 0 else slice(64, 128)
                    if sink_l is not None:
                        dve(_R("tensor_scalar", out=rs[r0:r0 + 1, 0:nq], in0=psum[po][r0:r0 + 1, 0:nq],
                               scalar1=esink[r0:r0 + 1, sink_l * 6 + h:sink_l * 6 + h + 1], scalar2=None, op0=ALU.add),
                            ["ps%d" % po, "esink"], ["rs"])
                        dve(_R("reciprocal", out=rs[r0:r0 + 1, 0:nq], in_=rs[r0:r0 + 1, 0:nq]), ["rs"], ["rs"])
                    else:
                        dve(_R("reciprocal", out=rs[r0:r0 + 1, 0:nq], in_=psum[po][r0:r0 + 1, 0:nq]),
                            ["ps%d" % po], ["rs"])
                    mm(psum[6][:, 0:nq], ones_f[r0:r0 + 1, :], rs[r0:r0 + 1, 0:nq], True, True,
                       ["rs", "ones_f"], ["ps6"])
                    act(_R("copy", out=bcs[:, 0:nq], in_=psum[6][:, 0:nq]), ["ps6"], ["bcs"])
                    dve(_R("tensor_tensor", out=mixT3[rows, mix_chunk0 + h // 2, 0:nq], in0=psum[po][rows, 0:nq],
                           in1=bcs[rows, 0:nq], op=ALU.mult), ["ps%d" % po, "bcs"], ["mixT"])

                for h in heads:
                    po = 4 + (h % 2)
                    blocks = kblocks(h)
                    ptinfo = {}

                    def issue_S(bi, h=h, blocks=blocks, ptinfo=ptinfo):
                        kb, c0, c1, mks = blocks[bi]
                        psn = SBK[att_state["pti"] % len(SBK)]
                        mm(psum[psn][:, c0:c1], klhs(h, kb), qlhs(h, c0, c1), True, True,
                           ["KT_mla", "KT_swa", "QT"], ["ps%d" % psn])
                        pt = PT[att_state["pti"] % NPT]
                        ptn = "PT%d" % (att_state["pti"] % NPT)
                        att_state["pti"] += 1
                        ptinfo[bi] = (pt, ptn)
                        act(_R("activation", out=pt[:, c0:c1], in_=psum[psn][:, c0:c1], func=AF.Exp, scale=scale),
                            ["ps%d" % psn], [ptn])
                        for (m0, mk) in mks:
                            pool(_R("tensor_tensor", out=pt[:, m0:m0 + 128], in0=pt[:, m0:m0 + 128],
                                    in1=masks[:, mk * 128:(mk + 1) * 128], op=ALU.mult), [ptn, "masks"], [ptn])

                    for bi in range(min(LOOK, len(blocks))):
                        issue_S(bi)
                    for bi, (kb, c0, c1, mks) in enumerate(blocks):
                        if bi + LOOK < len(blocks):
                            issue_S(bi + LOOK)
                        if bi == 1 and att_state["pending"] is not None:
                            att_state["pending"]()
                            att_state["pending"] = None
                        pt, ptn = ptinfo[bi]
                        mm(psum[po][:, c0:c1], vwin(h, kb), pt[:, c0:c1], bi == 0, bi == len(blocks) - 1,
                           [ptn, "V_mla", "V_swa"], ["ps%d" % po])
                    if att_state["pending"] is not None:
                        att_state["pending"]()
                    att_state["pending"] = (lambda h=h, po=po: finalize(h, po))

            def attention_flush():
                if att_state["pending"] is not None:
                    att_state["pending"]()
                    att_state["pending"] = None

            first_lat = True
            for (t0, nt) in chunks:
                if t0 == 0 and l == 1:
                    continue
                seg = 0 if t0 == 0 else 1
                nq = nt * 128
                if t0 == 0 or first_lat:
                    gbuild(Gb, modcol(l, 2, NS if t0 == 0 else s), 7)
                    if t0 != 0:
                        first_lat = False
                load_chunk(t0, nt)
                norm_T(t0, nt, 0, hT3, 0, 0)
                def qM0(j, i):
                    p = i % 2
                    hsl = slice(j * 128, (j + 1) * 128)
                    pq, ps_ = (2, 3) if p == 0 else (0, 1)
                    for k in range(8):
                        mm(psum[pq][:, 0:256], hT3[:, k, hsl], wB3[:, k, 0:256], k == 0, k == 7, ["hT", "wB"], ["ps%d" % pq])
                    for k in range(8):
                        mm(psum[ps_][:, 0:384], hT3[:, k, hsl], wB3[:, k, 256:640], k == 0, k == 7, ["hT", "wB"], ["ps%d" % ps_])
                    dve(_R("memset", ssqb[p][:, :], 0.0), [], ["ssq%d" % p])
                    act(_R("activation", out=junk[:, 0:256], in_=psum[pq][:, 0:256], func=AF.Square,
                           scale=1.0 / 16.0, accum_out=ssqb[p][:, 0:1]), ["ps%d" % pq, "ssq%d" % p], ["junk", "ssq%d" % p])
                    rsq(ssqb[p][:, 0:1], ["ssq%d" % p], ["ssq%d" % p])
                    act(_R("activation", out=cqnb[p][:, :], in_=psum[pq][:, 0:256], func=AF.Copy, scale=ssqb[p][:, 0:1]),
                        ["ps%d" % pq, "ssq%d" % p], ["cqn%d" % p])
                    if i >= 2:
                        rope(qswb[p].rearrange("p (h d) -> p h d", h=6),
                             psum[ps_][:, 0:384].rearrange("p (h d) -> p h d", h=6), i - 2, "s", 6, ru, rv,
                             ["ps%d" % ps_, "ropes"], ["qsw%d" % p])
                    else:
                        act(_R("copy", out=qswb[p][:, :], in_=psum[ps_][:, 0:384]), ["ps%d" % ps_], ["qsw%d" % p])

                def qM1(j, i):
                    p = i % 2
                    hsl = slice(j * 128, (j + 1) * 128)
                    for r_ in range(2):
                        tr(psb(4)[:, r_ * 128:(r_ + 1) * 128], cqnb[p][:, r_ * 128:(r_ + 1) * 128], ident_b[:, :],
                           ["cqn%d" % p, "ident_b"], ["ps4"])
                    for pr in range(3):
                        tr(psb(4)[:, 256 + pr * 128:256 + (pr + 1) * 128], qswb[p][:, pr * 128:(pr + 1) * 128], ident_b[:, :],
                           ["qsw%d" % p, "ident_b"], ["ps4"])
                    act(_R("copy", out=cqnTb[p][:, :], in_=psb(4)[:, 0:256]), ["ps4"], ["cqnT%d" % p])
                    act(_R("copy", out=QT_swa3[:, :, hsl], in_=psb(4)[:, 256:640].rearrange("p (h t) -> p h t", h=3)),
                        ["ps4"], ["QT"])

                def qM2(j, i):
                    p = i % 2
                    for hh in range(2):
                        for r_ in range(2):
                            mm(psum[5 + hh][:, 0:288], cqnTb[p][:, r_ * 128:(r_ + 1) * 128],
                               wuq3[:, r_, hh * 288:(hh + 1) * 288], r_ == 0, r_ == 1, ["cqnT%d" % p, "wuq"], ["ps%d" % (5 + hh)])
                    qt3 = qtokb[p].rearrange("p (h d) -> p h d", h=6)
                    for hh in range(2):
                        pq3 = psum[5 + hh][:, 0:288].rearrange("p (h d) -> p h d", h=3)
                        if i >= 2:
                            act(_R("copy", out=qt3[:, hh * 3:(hh + 1) * 3, 0:64], in_=pq3[:, :, 0:64]),
                                ["ps%d" % (5 + hh)], ["qtok%d" % p])
                            rope(qt3[:, hh * 3:(hh + 1) * 3, 64:96], pq3[:, :, 64:96], i - 2, "m", 3, ru, rv,
                                 ["ps%d" % (5 + hh), "ropem"], ["qtok%d" % p])
                        else:
                            act(_R("copy", out=qt3[:, hh * 3:(hh + 1) * 3, :], in_=pq3), ["ps%d" % (5 + hh)], ["qtok%d" % p])

                def qM3(j, i):
                    p = i % 2
                    hsl = slice(j * 128, (j + 1) * 128)
                    qt3 = qtokb[p].rearrange("p (h d) -> p h d", h=6)
                    for h in range(6):
                        tr(psb(7)[0:96, h * 128:(h + 1) * 128], qt3[:, h, :], ident_b[:, :], ["qtok%d" % p, "ident_b"], ["ps7"])
                    dve(_R("tensor_copy", out=QT_mla3[0:96, :, hsl],
                           in_=psb(7)[0:96, 0:768].rearrange("p (h t) -> p h t", h=6)), ["ps7"], ["QT"])

                q_stages = [qM0, qM1, qM2, qM3]
                for step in range(nt + len(q_stages) - 1):
                    for sidx in reversed(range(len(q_stages))):
                        j = step - sidx
                        if 0 <= j < nt:
                            q_stages[sidx](j, t0 + j)

                if t0 == 0:
                    mla_blocks = lambda h: [(kb, 0, nq, []) for kb in range(2)]
                else:
                    mla_blocks = lambda h: [(kb, 0, nq, []) for kb in range(NT)]
                attention(nq, range(6),
                          lambda h, c0, c1: QT_mla3[0:96, h, c0:c1],
                          lambda h, kb: KT_mla3[0:96, h, kb * 128:(kb + 1) * 128],
                          lambda h, kb: V_mla4[:, kb, h // 2, (0 if h % 2 == 0 else 32):(128 if h % 2 == 0 else 160)],
                          mla_blocks, MLA_SCALE, None, 0)
                if t0 == 0:
                    swa_blocks = lambda h: [(kb, 0, nq, []) for kb in range(2)]
                else:
                    qb0 = t0 - 2

                    def swa_blocks(h, qb0=qb0):
                        bl = [(kb, 0, nq, []) for kb in range(2)]
                        for lb in range(max(0, qb0 - 1), min(15, qb0 + 4) + 1):
                            qlo = max(lb - 1, qb0)
                            qhi = min(lb + 1, qb0 + 3)
                            mks = []
                            for qb in range(qlo, qhi + 1):
                                if qb == lb + 1:
                                    mks.append(((qb - qb0) * 128, 0))
                                elif qb == lb - 1:
                                    mks.append(((qb - qb0) * 128, 1))
                            bl.append((2 + lb, (qlo - qb0) * 128, (qhi - qb0 + 1) * 128, mks))
                        return bl
                attention(nq, range(6),
                          lambda h, c0, c1: QT_swa3[(h % 2) * 64:(h % 2) * 64 + 64, h // 2, c0:c1],
                          lambda h, kb: KT_swa3[(h % 2) * 64:(h % 2) * 64 + 64, h // 3, kb * 128:(kb + 1) * 128],
                          lambda h, kb: V_swa4[:, kb, h // 3, (0 if h % 2 == 0 else 32):(128 if h % 2 == 0 else 160)],
                          swa_blocks, SWA_SCALE, l, 3)
                attention_flush()
                for j in range(nt):
                    i = t0 + j
                    for dh in range(2):
                        pbk = dh
                        for f in range(8):
                            lhs = mixT3[:, f, j * 128:(j + 1) * 128] if f < 6 else convT3[:, f - 6, i * 128:(i + 1) * 128]
                            mm(psum[pbk][:, :], lhs, wo3[:, f, dh * 512:(dh + 1) * 512], f == 0, f == 7,
                               ["mixT", "convT", "wo"], ["ps%d" % pbk])
                        dve(_R("tensor_tensor",
                            out=tmpf[:, dh * 512:(dh + 1) * 512], in0=psum[pbk][:, :], in1=Gb[:, dh * 512:(dh + 1) * 512],
                            op=ALU.mult), ["ps%d" % pbk, "gb"], ["tmpf"])
                        pool(_R("tensor_tensor",
                            out=xc3[:, j, dh * 512:(dh + 1) * 512], in0=xc3[:, j, dh * 512:(dh + 1) * 512],
                            in1=tmpf[:, dh * 512:(dh + 1) * 512], op=ALU.add), ["tmpf", "xc"], ["xc"])
                dma("sp", xmid_d[t0 * 128:(t0 + nt) * 128, :].rearrange("(n p) d -> p n d", p=128), xc3[:, 0:nt, :],
                    ["xc"], ["xmid"])
            P.barrier()
            if STOP <= 3:
                continue

            has_ctx = (l == 0)
            tlo = 0 if has_ctx else 2
            ar.off = 0
            AB2 = ar.alloc(4 * 2 * 8, F32)
            X = ar.alloc(NT * D, F32, "X")
            X3 = X.rearrange("p (n d) -> p n d", n=NT)
            G2 = [ar.alloc(D, F32), ar.alloc(D, F32)]
            wsel = ar.alloc(NT * NE, F32, "wsel")
            wsel3 = wsel.rearrange("p (t e) -> p t e", t=NT)
            maskf = ar.alloc(NT * NE, F32, "maskf")
            maskf3 = maskf.rearrange("p (t e) -> p t e", t=NT)
            rank = ar.alloc(16 * NE, F32, "rank")
            rank3 = rank.rearrange("p (t e) -> p t e", t=16)
            xn_all = ar.alloc(16 * D, BF, "xn_all")
            h2c = ar.alloc(8 * 256, BF, "h2c")
            h2c3 = h2c.rearrange("p (k t) -> p k t", k=8)
            c2_off = ar.off
            h2T = ar.alloc(8 * NTOK, BF, "h2T")
            h2T3 = h2T.rearrange("p (k t) -> p k t", k=8)
            maskb = ar.alloc(NT * NE, BF, "maskb")
            maskb3 = maskb.rearrange("p (t e) -> p t e", t=NT)
            xn = [ar.alloc(D, BF), ar.alloc(D, BF)]
            junk = ar.alloc(D, BF, "junk")
            tmpf = ar.alloc(D, F32, "tmpf")
            ss = ar.alloc(8, F32, "ss")
            gtmp = ar.alloc(512, F32, "gtmp")
            rwb = ar.alloc(8 * NE, BF, "rwb")
            rwb3 = rwb.rearrange("p (k e) -> p k e", k=8)
            aff = ar.alloc(NT * NE, F32, "aff")
            aff3 = aff.rearrange("p (t e) -> p t e", t=NT)
            cmpb = ar.alloc(NT * NE, BF, "cmpb")
            cmp3 = cmpb.rearrange("p (t e) -> p t e", t=NT)
            mx = ar.alloc(NT, F32, "mx")
            lo = ar.alloc(32, F32, "lo")
            c32 = ar.alloc(32, F32, "c32")
            xc3 = X3

            for (t0, nt) in chunks:
                if t0 == 0 and not has_ctx:
                    continue
                dma("sp", X3[:, t0:t0 + nt, :], xmid_d[t0 * 128:(t0 + nt) * 128, :].rearrange("(n p) d -> p n d", p=128),
                    ["xmid"], ["xld%d" % t0])
            dma("pool", rwb3[:, :, :], rw_d[l].rearrange("(k p) e -> p k e", p=128), [], ["rwb"])
            if has_ctx:
                gbuild(G2[0], modcol(l, 5, NS), 7)
            gbuild(G2[1], modcol(l, 5, s), 7)
            for (t0, nt) in chunks:
                if t0 == 0 and not has_ctx:
                    continue
                xc3 = X3[:, t0:t0 + nt, :]
                keep = None
                if t0 >= 2:
                    keep = (lambda j, t0=t0: (xn_all[:, (t0 - 2 + j) * D:(t0 - 1 + j) * D], "xnall"))
                norm_T(t0, nt, 1, h2T3, t0 * 128, 0, keep=keep, xname="xld%d" % t0)
            xc3 = X3
            first = True
            for i in range(tlo, NT):
                for k in range(8):
                    mm(psum[2][:, i * NE:(i + 1) * NE], h2T3[:, k, i * 128:(i + 1) * 128], rwb3[:, k, :],
                       first, (i == NT - 1 and k == 7), ["hT", "rwb"], ["ps2"])
                    first = False
            T0 = tlo
            nT = NT - tlo
            lg3 = psum[2][:, T0 * NE:NT * NE].rearrange("p (t e) -> p t e", e=NE)
            a3 = aff3[:, T0:NT, :]
            dve(_R("tensor_reduce", out=mx[:, T0:NT], in_=lg3, axis=AX.X, op=ALU.max), ["ps2"], ["mx"])
            dve(_R("tensor_tensor", out=a3, in0=lg3, in1=mx[:, T0:NT].unsqueeze(2).broadcast_to([128, nT, NE]),
                                          op=ALU.subtract), ["ps2", "mx"], ["aff"])
            act(_R("activation", out=a3, in_=a3, func=AF.Exp), ["aff"], ["aff"])
            dve(_R("tensor_reduce", out=mx[:, T0:NT], in_=a3, axis=AX.X, op=ALU.add), ["aff"], ["mx"])
            dve(_R("reciprocal", out=mx[:, T0:NT], in_=mx[:, T0:NT]), ["mx"], ["mx"])
            dve(_R("tensor_tensor", out=a3, in0=a3, in1=mx[:, T0:NT].unsqueeze(2).broadcast_to([128, nT, NE]),
                                          op=ALU.mult), ["aff", "mx"], ["aff"])
            dve(_R("memset", lo[:, :], 0.0), [], ["lo"])
            segs = ([(0, 2, 0)] if has_ctx else []) + [(2, 16, 16)]
            for it in range(NBIS):
                hstep = 2.0 ** -(it + 1)
                for (ta, tn, lc) in segs:
                    dve(_R("scalar_tensor_tensor",
                        out=cmp3[:, ta:ta + tn, :], in0=aff3[:, ta:ta + tn, :], scalar=-hstep,
                        in1=lo[:, lc:lc + 16].unsqueeze(1).broadcast_to([128, tn, NE]), op0=ALU.add, op1=ALU.is_ge),
                        ["aff", "lo"], ["cmp"])
                mm(psum[3][:, T0 * NE:NT * NE], ones_b[:, :], cmpb[:, T0 * NE:NT * NE], True, True, ["cmp", "ones_b"], ["ps3"])
                for (ta, tn, lc) in segs:
                    dve(_R("tensor_reduce",
                        out=c32[:, lc:lc + 16],
                        in_=psum[3][:, ta * NE:(ta + tn) * NE].rearrange("p (t e) -> p e t", e=NE),
                        axis=AX.X, op=ALU.add), ["ps3"], ["c32"])
                lsl = slice(0 if has_ctx else 16, 32)
                dve(_R("tensor_tensor", out=c32[:, lsl], in0=c32[:, lsl], in1=kvec[:, lsl], op=ALU.is_ge),
                    ["c32", "kvec"], ["c32"])
                dve(_R("scalar_tensor_tensor",
                    out=lo[:, lsl], in0=c32[:, lsl], scalar=hstep, in1=lo[:, lsl], op0=ALU.mult, op1=ALU.add),
                    ["c32", "lo"], ["lo"])
            for (ta, tn, lc) in segs:
                dve(_R("tensor_tensor", out=maskf3[:, ta:ta + tn, :], in0=aff3[:, ta:ta + tn, :],
                       in1=lo[:, lc:lc + 16].unsqueeze(1).broadcast_to([128, tn, NE]), op=ALU.is_ge), ["aff", "lo"], ["maskf"])
            dve(_R("tensor_tensor", out=wsel3[:, T0:NT, :], in0=maskf3[:, T0:NT, :], in1=a3, op=ALU.mult),
                ["maskf", "aff"], ["wsel"])
            dve(_R("tensor_copy", out=maskb3[:, 2:NT, :], in_=maskf3[:, 2:NT, :]), ["maskf"], ["maskb"])
            firstr = True
            for li in range(16):
                mm(psum[3][:, li * NE:(li + 1) * NE], masks[:, 256:384], maskb3[:, 2 + li, :], firstr, False,
                   ["maskb", "masks"], ["ps3"])
                firstr = False
                for l2 in range(li):
                    mm(psum[3][:, li * NE:(li + 1) * NE], ones_b[:, :], maskb3[:, 2 + l2, :], False,
                       (li == 15 and l2 == li - 1), ["maskb", "ones_b"], ["ps3"])
            dve(_R("tensor_copy", out=rank[:, :], in_=psum[3][:, 0:16 * NE]), ["ps3"], ["rank"])
            if has_ctx:
                pool(_R("tensor_copy", out=h2c3, in_=h2T3[:, :, 0:256]), ["hT"], ["h2c"])
            dump("aff", aff[:, :])
            dump("wsel", wsel[:, :])

            P.barrier()
            ar.off = c2_off
            wgb = ar.alloc(8 * 512, BF, "wgb")
            wub = ar.alloc(8 * 512, BF, "wub")
            wdf = ar.alloc(D, F32, "wdf")
            wdb = ar.alloc(4 * D, BF, "wdb")
            wdc = ar.alloc(4 * D, BF, "wdc")
            Sel = ar.alloc(16 * 256, BF, "Sel")
            Sel3 = Sel.rearrange("p (t s) -> p t s", t=16)
            SelWT = ar.alloc(2 * 2048, BF, "SelWT")
            SelWT3 = SelWT.rearrange("p (c t) -> p c t", c=2)
            selw = [ar.alloc(256, BF) for _ in range(2)]
            xgT = ar.alloc(8 * 256, BF, "xgT")
            xgT3 = xgT.rearrange("p (k t) -> p k t", k=8)
            ysb = ar.alloc(2 * D, BF, "ysb")
            ysb3 = ysb.rearrange("p (c d) -> p c d", c=2)
            hid = ar.alloc(4 * 256, BF, "hid")
            hid3 = hid.rearrange("p (f t) -> p f t", f=4)
            sgb = ar.alloc(256, F32, "sgb")
            mx = ar.alloc(NT, F32, "mx2")
            junk = SelWT[:, 0:D]
            wg3 = wgb.rearrange("p (k f) -> p k f", k=8)
            wu3 = wub.rearrange("p (k f) -> p k f", k=8)
            wd3 = wdb.rearrange("p (f d) -> p f d", f=4)
            wdc3 = wdc.rearrange("p (f d) -> p f d", f=4)
            A2l = ABv(2, 1)
            B2l = ABv(3, 1)

            def ffn_hidden(rhs_of_k, n, extra=None):
                for fc in range(4):
                    pg = 2 * (fc % 2)
                    for k in range(8):
                        mm(psum[pg][:, 0:n], wg3[:, k, fc * 128:(fc + 1) * 128], rhs_of_k(k), k == 0, k == 7,
                           ["xgT", "h2c", "wg"], ["ps%d" % pg])
                    for k in range(8):
                        mm(psum[pg + 1][:, 0:n], wu3[:, k, fc * 128:(fc + 1) * 128], rhs_of_k(k), k == 0, k == 7,
                           ["xgT", "h2c", "wu"], ["ps%d" % (pg + 1)])
                    act(_R("activation", out=sgb[:, 0:n], in_=psum[pg][:, 0:n], func=AF.Silu), ["ps%d" % pg], ["sg0"])
                    dve(_R("tensor_tensor", out=hid3[:, fc, 0:n], in0=sgb[:, 0:n], in1=psum[pg + 1][:, 0:n], op=ALU.mult),
                        ["sg0", "ps%d" % (pg + 1)], ["hid"])
                    if extra is not None:
                        extra(fc)

            srot = [0]

            def load_weights(ex):
                dma("pool", wg3, wg_d[l, ex].rearrange("(k p) f -> p k f", p=128), [], ["wg"])
                dma("pool", wu3, wu_d[l, ex].rearrange("(k p) f -> p k f", p=128), [], ["wu"])
                for fc in range(4):
                    dma("sp", wdf[:, :], wd_d[l, ex, fc * 128:(fc + 1) * 128, :], [], ["wdf"])
                    pool(_R("tensor_tensor", out=wdb[:, fc * D:(fc + 1) * D], in0=wdf[:, :], in1=G2[1][:, :], op=ALU.mult),
                         ["wdf", "gb"], ["wd"])
                    if has_ctx:
                        pool(_R("tensor_tensor", out=wdc[:, fc * D:(fc + 1) * D], in0=wdf[:, :], in1=G2[0][:, :],
                                op=ALU.mult), ["wdf", "gb"], ["wdc"])

            def build_sel(ex, lis):
                for li in lis:
                    dve(_R("tensor_scalar", out=Sel3[:, li, :], in0=iota_f[:, :], scalar1=rank3[:, li, ex:ex + 1],
                           scalar2=maskf3[:, 2 + li, ex:ex + 1], op0=ALU.is_equal, op1=ALU.mult),
                        ["iota_f", "rank", "maskf"], ["Sel"])

            def ctx_dense(ex):
                ffn_hidden(lambda k: h2c3[:, k, 0:256], 256)
                for j in range(2):
                    for dh in range(2):
                        py = 4 + ((j * 2 + dh) % 2)
                        for fc in range(4):
                            mm(psum[py][:, :], hid3[:, fc, j * 128:(j + 1) * 128], wdc3[:, fc, dh * 512:(dh + 1) * 512],
                               fc == 0, fc == 3, ["hid", "wdc"], ["ps%d" % py])
                        dve(_R("scalar_tensor_tensor", out=X3[:, j, dh * 512:(dh + 1) * 512], in0=psum[py][:, :],
                               scalar=wsel3[:, j, ex:ex + 1], in1=X3[:, j, dh * 512:(dh + 1) * 512],
                               op0=ALU.mult, op1=ALU.add), ["ps%d" % py, "wsel", "xc"], ["xc"])

            def gather_k(ex, k):
                pb = k % 2
                for li in range(16):
                    mm(psum[pb][:, 0:256], xn_all[:, li * D + k * 128:li * D + (k + 1) * 128], Sel3[:, li, :],
                       li == 0, li == 15, ["xnall", "Sel"], ["ps%d" % pb])
                act(_R("activation", out=xgT3[:, k, :], in_=psum[pb][:, 0:256], func=AF.Identity,
                       scale=A2l[:, k:k + 1], bias=B2l[:, k:k + 1]), ["ps%d" % pb, "AB"], ["xgT"])

            def down_proj(ex):
                for sc in range(2):
                    for dh in range(2):
                        py = 4 + ((sc * 2 + dh) % 2)
                        for fc in range(4):
                            mm(psum[py][:, :], hid3[:, fc, sc * 128:(sc + 1) * 128], wd3[:, fc, dh * 512:(dh + 1) * 512],
                               fc == 0, fc == 3, ["hid", "wd"], ["ps%d" % py])
                        act(_R("copy", out=ysb3[:, sc, dh * 512:(dh + 1) * 512], in_=psum[py][:, :]), ["ps%d" % py], ["ysb"])

            def selw_tr(ex, li):
                sw = selw[li % 2]
                swn = "selw%d" % (li % 2)
                dve(_R("tensor_scalar", out=sw[:, :], in0=iota_f[:, :], scalar1=rank3[:, li, ex:ex + 1],
                       scalar2=wsel3[:, 2 + li, ex:ex + 1], op0=ALU.is_equal, op1=ALU.mult),
                    ["iota_f", "rank", "wsel"], [swn])
                for sc in range(2):
                    tr(psb(6)[:, ((li % 4) * 2 + sc) * 128:((li % 4) * 2 + sc + 1) * 128], sw[:, sc * 128:(sc + 1) * 128],
                       ident_b[:, :], [swn, "ident_b"], ["ps6"])
                if li % 4 == 3:
                    for sc in range(2):
                        act(_R("copy", out=SelWT3[:, sc, (li - 3) * 128:(li + 1) * 128].rearrange("p (i t) -> p i t", i=4),
                               in_=psb(6)[:, :].rearrange("p (i c t) -> p i c t", i=4, c=2)[:, :, sc, :]),
                            ["ps6"], ["SelWT"])

            def scatter_tile(ex, li):
                for dh in range(2):
                    py = (7, 3)[srot[0] % 2]
                    srot[0] += 1
                    for sc in range(2):
                        mm(psum[py][:, :], SelWT3[:, sc, li * 128:(li + 1) * 128], ysb3[:, sc, dh * 512:(dh + 1) * 512],
                           sc == 0, sc == 1, ["SelWT", "ysb"], ["ps%d" % py])
                    dve(_R("tensor_tensor", out=X3[:, 2 + li, dh * 512:(dh + 1) * 512], in0=psum[py][:, :],
                           in1=X3[:, 2 + li, dh * 512:(dh + 1) * 512], op=ALU.add), ["ps%d" % py, "xc"], ["xc"])

            load_weights(0)
            build_sel(0, range(16))
            for ex in range(NE):
                for k in range(8):
                    gather_k(ex, k)
                    if ex > 0:
                        scatter_tile(ex - 1, 2 * k)
                        scatter_tile(ex - 1, 2 * k + 1)
                if has_ctx:
                    ctx_dense(ex)
                ffn_hidden(lambda k: xgT3[:, k, :], 256,
                           extra=(lambda fc, ex=ex: build_sel(ex + 1, range(4 * fc, 4 * fc + 4))) if ex + 1 < NE else None)
                down_proj(ex)
                if ex + 1 < NE:
                    load_weights(ex + 1)
                for li in range(16):
                    selw_tr(ex, li)
            for li in range(16):
                scatter_tile(NE - 1, li)
            if l == 0:
                dma("sp", xl1_d.rearrange("(n p) d -> p n d", p=128), X3[:, :, :], ["xc"], ["xsrc"])
            else:
                dve(_R("memset", mx[:, :], 0.0), [], ["mx"])
                for i in range(2, NT):
                    act(_R("activation", out=junk[:, :], in_=X3[:, i, :], func=AF.Square, scale=1.0 / 32.0,
                                                    accum_out=mx[:, i:i + 1]), ["xc", "mx"], ["junk", "mxa%d" % i])
                rsq(mx[:, 2:NT], ["mx"] + ["mxa%d" % i for i in range(2, NT)], ["mx"])
                for i in range(2, NT):
                    act(_R("activation", out=X3[:, i, :], in_=X3[:, i, :], func=AF.Copy, scale=mx[:, i:i + 1]),
                        ["xc", "mx"], ["xc"])
                    pool(_R("tensor_tensor", out=X3[:, i, :], in0=X3[:, i, :], in1=fgb[:, :], op=ALU.mult),
                         ["xc", "fgb"], ["xc"])
                dma("sp", out_d[s].rearrange("(n p) d -> p n d", p=128), X3[:, 2:NT, :], ["xc"], ["out"])
            P.barrier()

    P.barrier()
    P.emit(nc, stack)
    stack.close()
    return nc


def _consts():
    ident = np.eye(128, dtype=np.float32)
    k = np.arange(128)[:, None]
    q = np.arange(128)[None, :]
    masks = np.concatenate([(k >= q), (k <= q), (k < q)], axis=1).astype(np.float32)
    rows = SEQ // 64
    row = np.repeat(np.arange(rows, dtype=np.float32), 64)
    col = np.tile(np.arange(64, dtype=np.float32), rows)

    def table(rot):
        nf = rot // 4
        inv = (np.float32(10000.0) ** (-np.arange(nf, dtype=np.float32) / np.float32(nf))).astype(np.float32)
        ang = np.concatenate([row[:, None] * inv, col[:, None] * inv], axis=-1).astype(np.float32)
        cs, sn = np.cos(ang), np.sin(ang)
        t = np.concatenate([cs, cs, sn, sn], axis=-1).astype(np.float32)
        return np.ascontiguousarray(t.reshape(16, 128, 2 * rot).transpose(1, 0, 2).reshape(128, 32 * rot))

    iota = np.ascontiguousarray(np.broadcast_to(np.arange(256, dtype=np.float32)[None, :], (128, 256)))
    return ident, masks, table(32), table(64), iota


def kernel(**inputs):
    NS = 4
    ident, masks, ropem, ropes, iota = _consts()
    nc = build(NS=NS, NL=2)
    f = lambda a: np.ascontiguousarray(np.asarray(a, dtype=np.float32))
    shared = {k: f(inputs[k]) for k in ("ada_w", "ada_b", "norm1_g", "w_in", "mla_q_norm_g", "mla_w_uq",
                                        "mla_kv_norm_g", "mla_w_ukv", "swa_sink", "conv_w", "w_o", "norm2_g",
                                        "router_w", "exp_w_gate", "exp_w_up", "exp_w_down", "final_norm_g")}
    shared.update(k_ident=ident, k_masks=masks, k_ropem=ropem, k_ropes=ropes, k_iota=iota)
    x = f(inputs["x"])
    ctx = f(inputs["ctx"])
    c = f(inputs["c"])
    c_ctx = f(inputs["c_ctx"])
    in_maps = []
    for core in range(N_CORES):
        sl = slice(core * NS, (core + 1) * NS)
        m = dict(shared)
        m["x"] = np.ascontiguousarray(x[sl])
        m["ctx"] = np.ascontiguousarray(ctx[sl])
        m["cc"] = np.ascontiguousarray(np.concatenate([c[sl], c_ctx[None, :]], axis=0))
        in_maps.append(m)
    res = run_bass_kernel_spmd(nc, in_maps, core_ids=list(range(N_CORES)))
    return np.concatenate([r["out"] for r in res.results], axis=0)
```

```python
import math
from contextlib import ExitStack
import numpy as np
import concourse.bass as bass
import concourse.mybir as mybir
from concourse.bass_utils import run_bass_kernel_spmd

F32 = mybir.dt.float32
BF = mybir.dt.bfloat16
AF = mybir.ActivationFunctionType
ALU = mybir.AluOpType
AX = mybir.AxisListType

D = 1024
SEQ = 2048
CTX = 256
NT = 18
NTOK = NT * 128
EPS = 1e-6
MLA_SCALE = 96 ** -0.5
SWA_SCALE = 64 ** -0.5
NE = 16
NBIS = 22
N_CORES = 8

EP = 60000
EPD = 3900
NDMA = 8


_POS = {"memset": ("ap", "constant"), "matmul": ("out", "lhsT", "rhs"), "transpose": ("out", "in_", "identity")}


def _R(meth, *a, **k):
    for nm, v in zip(_POS.get(meth, ()), a):
        k[nm] = v
    return (meth, k)


class Prog:
    def __init__(self):
        self.streams = {e: [] for e in ("pe", "act", "dve", "pool", "sp")}
        self.count = {}
        self.lastw = {}
        self.readers = {}
        self.seen = {e: {} for e in self.streams}
        self.waited = {}
        self.rr = 0
        self.rrq = {}

    def _collect(self, eng, r, w, is_dma):
        deps = {}

        def add(t, same_ok):
            if t is None:
                return
            g, i = t
            if g == eng and not same_ok:
                return
            if deps.get(g, 0) < i:
                deps[g] = i

        for x in r:
            add(self.lastw.get(x), is_dma or eng != "pe")
        for x in w:
            add(self.lastw.get(x), is_dma)
            for g, i in self.readers.get(x, {}).items():
                add((g, i), is_dma)
        return deps

    def _waits(self, eng, deps):
        waits = []
        for g, i in deps.items():
            if self.seen[eng].get(g, 0) >= i:
                continue
            self.seen[eng][g] = i
            waits.append((g, i))
            self.waited.setdefault(g, set()).add(i)
        return waits

    def _mark(self, t, r, w):
        for x in r:
            d = self.readers.setdefault(x, {})
            if d.get(t[0], 0) < t[1]:
                d[t[0]] = t[1]
        for x in w:
            self.lastw[x] = t
            self.readers[x] = {}

    limit = None
    nrec = 0

    def _over(self):
        self.nrec += 1
        return self.limit is not None and self.nrec > self.limit

    def op(self, eng, fn, r=(), w=()):
        if self._over():
            return
        deps = self._collect(eng, r, w, False)
        waits = self._waits(eng, deps)
        n = self.count.get(eng, 0) + 1
        self.count[eng] = n
        self.streams[eng].append(("c", fn, waits, (eng, n)))
        self._mark((eng, n), r, w)

    def dma(self, q, fn, r=(), w=()):
        if self._over():
            return
        k = self.rrq.get(q, 0)
        self.rrq[q] = (k + 1) % NDMA
        g = "q%s%d" % (q, k)
        deps = self._collect(q, r, w, True)
        n = self.count.get(g, 0) + 1
        if n > 1:
            deps[g] = max(deps.get(g, 0), n - 1)
        waits = self._waits(q, deps)
        self.count[g] = n
        self.streams[q].append(("d", fn, waits, (g, n)))
        self._mark((g, n), r, w)

    def barrier(self):
        for e in self.streams:
            deps = {g: c for g, c in self.count.items() if g != e and c > 0}
            waits = self._waits(e, deps)
            if waits:
                self.streams[e].append(("w", None, waits, None))

    def emit(self, nc, stack):
        rank = {}
        nsem = {}
        for g, c in self.count.items():
            if g.startswith("q"):
                nsem[g] = (c + EPD - 1) // EPD
            else:
                ws = sorted(self.waited.get(g, ()))
                rank[g] = {i: k + 1 for k, i in enumerate(ws)}
                nsem[g] = max(1, (len(ws) + EP - 1) // EP)
        sems = {g: [stack.enter_context(nc.semaphore("s_%s_%d" % (g, k))) for k in range(n)]
                for g, n in nsem.items()}

        def semval(g, i):
            if g.startswith("q"):
                ep = (i - 1) // EPD
                return sems[g][ep], (i - ep * EPD) * 16
            v = rank[g][i]
            ep = (v - 1) // EP
            return sems[g][ep], v - ep * EP

        def replay(name, e):
            for kind, fn, waits, t in self.streams[name]:
                for g, i in waits:
                    s, v = semval(g, i)
                    e.wait_ge(s, v)
                if kind == "w":
                    continue
                ins = getattr(e, fn[0])(**fn[1])
                if kind == "d":
                    s, _ = semval(*t)
                    ins.then_inc(s, 16)
                elif t[1] in rank.get(t[0], {}):
                    s, _ = semval(*t)
                    ins.then_inc(s, 1)

        block = stack.enter_context(nc.Block())

        @block.tensor
        def _(e):
            replay("pe", e)

        @block.scalar
        def _(e):
            replay("act", e)

        @block.vector
        def _(e):
            replay("dve", e)

        @block.gpsimd
        def _(e):
            replay("pool", e)

        @block.sync
        def _(e):
            replay("sp", e)


LAYOUT = {}


class Arena:
    def __init__(self, t):
        self.t = t
        self.off = 0

    def alloc(self, nelem, dt, name=None):
        nb4 = nelem if dt == F32 else (nelem + 1) // 2
        if name is not None:
            LAYOUT[name] = (self.off, nelem, "f32" if dt == F32 else "bf16")
        v = self.t[:, self.off:self.off + nb4]
        self.off += nb4
        LAYOUT["_peak"] = max(LAYOUT.get("_peak", 0), self.off)
        assert self.off <= self.t.shape[1], ("arena overflow", self.off)
        if dt == F32:
            return v
        return v.bitcast(dt)[:, 0:nelem]


class _Stop(Exception):
    pass


NOROPE = False


def build(NS=4, NL=2, dumps=None, STOP=99, LIMIT=None):
    dumps = dumps or {}
    nc = bass.Bass("TRN2", target_bir_lowering=False)
    P = Prog()
    P.limit = LIMIT
    stack = ExitStack()

    def din(name, shape):
        return nc.dram_tensor(name, list(shape), F32, kind="ExternalInput").ap()

    x_d = din("x", (NS, SEQ, D))
    ctx_d = din("ctx", (NS, CTX, D))
    cc_d = din("cc", (NS + 1, D))
    ada_w = din("ada_w", (2, D, 6 * D))
    ada_b = din("ada_b", (2, 6 * D))
    n1g = din("norm1_g", (2, D))
    w_in = din("w_in", (2, D, 1824))
    qg_d = din("mla_q_norm_g", (2, 256))
    w_uq = din("mla_w_uq", (2, 256, 576))
    kvg_d = din("mla_kv_norm_g", (2, 128))
    w_ukv = din("mla_w_ukv", (2, 128, 768))
    sink_d = din("swa_sink", (2, 6))
    convw_d = din("conv_w", (2, 3, 256))
    w_o = din("w_o", (2, D, D))
    n2g = din("norm2_g", (2, D))
    rw_d = din("router_w", (2, D, NE))
    wg_d = din("exp_w_gate", (2, NE, D, 512))
    wu_d = din("exp_w_up", (2, NE, D, 512))
    wd_d = din("exp_w_down", (2, NE, 512, D))
    fg_d = din("final_norm_g", (D,))
    k_ident = din("k_ident", (128, 128))
    k_masks = din("k_masks", (128, 384))
    k_iota = din("k_iota", (128, 256))
    k_ropem = din("k_ropem", (128, 16 * 64))
    k_ropes = din("k_ropes", (128, 16 * 128))
    out_d = nc.dram_tensor("out", [NS, SEQ, D], F32, kind="ExternalOutput").ap()
    xmid_d = nc.dram_tensor("xmid", [NTOK, D], F32, kind="Internal").ap()
    xl1_d = nc.dram_tensor("xl1", [NTOK, D], F32, kind="Internal").ap()
    dump_d = {k: nc.dram_tensor("dbg_" + k, list(shp), F32, kind="ExternalOutput").ap()
              for k, shp in dumps.items()}

    def sb(name, shape, dt):
        return stack.enter_context(nc.sbuf_tensor(name, list(shape), dt))

    ident_f = sb("ident_f", (128, 128), F32)
    ident_b = sb("ident_b", (128, 128), BF)
    ones_f = sb("ones_f", (128, 128), F32)
    ones_b = sb("ones_b", (128, 128), BF)
    masks = sb("masks", (128, 384), BF)
    iota_f = sb("iota_f", (128, 256), F32)
    ropem = sb("ropem", (128, 1024), F32)
    ropes = sb("ropes", (128, 2048), F32)
    modT = sb("modT", (128, 2 * 48 * 5), F32)
    gT = sb("gT", (128, 2 * 2 * 8), F32)
    qgT = sb("qgT", (128, 4), F32)
    kvgT = sb("kvgT", (128, 2), F32)
    cwT = sb("cwT", (128, 2 * 2 * 3), F32)
    esink = sb("esink", (128, 12), F32)
    abT = sb("abT", (128, 2 * 48), F32)
    fgb = sb("fgb", (128, D), F32)
    kvec = sb("kvec", (128, 32), F32)
    AR = sb("arena", (128, 47300), F32)
    ar = Arena(AR)
    psum = [stack.enter_context(nc.psum_tensor("ps%d" % i, [128, 512], F32)) for i in range(8)]

    def psb(i):
        return psum[i][:, :].bitcast(BF)

    def act(fn, r, w):
        P.op("act", fn, r, w)

    def dve(fn, r, w):
        P.op("dve", fn, r, w)

    def pool(fn, r, w):
        P.op("pool", fn, r, w)

    def mm(out, lhsT, rhs, start, stop, r, w):
        P.op("pe", _R("matmul", out, lhsT, rhs, start=start, stop=stop, skip_group_check=True), r, w)

    def tr(out, in_, ident, r, w):
        P.op("pe", _R("transpose", out, in_, ident), r, w)

    def dma(q, out, in_, r, w, slow=False):
        if slow:
            P.dma(q, _R("dma_start", out=out, in_=in_, allow_slow_non_contiguous=True), r, w)
        else:
            P.dma(q, _R("dma_start", out=out, in_=in_), r, w)

    def rsq(ap, r, w):
        dve(_R("tensor_scalar", out=ap, in0=ap, scalar1=EPS, scalar2=None, op0=ALU.add), r, w)
        act(_R("activation", out=ap, in_=ap, func=AF.Ln), w, w)
        act(_R("activation", out=ap, in_=ap, func=AF.Exp, scale=-0.5), w, w)

    def dump(name, ap, idx=None):
        if name in dump_d:
            d = dump_d[name] if idx is None else dump_d[name][idx]
            P.barrier()
            dma("sp", d, ap, [], ["dump_" + name])


    dma("sp", ident_f[:, :], k_ident, [], ["ident_f"])
    dma("pool", ident_b[:, :], k_ident, [], ["ident_b"])
    dma("pool", masks[:, :], k_masks, [], ["masks"])
    dma("sp", iota_f[:, :], k_iota, [], ["iota_f"])
    dma("sp", ropem[:, :], k_ropem, [], ["ropem"])
    dma("sp", ropes[:, :], k_ropes, [], ["ropes"])
    dve(_R("memset", ones_f[:, :], 1.0), [], ["ones_f"])
    dve(_R("memset", ones_b[:, :], 1.0), [], ["ones_b"])
    dve(_R("memset", kvec[:, 0:16], 32.0), [], ["kvec"])
    dve(_R("memset", kvec[:, 16:32], 256.0), [], ["kvec"])
    ctxmgr = nc.allow_non_contiguous_dma(reason="tiny parameter vectors")
    ctxmgr.__enter__()
    for l in range(2):
        dma("sp", gT[:, (l * 2) * 8:(l * 2 + 1) * 8], n1g[l].rearrange("(k p) -> p k", p=128), [], ["gT"], slow=True)
        dma("sp", gT[:, (l * 2 + 1) * 8:(l * 2 + 2) * 8], n2g[l].rearrange("(k p) -> p k", p=128), [], ["gT"], slow=True)
        dma("sp", qgT[:, l * 2:l * 2 + 2], qg_d[l].rearrange("(k p) -> p k", p=128), [], ["qgT"], slow=True)
        dma("sp", kvgT[:, l:l + 1], kvg_d[l].rearrange("(k p) -> p k", p=128), [], ["kvgT"], slow=True)
        for c2 in range(2):
            dma("sp", cwT[:, (l * 2 + c2) * 3:(l * 2 + c2) * 3 + 3],
                convw_d[l][:, c2 * 128:(c2 + 1) * 128].rearrange("k p -> p k"), [], ["cwT"], slow=True)
        dma("sp", abT[:, l * 48:(l + 1) * 48], ada_b[l].rearrange("(j p) -> p j", p=128), [], ["abT"], slow=True)
        dma("sp", esink[:, l * 6:(l + 1) * 6], sink_d[l].partition_broadcast(128), [], ["esink"])
    ctxmgr.__exit__(None, None, None)
    dma("sp", fgb[:, :], fg_d.partition_broadcast(128), [], ["fgb"])
    act(_R("activation", out=esink[:, :], in_=esink[:, :], func=AF.Exp), ["esink"], ["esink"])

    ar.off = 0
    cc_sb = ar.alloc(D, F32, "cc_sb")
    cs_bf = ar.alloc(D, BF, "cs_bf")
    sT = ar.alloc(64, BF, "sT")
    awb = [ar.alloc(6 * D, BF), ar.alloc(6 * D, BF)]
    NB = NS + 1
    if STOP >= 1:
        dma("sp", cc_sb[0:NB, :], cc_d, [], ["cc_sb"])
        act(_R("activation", out=cs_bf[0:NB, :], in_=cc_sb[0:NB, :], func=AF.Silu), ["cc_sb"], ["cs_bf"])
        for k in range(8):
            tr(psb(0)[:, k * 8:k * 8 + NB], cs_bf[0:NB, k * 128:(k + 1) * 128], ident_b[0:NB, 0:NB],
               ["cs_bf", "ident_b"], ["ps0"])
        dve(_R("tensor_copy", out=sT[:, :].rearrange("p (k b) -> p k b", b=8)[:, :, 0:NB],
               in_=psb(0)[:, 0:64].rearrange("p (k b) -> p k b", b=8)[:, :, 0:NB]), ["ps0"], ["sT"])
        for l in range(NL):
            for k in range(8):
                b = awb[k % 2]
                dma("pool", b[:, :], ada_w[l, k * 128:(k + 1) * 128, :], [], ["awb%d" % (k % 2)])
                for j in range(48):
                    mm(psum[1][:, j * NB:(j + 1) * NB], b[:, j * 128:(j + 1) * 128], sT[:, k * 8:k * 8 + NB],
                       (k == 0 and j == 0), (k == 7 and j == 47), ["awb%d" % (k % 2), "sT"], ["ps1"])
            mview = modT[:, l * 240:l * 240 + 48 * NB].rearrange("p (j b) -> p j b", b=NB) if NB == 5 else \
                modT[:, l * 240:l * 240 + 48 * NB].rearrange("p (j b) -> p j b", b=NB)
            dve(_R("tensor_tensor",
                out=mview, in0=psum[1][:, 0:48 * NB].rearrange("p (j b) -> p j b", b=NB),
                in1=abT[:, l * 48:(l + 1) * 48].unsqueeze(2).broadcast_to([128, 48, NB]), op=ALU.add),
                ["ps1", "abT"], ["modT"])

    def modcol(l, m, b):
        return modT[:, l * 240:l * 240 + 48 * NB].rearrange("p (j b) -> p j b", b=NB)[:, m * 8:(m + 1) * 8, b]

    P.barrier()

    chunks = [(0, 2)] + [(2 + 4 * j, 4) for j in range(4)]
    LAYOUT["n1"] = P.nrec
    if STOP <= 1:
        NS = 0

    for s in range(NS):
        for l in range(NL):
            src_lat = x_d[s] if l == 0 else xl1_d[256:NTOK, :]
            src_ctx = ctx_d[s] if l == 0 else xl1_d[0:256, :]

            def src_tiles(t0, nt):
                if t0 == 0:
                    return src_ctx.rearrange("(n p) d -> p n d", p=128)
                return src_lat[(t0 - 2) * 128:(t0 - 2 + nt) * 128, :].rearrange("(n p) d -> p n d", p=128)

            ar.off = 0
            AB = ar.alloc(4 * 2 * 8, F32)
            KT_mla = ar.alloc(6 * NTOK, BF, "KT_mla")
            V_mla = ar.alloc(NT * 3 * 160, BF, "V_mla")
            KT_swa = ar.alloc(2 * NTOK, BF, "KT_swa")
            V_swa = ar.alloc(NT * 2 * 160, BF, "V_swa")
            convT = ar.alloc(2 * NTOK, BF, "convT")
            xc = ar.alloc(4 * D, F32, "xc")
            hT = ar.alloc(8 * 512, BF, "hT")
            xn = [ar.alloc(D, BF), ar.alloc(D, BF)]
            junk = ar.alloc(D, BF, "junk")
            tmpf = ar.alloc(D, F32, "tmpf")
            ss = ar.alloc(8, F32, "ss")
            gtmp = ar.alloc(512, F32, "gtmp")
            rx = ar.alloc(512, F32, "rx")
            ph_off = ar.off
            KT_mla3 = KT_mla.rearrange("p (h t) -> p h t", h=6)
            KT_swa3 = KT_swa.rearrange("p (g t) -> p g t", g=2)
            V_mla4 = V_mla.rearrange("p (i q c) -> p i q c", i=NT, q=3)
            V_swa4 = V_swa.rearrange("p (i g c) -> p i g c", i=NT, g=2)
            convT3 = convT.rearrange("p (c t) -> p c t", c=2)
            hT3 = hT.rearrange("p (k t) -> p k t", k=8)
            xc3 = xc.rearrange("p (n d) -> p n d", n=4)

            def ABv(which, seg):
                o = (which * 2 + seg) * 8
                return AB[:, o:o + 8]

            for seg in range(2):
                b = NS if seg == 0 else s
                for nrm in range(2):
                    dve(_R("scalar_tensor_tensor",
                        out=ABv(2 * nrm, seg), in0=modcol(l, 3 * nrm + 1, b), scalar=1.0,
                        in1=gT[:, (l * 2 + nrm) * 8:(l * 2 + nrm + 1) * 8], op0=ALU.add, op1=ALU.mult),
                        ["modT", "gT"], ["AB"])
                    dve(_R("tensor_copy",
                        out=ABv(2 * nrm + 1, seg), in_=modcol(l, 3 * nrm, b)), ["modT"], ["AB"])


            def gbuild(dst, col8, pbank):
                for k in range(8):
                    dve(_R("tensor_scalar", out=gtmp[:, (k % 4) * 128:(k % 4 + 1) * 128], in0=ident_f[:, :],
                                                       scalar1=col8[:, k:k + 1], scalar2=None, op0=ALU.mult),
                        ["ident_f", "modT"], ["gtmp%d" % (k % 4)])
                    mm(psum[pbank][:, (k % 4) * 128:(k % 4 + 1) * 128], ones_f[:, :],
                       gtmp[:, (k % 4) * 128:(k % 4 + 1) * 128], k % 4 == 0, k % 4 == 3,
                       ["ones_f", "gtmp%d" % (k % 4)], ["ps%d" % pbank])
                    if k % 4 == 3:
                        act(_R("copy", out=dst[:, (k - 3) * 128:(k + 1) * 128], in_=psum[pbank][:, :]),
                            ["ps%d" % pbank], ["gb"])

            def load_chunk(t0, nt):
                dma("sp", xc3[:, 0:nt, :], src_tiles(t0, nt), ["xsrc"], ["xc"])

            def norm_T(t0, nt, which, dst3, dcol0, pb0, hname="hT", keep=None, xname="xc"):
                seg = 0 if t0 == 0 else 1
                A = ABv(2 * which, seg)
                B = ABv(2 * which + 1, seg)
                dve(_R("memset", ss[:, :], 0.0), [], ["ss"])
                for j in range(nt):
                    act(_R("activation", out=junk[:, :], in_=xc3[:, j, :], func=AF.Square, scale=1.0 / 32.0,
                                                    accum_out=ss[:, j:j + 1]), [xname, "ss"], ["junk", "ss%d" % j])
                rsq(ss[:, 0:nt], ["ss"] + ["ss%d" % j for j in range(nt)], ["ss"])
                for j in range(nt):
                    xb = xn[j % 2]
                    xbn = "xn%d" % (j % 2)
                    if keep is not None:
                        xb, xbn = keep(j)
                    pb = pb0 + (j % 2)
                    act(_R("activation", out=xb[:, :], in_=xc3[:, j, :], func=AF.Copy,
                                                           scale=ss[:, j:j + 1]), [xname, "ss"], [xbn])
                    for k in range(8):
                        tr(psb(pb)[:, k * 128:(k + 1) * 128], xb[:, k * 128:(k + 1) * 128], ident_b[:, :],
                           [xbn, "ident_b"], ["ps%d" % pb])
                    dve(_R("tensor_tensor",
                        out=tmpf.rearrange("p (k t) -> p k t", k=8),
                        in0=psb(pb).rearrange("p (k t) -> p k t", k=8),
                        in1=A.unsqueeze(2).broadcast_to([128, 8, 128]), op=ALU.mult),
                        ["ps%d" % pb, "AB"], ["tmpf"])
                    dve(_R("tensor_tensor",
                        out=dst3[:, :, dcol0 + j * 128:dcol0 + (j + 1) * 128],
                        in0=tmpf.rearrange("p (k t) -> p k t", k=8),
                        in1=B.unsqueeze(2).broadcast_to([128, 8, 128]), op=ALU.add),
                        ["tmpf", "AB"], [hname])

            def rope(out3, in3, ti, kind, H, u, v, rr, ww):
                if NOROPE:
                    act(_R("copy", out=out3, in_=in3), rr, ww)
                    return
                rot = 32 if kind == "m" else 64
                half = rot // 2
                tab = ropem if kind == "m" else ropes
                cosd = tab[:, ti * 2 * rot:ti * 2 * rot + rot].unsqueeze(1).broadcast_to([128, H, rot])
                sind = tab[:, ti * 2 * rot + rot:(ti + 1) * 2 * rot].unsqueeze(1).broadcast_to([128, H, rot])
                n = H * rot
                u3 = u[:, 0:n].rearrange("p (h d) -> p h d", h=H)
                v3 = v[:, 0:n].rearrange("p (h d) -> p h d", h=H)
                x3 = rx[:, 0:n].rearrange("p (h d) -> p h d", h=H)
                act(_R("copy", out=x3, in_=in3), rr, ["rope_x"])
                pool(_R("tensor_tensor", out=u3, in0=x3, in1=cosd, op=ALU.mult), ["rope_x"] + rr, ["rope_u"])
                pool(_R("tensor_tensor", out=v3, in0=x3, in1=sind, op=ALU.mult), ["rope_x"] + rr, ["rope_v"])
                dve(_R("tensor_tensor", out=out3[:, :, 0:half], in0=u3[:, :, 0:half], in1=v3[:, :, half:rot],
                       op=ALU.subtract), ["rope_u", "rope_v"], ww)
                dve(_R("tensor_tensor", out=out3[:, :, half:rot], in0=u3[:, :, half:rot], in1=v3[:, :, 0:half],
                       op=ALU.add), ["rope_u", "rope_v"], ww)

            ar.off = ph_off
            wA = ar.alloc(8 * 1184, BF, "wA")
            wA3 = wA.rearrange("p (k c) -> p k c", k=8)
            wkv_f = ar.alloc(768, F32, "wkv_f")
            wkv = ar.alloc(768, BF, "wkv")
            zT = ar.alloc(2 * 2308, BF, "zT")
            zT3 = zT.rearrange("p (c t) -> p c t", c=2)
            BT = ar.alloc(2 * NTOK, BF, "BT")
            BT3 = BT.rearrange("p (c t) -> p c t", c=2)
            ckvnb = [ar.alloc(128, BF) for _ in range(2)]
            ckvnTb = [ar.alloc(128, BF) for _ in range(2)]
            krb = [ar.alloc(32, BF) for _ in range(2)]
            ktokb = [ar.alloc(6 * 96, BF) for _ in range(2)]
            kswb = [ar.alloc(128, BF) for _ in range(2)]
            kdupb = [ar.alloc(256, BF) for _ in range(2)]
            sskb = [ar.alloc(2, F32) for _ in range(2)]
            hT_b = ar.alloc(8 * 512, BF, "hT_b")
            hTdb = [hT3, hT_b.rearrange("p (k t) -> p k t", k=8)]
            ru = ar.alloc(512, F32, "ru")
            rv = ar.alloc(512, F32, "rv")
            tmpC = ar.alloc(512, F32, "tmpC")
            ssk = ar.alloc(2, F32, "ssk")

            w_l = w_in[l].rearrange("(k p) c -> p k c", p=128)
            dma("pool", wA3[:, :, 0:160], w_l[:, :, 256:416], [], ["wA"])
            dma("pool", wA3[:, :, 160:416], w_l[:, :, 800:1056], [], ["wA"])
            dma("pool", wA3[:, :, 416:1184], w_l[:, :, 1056:1824], [], ["wAc"])
            with nc.allow_non_contiguous_dma(reason="head-split weight layout"):
                for two in range(2):
                    dma("sp", wkv_f[:, two * 384:(two + 1) * 384].rearrange("p (h d) -> p h d", h=6),
                        w_ukv[l].rearrange("r (h two d) -> r two h d", h=6, two=2)[:, two], [], ["wkv_f"], slow=True)
            dve(_R("tensor_scalar", out=wkv[:, :], in0=wkv_f[:, :], scalar1=kvgT[:, l:l + 1], scalar2=None,
                                          op0=ALU.mult), ["wkv_f", "kvgT"], ["wkv"])
            for (za, zb) in ((0, 1), (257, 259), (2307, 2308)):
                dve(_R("memset", zT3[:, :, za:zb], 0.0), [], ["zT"])
            dve(_R("memset", V_mla4[:, :, :, 64:96], 0.0), [], ["V_mla"])
            dve(_R("memset", V_swa4[:, :, :, 64:96], 0.0), [], ["V_swa"])
            dve(_R("memset", V_mla4[:, :, :, 64:65], 1.0), [], ["V_mla"])
            dve(_R("memset", V_swa4[:, :, :, 64:65], 1.0), [], ["V_swa"])

            def zcol(t):
                return 1 + t if t < 256 else 3 + t

            def kvM0(j, i, hTb, hname):
                p = i % 2
                hsl = slice(j * 128, (j + 1) * 128)
                pa = 2 + p
                pan = "ps%d" % pa
                for k in range(8):
                    mm(psum[pa][:, 0:416], hTb[:, k, hsl], wA3[:, k, 0:416], k == 0, k == 7, [hname, "wA"], [pan])
                dve(_R("memset", sskb[p][:, :], 0.0), [], ["ssk%d" % p])
                act(_R("activation", out=junk[:, 0:128], in_=psum[pa][:, 0:128], func=AF.Square,
                       scale=128 ** -0.5, accum_out=sskb[p][:, 0:1]), [pan, "ssk%d" % p], ["junk", "ssk%d" % p])
                rsq(sskb[p][:, 0:1], ["ssk%d" % p], ["ssk%d" % p])
                act(_R("activation", out=ckvnb[p][:, :], in_=psum[pa][:, 0:128], func=AF.Copy,
                       scale=sskb[p][:, 0:1]), [pan, "ssk%d" % p], ["ckvn%d" % p])
                if i >= 2:
                    ti = i - 2
                    rope(krb[p].rearrange("p (h d) -> p h d", h=1),
                         psum[pa][:, 128:160].rearrange("p (h d) -> p h d", h=1), ti, "m", 1, ru, rv,
                         [pan, "ropem"], ["kr%d" % p])
                    rope(kswb[p].rearrange("p (h d) -> p h d", h=2),
                         psum[pa][:, 160:288].rearrange("p (h d) -> p h d", h=2), ti, "s", 2, ru, rv,
                         [pan, "ropes"], ["ksw%d" % p])
                else:
                    act(_R("copy", out=krb[p][:, :], in_=psum[pa][:, 128:160]), [pan], ["kr%d" % p])
                    act(_R("copy", out=kswb[p][:, :], in_=psum[pa][:, 160:288]), [pan], ["ksw%d" % p])
                pool(_R("tensor_copy", out=kdupb[p].rearrange("p (g c d) -> p g c d", g=2, c=2),
                        in_=kswb[p].rearrange("p (g d) -> p g d", g=2).unsqueeze(2).broadcast_to([128, 2, 2, 64])),
                     ["ksw%d" % p], ["kdup%d" % p])
                for c0 in (0, 96):
                    act(_R("copy", out=V_swa4[:, i, :, c0:c0 + 64],
                           in_=psum[pa][:, 288:416].rearrange("p (g d) -> p g d", g=2)), [pan], ["V_swa"])

            def kvM1(j, i, hTb, hname):
                p = i % 2
                cols = slice(i * 128, (i + 1) * 128)
                tr(psb(4)[:, 0:128], ckvnb[p][:, :], ident_b[:, :], ["ckvn%d" % p, "ident_b"], ["ps4"])
                for g in range(2):
                    tr(psb(4)[:, 128 + g * 128:256 + g * 128], kdupb[p][:, g * 128:(g + 1) * 128], ident_b[:, :],
                       ["kdup%d" % p, "ident_b"], ["ps4"])
                act(_R("copy", out=ckvnTb[p][:, :], in_=psb(4)[:, 0:128]), ["ps4"], ["ckvnT%d" % p])
                act(_R("copy", out=KT_swa3[:, :, cols], in_=psb(4)[:, 128:384].rearrange("p (g t) -> p g t", g=2)),
                    ["ps4"], ["KT_swa"])

            def kvM2(j, i, hTb, hname):
                p = i % 2
                mm(psum[5][:, 0:384], ckvnTb[p][:, :], wkv[:, 0:384], True, True, ["ckvnT%d" % p, "wkv"], ["ps5"])
                mm(psum[6][:, 0:384], ckvnTb[p][:, :], wkv[:, 384:768], True, True, ["ckvnT%d" % p, "wkv"], ["ps6"])
                pv4 = psum[6][:, 0:384].rearrange("p (q two d) -> p q two d", q=3, two=2)
                act(_R("copy", out=V_mla4[:, i, :, 0:64], in_=pv4[:, :, 0, :]), ["ps6"], ["V_mla"])
                act(_R("copy", out=V_mla4[:, i, :, 96:160], in_=pv4[:, :, 1, :]), ["ps6"], ["V_mla"])
                kt3 = ktokb[p].rearrange("p (h d) -> p h d", h=6)
                dve(_R("tensor_copy", out=kt3[:, :, 0:64], in_=psum[5][:, 0:384].rearrange("p (h d) -> p h d", h=6)),
                    ["ps5"], ["ktok%d" % p])
                pool(_R("tensor_copy", out=kt3[:, :, 64:96], in_=krb[p].unsqueeze(1).broadcast_to([128, 6, 32])),
                     ["kr%d" % p], ["ktok%d" % p])

            def kvM3(j, i, hTb, hname):
                p = i % 2
                cols = slice(i * 128, (i + 1) * 128)
                kt3 = ktokb[p].rearrange("p (h d) -> p h d", h=6)
                for h in range(6):
                    tr(psb(7)[0:96, h * 128:(h + 1) * 128], kt3[:, h, :], ident_b[:, :], ["ktok%d" % p, "ident_b"], ["ps7"])
                dve(_R("tensor_copy", out=KT_mla3[0:96, :, cols],
                       in_=psb(7)[0:96, 0:768].rearrange("p (h t) -> p h t", h=6)), ["ps7"], ["KT_mla"])

            def conv_chunk(t0, nt, hTb, hname):
                n = nt * 128
                g0 = t0 * 128
                for c2 in range(2):
                    for which, pbk in ((1, 5), (2, 6), (0, 7)):
                        cb = 416 + (which * 2 + c2) * 128
                        for k in range(8):
                            mm(psum[pbk][:, 0:n], wA3[:, k, cb:cb + 128], hTb[:, k, 0:n], k == 0, k == 7,
                               [hname, "wAc"], ["ps%d" % pbk])
                    act(_R("copy", out=tmpC[:, 0:n], in_=psum[5][:, 0:n]), ["ps5"], ["tmpC"])
                    dve(_R("tensor_tensor", out=zT3[:, c2, zcol(g0):zcol(g0) + n], in0=tmpC[:, 0:n], in1=psum[6][:, 0:n],
                           op=ALU.mult), ["tmpC", "ps6"], ["zT"])
                    act(_R("copy", out=BT3[:, c2, g0:g0 + n], in_=psum[7][:, 0:n]), ["ps7"], ["BT"])

            kv_stages = [kvM0, kvM1, kvM2, kvM3]
            tiles_a = []
            for ci, (t0, nt) in enumerate(chunks):
                for j in range(nt):
                    tiles_a.append((ci, t0, nt, j, t0 + j))
            for step in range(len(tiles_a) + len(kv_stages) - 1):
                if step < len(tiles_a):
                    ci, t0, nt, j, i = tiles_a[step]
                    if j == 0:
                        load_chunk(t0, nt)
                        norm_T(t0, nt, 0, hTdb[ci % 2], 0, 0, hname="hT%d" % (ci % 2))
                for sidx in reversed(range(len(kv_stages))):
                    t = step - sidx
                    if 0 <= t < len(tiles_a):
                        ci, t0, nt, j, i = tiles_a[t]
                        kv_stages[sidx](j, i, hTdb[ci % 2], "hT%d" % (ci % 2))
                        if sidx == 0 and j == nt - 1:
                            conv_chunk(t0, nt, hTdb[ci % 2], "hT%d" % (ci % 2))
            for c2 in range(2):
                cw = cwT[:, (l * 2 + c2) * 3:(l * 2 + c2) * 3 + 3]
                for g0, n in [(0, 256)] + [(256 + 512 * j, 512) for j in range(4)]:
                    zc = zcol(g0)
                    dve(_R("tensor_scalar",
                        out=tmpf[:, 0:n], in0=zT3[:, c2, zc - 1:zc - 1 + n], scalar1=cw[:, 0:1], scalar2=None,
                        op0=ALU.mult), ["zT", "cwT"], ["tmpf"])
                    dve(_R("scalar_tensor_tensor",
                        out=tmpf[:, 0:n], in0=zT3[:, c2, zc:zc + n], scalar=cw[:, 1:2], in1=tmpf[:, 0:n],
                        op0=ALU.mult, op1=ALU.add), ["zT", "cwT", "tmpf"], ["tmpf"])
                    dve(_R("scalar_tensor_tensor",
                        out=tmpf[:, 0:n], in0=zT3[:, c2, zc + 1:zc + 1 + n], scalar=cw[:, 2:3], in1=tmpf[:, 0:n],
                        op0=ALU.mult, op1=ALU.add), ["zT", "cwT", "tmpf"], ["tmpf"])
                    dve(_R("tensor_tensor",
                        out=convT3[:, c2, g0:g0 + n], in0=tmpf[:, 0:n], in1=BT3[:, c2, g0:g0 + n], op=ALU.mult),
                        ["tmpf", "BT"], ["convT"])
            P.barrier()
            LAYOUT["n2"] = P.nrec
            if STOP <= 2:
                continue

            ar.off = ph_off
            wB = ar.alloc(8 * 640, BF, "wB")
            wB3 = wB.rearrange("p (k c) -> p k c", k=8)
            wuq_f = ar.alloc(2 * 576, F32, "wuq_f")
            wuq = ar.alloc(2 * 576, BF, "wuq")
            wuq3 = wuq.rearrange("p (r c) -> p r c", r=2)
            wo = ar.alloc(8 * D, BF, "wo")
            wo3 = wo.rearrange("p (f d) -> p f d", f=8)
            Gb = ar.alloc(D, F32, "Gb")
            QT_mla = ar.alloc(6 * 512, BF, "QT_mla")
            QT_mla3 = QT_mla.rearrange("p (h t) -> p h t", h=6)
            QT_swa = ar.alloc(3 * 512, BF, "QT_swa")
            QT_swa3 = QT_swa.rearrange("p (h t) -> p h t", h=3)
            mixT = ar.alloc(6 * 512, BF, "mixT")
            mixT3 = mixT.rearrange("p (f t) -> p f t", f=6)
            PT = [ar.alloc(512, BF) for _ in range(6)]
            cqnb = [ar.alloc(256, BF) for _ in range(2)]
            cqnTb = [ar.alloc(256, BF) for _ in range(2)]
            qtokb = [ar.alloc(6 * 96, BF) for _ in range(2)]
            qswb = [ar.alloc(384, BF) for _ in range(2)]
            ru = ar.alloc(512, F32, "ru")
            rv = ar.alloc(512, F32, "rv")
            rs = ar.alloc(512, F32, "rs")
            bcs = ar.alloc(512, F32, "bcs")
            ssqb = [ar.alloc(2, F32) for _ in range(2)]

            dma("pool", wB3[:, :, 0:256], w_l[:, :, 0:256], [], ["wB"])
            dma("pool", wB3[:, :, 256:640], w_l[:, :, 416:800], [], ["wB"])
            dma("sp", wuq_f.rearrange("p (r c) -> p r c", r=2), w_uq[l].rearrange("(r p) c -> p r c", p=128),
                [], ["wuq_f"])
            for r_ in range(2):
                dve(_R("tensor_scalar", out=wuq3[:, r_, :], in0=wuq_f[:, r_ * 576:(r_ + 1) * 576],
                                                     scalar1=qgT[:, l * 2 + r_:l * 2 + r_ + 1], scalar2=None,
                                                     op0=ALU.mult), ["wuq_f", "qgT"], ["wuq"])
            dma("pool", wo3[:, :, :], w_o[l].rearrange("(f p) d -> p f d", p=128), [], ["wo"])

            SBK = [0, 1, 2, 3, 7]
            NPT = len(PT)
            att_state = {"pti": 0, "pending": None}

            def attention(nq, heads, qlhs, klhs, vwin, kblocks, scale, sink_l, mix_chunk0):
                LOOK = 4

                def finalize(h, po):
                    half = h % 2
                    r0 = 64 if half == 0 else 32
                    rows = slice(0, 64) if half == 0 else slice(64, 128)
                    if sink_l is not None:
                        dve(_R("tensor_scalar", out=rs[r0:r0 + 1, 0:nq], in0=psum[po][r0:r0 + 1, 0:nq],
                               scalar1=esink[r0:r0 + 1, sink_l * 6 + h:sink_l * 6 + h + 1], scalar2=None, op0=ALU.add),
                            ["ps%d" % po, "esink"], ["rs"])
                        dve(_R("reciprocal", out=rs[r0:r0 + 1, 0:nq], in_=rs[r0:r0 + 1, 0:nq]), ["rs"], ["rs"])
                    else:
                        dve(_R("reciprocal", out=rs[r0:r0 + 1, 0:nq], in_=psum[po][r0:r0 + 1, 0:nq]),
                            ["ps%d" % po], ["rs"])
                    mm(psum[6][:, 0:nq], ones_f[r0:r0 + 1, :], rs[r0:r0 + 1, 0:nq], True, True,
                       ["rs", "ones_f"], ["ps6"])
                    act(_R("copy", out=bcs[:, 0:nq], in_=psum[6][:, 0:nq]), ["ps6"], ["bcs"])
                    dve(_R("tensor_tensor", out=mixT3[rows, mix_chunk0 + h // 2, 0:nq], in0=psum[po][rows, 0:nq],
                           in1=bcs[rows, 0:nq], op=ALU.mult), ["ps%d" % po, "bcs"], ["mixT"])

                for h in heads:
                    po = 4 + (h % 2)
                    blocks = kblocks(h)
                    ptinfo = {}

                    def issue_S(bi, h=h, blocks=blocks, ptinfo=ptinfo):
                        kb, c0, c1, mks = blocks[bi]
                        psn = SBK[att_state["pti"] % len(SBK)]
                        mm(psum[psn][:, c0:c1], klhs(h, kb), qlhs(h, c0, c1), True, True,
                           ["KT_mla", "KT_swa", "QT"], ["ps%d" % psn])
                        pt = PT[att_state["pti"] % NPT]
                        ptn = "PT%d" % (att_state["pti"] % NPT)
                        att_state["pti"] += 1
                        ptinfo[bi] = (pt, ptn)
                        act(_R("activation", out=pt[:, c0:c1], in_=psum[psn][:, c0:c1], func=AF.Exp, scale=scale),
                            ["ps%d" % psn], [ptn])
                        for (m0, mk) in mks:
                            pool(_R("tensor_tensor", out=pt[:, m0:m0 + 128], in0=pt[:, m0:m0 + 128],
                                    in1=masks[:, mk * 128:(mk + 1) * 128], op=ALU.mult), [ptn, "masks"], [ptn])

                    for bi in range(min(LOOK, len(blocks))):
                        issue_S(bi)
                    for bi, (kb, c0, c1, mks) in enumerate(blocks):
                        if bi + LOOK < len(blocks):
                            issue_S(bi + LOOK)
                        if bi == 1 and att_state["pending"] is not None:
                            att_state["pending"]()
                            att_state["pending"] = None
                        pt, ptn = ptinfo[bi]
                        mm(psum[po][:, c0:c1], vwin(h, kb), pt[:, c0:c1], bi == 0, bi == len(blocks) - 1,
                           [ptn, "V_mla", "V_swa"], ["ps%d" % po])
                    if att_state["pending"] is not None:
                        att_state["pending"]()
                    att_state["pending"] = (lambda h=h, po=po: finalize(h, po))

            def attention_flush():
                if att_state["pending"] is not None:
                    att_state["pending"]()
                    att_state["pending"] = None

            first_lat = True
            for (t0, nt) in chunks:
                if t0 == 0 and l == 1:
                    continue
                seg = 0 if t0 == 0 else 1
                nq = nt * 128
                if t0 == 0 or first_lat:
                    gbuild(Gb, modcol(l, 2, NS if t0 == 0 else s), 7)
                    if t0 != 0:
                        first_lat = False
                load_chunk(t0, nt)
                norm_T(t0, nt, 0, hT3, 0, 0)
                def qM0(j, i):
                    p = i % 2
                    hsl = slice(j * 128, (j + 1) * 128)
                    pq, ps_ = (2, 3) if p == 0 else (0, 1)
                    for k in range(8):
                        mm(psum[pq][:, 0:256], hT3[:, k, hsl], wB3[:, k, 0:256], k == 0, k == 7, ["hT", "wB"], ["ps%d" % pq])
                    for k in range(8):
                        mm(psum[ps_][:, 0:384], hT3[:, k, hsl], wB3[:, k, 256:640], k == 0, k == 7, ["hT", "wB"], ["ps%d" % ps_])
                    dve(_R("memset", ssqb[p][:, :], 0.0), [], ["ssq%d" % p])
                    act(_R("activation", out=junk[:, 0:256], in_=psum[pq][:, 0:256], func=AF.Square,
                           scale=1.0 / 16.0, accum_out=ssqb[p][:, 0:1]), ["ps%d" % pq, "ssq%d" % p], ["junk", "ssq%d" % p])
                    rsq(ssqb[p][:, 0:1], ["ssq%d" % p], ["ssq%d" % p])
                    act(_R("activation", out=cqnb[p][:, :], in_=psum[pq][:, 0:256], func=AF.Copy, scale=ssqb[p][:, 0:1]),
                        ["ps%d" % pq, "ssq%d" % p], ["cqn%d" % p])
                    if i >= 2:
                        rope(qswb[p].rearrange("p (h d) -> p h d", h=6),
                             psum[ps_][:, 0:384].rearrange("p (h d) -> p h d", h=6), i - 2, "s", 6, ru, rv,
                             ["ps%d" % ps_, "ropes"], ["qsw%d" % p])
                    else:
                        act(_R("copy", out=qswb[p][:, :], in_=psum[ps_][:, 0:384]), ["ps%d" % ps_], ["qsw%d" % p])

                def qM1(j, i):
                    p = i % 2
                    hsl = slice(j * 128, (j + 1) * 128)
                    for r_ in range(2):
                        tr(psb(4)[:, r_ * 128:(r_ + 1) * 128], cqnb[p][:, r_ * 128:(r_ + 1) * 128], ident_b[:, :],
                           ["cqn%d" % p, "ident_b"], ["ps4"])
                    for pr in range(3):
                        tr(psb(4)[:, 256 + pr * 128:256 + (pr + 1) * 128], qswb[p][:, pr * 128:(pr + 1) * 128], ident_b[:, :],
                           ["qsw%d" % p, "ident_b"], ["ps4"])
                    act(_R("copy", out=cqnTb[p][:, :], in_=psb(4)[:, 0:256]), ["ps4"], ["cqnT%d" % p])
                    act(_R("copy", out=QT_swa3[:, :, hsl], in_=psb(4)[:, 256:640].rearrange("p (h t) -> p h t", h=3)),
                        ["ps4"], ["QT"])

                def qM2(j, i):
                    p = i % 2
                    for hh in range(2):
                        for r_ in range(2):
                            mm(psum[5 + hh][:, 0:288], cqnTb[p][:, r_ * 128:(r_ + 1) * 128],
                               wuq3[:, r_, hh * 288:(hh + 1) * 288], r_ == 0, r_ == 1, ["cqnT%d" % p, "wuq"], ["ps%d" % (5 + hh)])
                    qt3 = qtokb[p].rearrange("p (h d) -> p h d", h=6)
                    for hh in range(2):
                        pq3 = psum[5 + hh][:, 0:288].rearrange("p (h d) -> p h d", h=3)
                        if i >= 2:
                            act(_R("copy", out=qt3[:, hh * 3:(hh + 1) * 3, 0:64], in_=pq3[:, :, 0:64]),
                                ["ps%d" % (5 + hh)], ["qtok%d" % p])
                            rope(qt3[:, hh * 3:(hh + 1) * 3, 64:96], pq3[:, :, 64:96], i - 2, "m", 3, ru, rv,
                                 ["ps%d" % (5 + hh), "ropem"], ["qtok%d" % p])
                        else:
                            act(_R("copy", out=qt3[:, hh * 3:(hh + 1) * 3, :], in_=pq3), ["ps%d" % (5 + hh)], ["qtok%d" % p])

                def qM3(j, i):
                    p = i % 2
                    hsl = slice(j * 128, (j + 1) * 128)
                    qt3 = qtokb[p].rearrange("p (h d) -> p h d", h=6)
                    for h in range(6):
                        tr(psb(7)[0:96, h * 128:(h + 1) * 128], qt3[:, h, :], ident_b[:, :], ["qtok%d" % p, "ident_b"], ["ps7"])
                    dve(_R("tensor_copy", out=QT_mla3[0:96, :, hsl],
                           in_=psb(7)[0:96, 0:768].rearrange("p (h t) -> p h t", h=6)), ["ps7"], ["QT"])

                q_stages = [qM0, qM1, qM2, qM3]
                for step in range(nt + len(q_stages) - 1):
                    for sidx in reversed(range(len(q_stages))):
                        j = step - sidx
                        if 0 <= j < nt:
                            q_stages[sidx](j, t0 + j)

                if t0 == 0:
                    mla_blocks = lambda h: [(kb, 0, nq, []) for kb in range(2)]
                else:
                    mla_blocks = lambda h: [(kb, 0, nq, []) for kb in range(NT)]
                attention(nq, range(6),
                          lambda h, c0, c1: QT_mla3[0:96, h, c0:c1],
                          lambda h, kb: KT_mla3[0:96, h, kb * 128:(kb + 1) * 128],
                          lambda h, kb: V_mla4[:, kb, h // 2, (0 if h % 2 == 0 else 32):(128 if h % 2 == 0 else 160)],
                          mla_blocks, MLA_SCALE, None, 0)
                if t0 == 0:
                    swa_blocks = lambda h: [(kb, 0, nq, []) for kb in range(2)]
                else:
                    qb0 = t0 - 2

                    def swa_blocks(h, qb0=qb0):
                        bl = [(kb, 0, nq, []) for kb in range(2)]
                        for lb in range(max(0, qb0 - 1), min(15, qb0 + 4) + 1):
                            qlo = max(lb - 1, qb0)
                            qhi = min(lb + 1, qb0 + 3)
                            mks = []
                            for qb in range(qlo, qhi + 1):
                                if qb == lb + 1:
                                    mks.append(((qb - qb0) * 128, 0))
                                elif qb == lb - 1:
                                    mks.append(((qb - qb0) * 128, 1))
                            bl.append((2 + lb, (qlo - qb0) * 128, (qhi - qb0 + 1) * 128, mks))
                        return bl
                attention(nq, range(6),
                          lambda h, c0, c1: QT_swa3[(h % 2) * 64:(h % 2) * 64 + 64, h // 2, c0:c1],
                          lambda h, kb: KT_swa3[(h % 2) * 64:(h % 2) * 64 + 64, h // 3, kb * 128:(kb + 1) * 128],
                          lambda h, kb: V_swa4[:, kb, h // 3, (0 if h % 2 == 0 else 32):(128 if h % 2 == 0 else 160)],
                          swa_blocks, SWA_SCALE, l, 3)
                attention_flush()
                for j in range(nt):
                    i = t0 + j
                    for dh in range(2):
                        pbk = dh
                        for f in range(8):
                            lhs = mixT3[:, f, j * 128:(j + 1) * 128] if f < 6 else convT3[:, f - 6, i * 128:(i + 1) * 128]
                            mm(psum[pbk][:, :], lhs, wo3[:, f, dh * 512:(dh + 1) * 512], f == 0, f == 7,
                               ["mixT", "convT", "wo"], ["ps%d" % pbk])
                        dve(_R("tensor_tensor",
                            out=tmpf[:, dh * 512:(dh + 1) * 512], in0=psum[pbk][:, :], in1=Gb[:, dh * 512:(dh + 1) * 512],
                            op=ALU.mult), ["ps%d" % pbk, "gb"], ["tmpf"])
                        dve(_R("tensor_tensor",
                            out=xc3[:, j, dh * 512:(dh + 1) * 512], in0=xc3[:, j, dh * 512:(dh + 1) * 512],
                            in1=tmpf[:, dh * 512:(dh + 1) * 512], op=ALU.add), ["tmpf", "xc"], ["xc"])
                dma("sp", xmid_d[t0 * 128:(t0 + nt) * 128, :].rearrange("(n p) d -> p n d", p=128), xc3[:, 0:nt, :],
                    ["xc"], ["xmid"])
            P.barrier()
            if STOP <= 3:
                continue

            has_ctx = (l == 0)
            tlo = 0 if has_ctx else 2
            ar.off = 0
            AB2 = ar.alloc(4 * 2 * 8, F32)
            X = ar.alloc(NT * D, F32, "X")
            X3 = X.rearrange("p (n d) -> p n d", n=NT)
            G2 = [ar.alloc(D, F32), ar.alloc(D, F32)]
            wsel = ar.alloc(NT * NE, F32, "wsel")
            wsel3 = wsel.rearrange("p (t e) -> p t e", t=NT)
            maskf = ar.alloc(NT * NE, F32, "maskf")
            maskf3 = maskf.rearrange("p (t e) -> p t e", t=NT)
            rank = ar.alloc(16 * NE, F32, "rank")
            rank3 = rank.rearrange("p (t e) -> p t e", t=16)
            xn_all = ar.alloc(16 * D, BF, "xn_all")
            h2c = ar.alloc(8 * 256, BF, "h2c")
            h2c3 = h2c.rearrange("p (k t) -> p k t", k=8)
            c2_off = ar.off
            h2T = ar.alloc(8 * NTOK, BF, "h2T")
            h2T3 = h2T.rearrange("p (k t) -> p k t", k=8)
            maskb = ar.alloc(NT * NE, BF, "maskb")
            maskb3 = maskb.rearrange("p (t e) -> p t e", t=NT)
            xn = [ar.alloc(D, BF), ar.alloc(D, BF)]
            junk = ar.alloc(D, BF, "junk")
            tmpf = ar.alloc(D, F32, "tmpf")
            ss = ar.alloc(8, F32, "ss")
            gtmp = ar.alloc(512, F32, "gtmp")
            rwb = ar.alloc(8 * NE, BF, "rwb")
            rwb3 = rwb.rearrange("p (k e) -> p k e", k=8)
            aff = ar.alloc(NT * NE, F32, "aff")
            aff3 = aff.rearrange("p (t e) -> p t e", t=NT)
            cmpb = ar.alloc(NT * NE, BF, "cmpb")
            cmp3 = cmpb.rearrange("p (t e) -> p t e", t=NT)
            mx = ar.alloc(NT, F32, "mx")
            lo = ar.alloc(32, F32, "lo")
            c32 = ar.alloc(32, F32, "c32")
            xc3 = X3

            for (t0, nt) in chunks:
                if t0 == 0 and not has_ctx:
                    continue
                dma("sp", X3[:, t0:t0 + nt, :], xmid_d[t0 * 128:(t0 + nt) * 128, :].rearrange("(n p) d -> p n d", p=128),
                    ["xmid"], ["xld%d" % t0])
            dma("pool", rwb3[:, :, :], rw_d[l].rearrange("(k p) e -> p k e", p=128), [], ["rwb"])
            if has_ctx:
                gbuild(G2[0], modcol(l, 5, NS), 7)
            gbuild(G2[1], modcol(l, 5, s), 7)
            for (t0, nt) in chunks:
                if t0 == 0 and not has_ctx:
                    continue
                xc3 = X3[:, t0:t0 + nt, :]
                keep = None
                if t0 >= 2:
                    keep = (lambda j, t0=t0: (xn_all[:, (t0 - 2 + j) * D:(t0 - 1 + j) * D], "xnall"))
                norm_T(t0, nt, 1, h2T3, t0 * 128, 0, keep=keep, xname="xld%d" % t0)
            xc3 = X3
            first = True
            for i in range(tlo, NT):
                for k in range(8):
                    mm(psum[2][:, i * NE:(i + 1) * NE], h2T3[:, k, i * 128:(i + 1) * 128], rwb3[:, k, :],
                       first, (i == NT - 1 and k == 7), ["hT", "rwb"], ["ps2"])
                    first = False
            T0 = tlo
            nT = NT - tlo
            lg3 = psum[2][:, T0 * NE:NT * NE].rearrange("p (t e) -> p t e", e=NE)
            a3 = aff3[:, T0:NT, :]
            dve(_R("tensor_reduce", out=mx[:, T0:NT], in_=lg3, axis=AX.X, op=ALU.max), ["ps2"], ["mx"])
            dve(_R("tensor_tensor", out=a3, in0=lg3, in1=mx[:, T0:NT].unsqueeze(2).broadcast_to([128, nT, NE]),
                                          op=ALU.subtract), ["ps2", "mx"], ["aff"])
            act(_R("activation", out=a3, in_=a3, func=AF.Exp), ["aff"], ["aff"])
            dve(_R("tensor_reduce", out=mx[:, T0:NT], in_=a3, axis=AX.X, op=ALU.add), ["aff"], ["mx"])
            dve(_R("reciprocal", out=mx[:, T0:NT], in_=mx[:, T0:NT]), ["mx"], ["mx"])
            dve(_R("tensor_tensor", out=a3, in0=a3, in1=mx[:, T0:NT].unsqueeze(2).broadcast_to([128, nT, NE]),
                                          op=ALU.mult), ["aff", "mx"], ["aff"])
            dve(_R("memset", lo[:, :], 0.0), [], ["lo"])
            segs = ([(0, 2, 0)] if has_ctx else []) + [(2, 16, 16)]
            for it in range(NBIS):
                hstep = 2.0 ** -(it + 1)
                for (ta, tn, lc) in segs:
                    dve(_R("scalar_tensor_tensor",
                        out=cmp3[:, ta:ta + tn, :], in0=aff3[:, ta:ta + tn, :], scalar=-hstep,
                        in1=lo[:, lc:lc + 16].unsqueeze(1).broadcast_to([128, tn, NE]), op0=ALU.add, op1=ALU.is_ge),
                        ["aff", "lo"], ["cmp"])
                mm(psum[3][:, T0 * NE:NT * NE], ones_b[:, :], cmpb[:, T0 * NE:NT * NE], True, True, ["cmp", "ones_b"], ["ps3"])
                for (ta, tn, lc) in segs:
                    dve(_R("tensor_reduce",
                        out=c32[:, lc:lc + 16],
                        in_=psum[3][:, ta * NE:(ta + tn) * NE].rearrange("p (t e) -> p e t", e=NE),
                        axis=AX.X, op=ALU.add), ["ps3"], ["c32"])
                lsl = slice(0 if has_ctx else 16, 32)
                dve(_R("tensor_tensor", out=c32[:, lsl], in0=c32[:, lsl], in1=kvec[:, lsl], op=ALU.is_ge),
                    ["c32", "kvec"], ["c32"])
                dve(_R("scalar_tensor_tensor",
                    out=lo[:, lsl], in0=c32[:, lsl], scalar=hstep, in1=lo[:, lsl], op0=ALU.mult, op1=ALU.add),
                    ["c32", "lo"], ["lo"])
            for (ta, tn, lc) in segs:
                dve(_R("tensor_tensor", out=maskf3[:, ta:ta + tn, :], in0=aff3[:, ta:ta + tn, :],
                       in1=lo[:, lc:lc + 16].unsqueeze(1).broadcast_to([128, tn, NE]), op=ALU.is_ge), ["aff", "lo"], ["maskf"])
            dve(_R("tensor_tensor", out=wsel3[:, T0:NT, :], in0=maskf3[:, T0:NT, :], in1=a3, op=ALU.mult),
                ["maskf", "aff"], ["wsel"])
            dve(_R("tensor_copy", out=maskb3[:, 2:NT, :], in_=maskf3[:, 2:NT, :]), ["maskf"], ["maskb"])
            firstr = True
            for li in range(16):
                mm(psum[3][:, li * NE:(li + 1) * NE], masks[:, 256:384], maskb3[:, 2 + li, :], firstr, False,
                   ["maskb", "masks"], ["ps3"])
                firstr = False
                for l2 in range(li):
                    mm(psum[3][:, li * NE:(li + 1) * NE], ones_b[:, :], maskb3[:, 2 + l2, :], False,
                       (li == 15 and l2 == li - 1), ["maskb", "ones_b"], ["ps3"])
            dve(_R("tensor_copy", out=rank[:, :], in_=psum[3][:, 0:16 * NE]), ["ps3"], ["rank"])
            if has_ctx:
                pool(_R("tensor_copy", out=h2c3, in_=h2T3[:, :, 0:256]), ["hT"], ["h2c"])
            dump("aff", aff[:, :])
            dump("wsel", wsel[:, :])

            P.barrier()
            ar.off = c2_off
            wgb = ar.alloc(8 * 512, BF, "wgb")
            wub = ar.alloc(8 * 512, BF, "wub")
            wdf = ar.alloc(D, F32, "wdf")
            wdb = ar.alloc(4 * D, BF, "wdb")
            wdc = ar.alloc(4 * D, BF, "wdc")
            Sel = ar.alloc(16 * 256, BF, "Sel")
            Sel3 = Sel.rearrange("p (t s) -> p t s", t=16)
            SelWT = ar.alloc(2 * 2048, BF, "SelWT")
            SelWT3 = SelWT.rearrange("p (c t) -> p c t", c=2)
            selw = [ar.alloc(256, BF) for _ in range(2)]
            xgT = ar.alloc(8 * 256, BF, "xgT")
            xgT3 = xgT.rearrange("p (k t) -> p k t", k=8)
            ysb = ar.alloc(2 * D, BF, "ysb")
            ysb3 = ysb.rearrange("p (c d) -> p c d", c=2)
            hid = ar.alloc(4 * 256, BF, "hid")
            hid3 = hid.rearrange("p (f t) -> p f t", f=4)
            sgb = ar.alloc(256, F32, "sgb")
            mx = ar.alloc(NT, F32, "mx2")
            junk = SelWT[:, 0:D]
            wg3 = wgb.rearrange("p (k f) -> p k f", k=8)
            wu3 = wub.rearrange("p (k f) -> p k f", k=8)
            wd3 = wdb.rearrange("p (f d) -> p f d", f=4)
            wdc3 = wdc.rearrange("p (f d) -> p f d", f=4)
            A2l = ABv(2, 1)
            B2l = ABv(3, 1)

            def ffn_hidden(rhs_of_k, n, extra=None):
                for fc in range(4):
                    pg = 2 * (fc % 2)
                    for k in range(8):
                        mm(psum[pg][:, 0:n], wg3[:, k, fc * 128:(fc + 1) * 128], rhs_of_k(k), k == 0, k == 7,
                           ["xgT", "h2c", "wg"], ["ps%d" % pg])
                    for k in range(8):
                        mm(psum[pg + 1][:, 0:n], wu3[:, k, fc * 128:(fc + 1) * 128], rhs_of_k(k), k == 0, k == 7,
                           ["xgT", "h2c", "wu"], ["ps%d" % (pg + 1)])
                    act(_R("activation", out=sgb[:, 0:n], in_=psum[pg][:, 0:n], func=AF.Silu), ["ps%d" % pg], ["sg0"])
                    dve(_R("tensor_tensor", out=hid3[:, fc, 0:n], in0=sgb[:, 0:n], in1=psum[pg + 1][:, 0:n], op=ALU.mult),
                        ["sg0", "ps%d" % (pg + 1)], ["hid"])
                    if extra is not None:
                        extra(fc)

            srot = [0]

            def load_weights(ex):
                dma("pool", wg3, wg_d[l, ex].rearrange("(k p) f -> p k f", p=128), [], ["wg"])
                dma("pool", wu3, wu_d[l, ex].rearrange("(k p) f -> p k f", p=128), [], ["wu"])
                for fc in range(4):
                    dma("sp", wdf[:, :], wd_d[l, ex, fc * 128:(fc + 1) * 128, :], [], ["wdf"])
                    pool(_R("tensor_tensor", out=wdb[:, fc * D:(fc + 1) * D], in0=wdf[:, :], in1=G2[1][:, :], op=ALU.mult),
                         ["wdf", "gb"], ["wd"])
                    if has_ctx:
                        pool(_R("tensor_tensor", out=wdc[:, fc * D:(fc + 1) * D], in0=wdf[:, :], in1=G2[0][:, :],
                                op=ALU.mult), ["wdf", "gb"], ["wdc"])

            def build_sel(ex, lis):
                for li in lis:
                    dve(_R("tensor_scalar", out=Sel3[:, li, :], in0=iota_f[:, :], scalar1=rank3[:, li, ex:ex + 1],
                           scalar2=maskf3[:, 2 + li, ex:ex + 1], op0=ALU.is_equal, op1=ALU.mult),
                        ["iota_f", "rank", "maskf"], ["Sel"])

            def ctx_dense(ex):
                ffn_hidden(lambda k: h2c3[:, k, 0:256], 256)
                for j in range(2):
                    for dh in range(2):
                        py = 4 + ((j * 2 + dh) % 2)
                        for fc in range(4):
                            mm(psum[py][:, :], hid3[:, fc, j * 128:(j + 1) * 128], wdc3[:, fc, dh * 512:(dh + 1) * 512],
                               fc == 0, fc == 3, ["hid", "wdc"], ["ps%d" % py])
                        dve(_R("scalar_tensor_tensor", out=X3[:, j, dh * 512:(dh + 1) * 512], in0=psum[py][:, :],
                               scalar=wsel3[:, j, ex:ex + 1], in1=X3[:, j, dh * 512:(dh + 1) * 512],
                               op0=ALU.mult, op1=ALU.add), ["ps%d" % py, "wsel", "xc"], ["xc"])

            def gather_k(ex, k):
                pb = k % 2
                for li in range(16):
                    mm(psum[pb][:, 0:256], xn_all[:, li * D + k * 128:li * D + (k + 1) * 128], Sel3[:, li, :],
                       li == 0, li == 15, ["xnall", "Sel"], ["ps%d" % pb])
                act(_R("activation", out=xgT3[:, k, :], in_=psum[pb][:, 0:256], func=AF.Identity,
                       scale=A2l[:, k:k + 1], bias=B2l[:, k:k + 1]), ["ps%d" % pb, "AB"], ["xgT"])

            def down_proj(ex):
                for sc in range(2):
                    for dh in range(2):
                        py = 4 + ((sc * 2 + dh) % 2)
                        for fc in range(4):
                            mm(psum[py][:, :], hid3[:, fc, sc * 128:(sc + 1) * 128], wd3[:, fc, dh * 512:(dh + 1) * 512],
                               fc == 0, fc == 3, ["hid", "wd"], ["ps%d" % py])
                        act(_R("copy", out=ysb3[:, sc, dh * 512:(dh + 1) * 512], in_=psum[py][:, :]), ["ps%d" % py], ["ysb"])

            def selw_tr(ex, li):
                sw = selw[li % 2]
                swn = "selw%d" % (li % 2)
                dve(_R("tensor_scalar", out=sw[:, :], in0=iota_f[:, :], scalar1=rank3[:, li, ex:ex + 1],
                       scalar2=wsel3[:, 2 + li, ex:ex + 1], op0=ALU.is_equal, op1=ALU.mult),
                    ["iota_f", "rank", "wsel"], [swn])
                for sc in range(2):
                    tr(psb(6)[:, ((li % 4) * 2 + sc) * 128:((li % 4) * 2 + sc + 1) * 128], sw[:, sc * 128:(sc + 1) * 128],
                       ident_b[:, :], [swn, "ident_b"], ["ps6"])
                if li % 4 == 3:
                    for sc in range(2):
                        act(_R("copy", out=SelWT3[:, sc, (li - 3) * 128:(li + 1) * 128].rearrange("p (i t) -> p i t", i=4),
                               in_=psb(6)[:, :].rearrange("p (i c t) -> p i c t", i=4, c=2)[:, :, sc, :]),
                            ["ps6"], ["SelWT"])

            def scatter_tile(ex, li):
                for dh in range(2):
                    py = (7, 3)[srot[0] % 2]
                    srot[0] += 1
                    for sc in range(2):
                        mm(psum[py][:, :], SelWT3[:, sc, li * 128:(li + 1) * 128], ysb3[:, sc, dh * 512:(dh + 1) * 512],
                           sc == 0, sc == 1, ["SelWT", "ysb"], ["ps%d" % py])
                    dve(_R("tensor_tensor", out=X3[:, 2 + li, dh * 512:(dh + 1) * 512], in0=psum[py][:, :],
                           in1=X3[:, 2 + li, dh * 512:(dh + 1) * 512], op=ALU.add), ["ps%d" % py, "xc"], ["xc"])

            load_weights(0)
            build_sel(0, range(16))
            for ex in range(NE):
                for k in range(8):
                    gather_k(ex, k)
                    if ex > 0:
                        scatter_tile(ex - 1, 2 * k)
                        scatter_tile(ex - 1, 2 * k + 1)
                if has_ctx:
                    ctx_dense(ex)
                ffn_hidden(lambda k: xgT3[:, k, :], 256,
                           extra=(lambda fc, ex=ex: build_sel(ex + 1, range(4 * fc, 4 * fc + 4))) if ex + 1 < NE else None)
                down_proj(ex)
                if ex + 1 < NE:
                    load_weights(ex + 1)
                for li in range(16):
                    selw_tr(ex, li)
            for li in range(16):
                scatter_tile(NE - 1, li)
            if l == 0:
                dma("sp", xl1_d.rearrange("(n p) d -> p n d", p=128), X3[:, :, :], ["xc"], ["xsrc"])
            else:
                dve(_R("memset", mx[:, :], 0.0), [], ["mx"])
                for i in range(2, NT):
                    act(_R("activation", out=junk[:, :], in_=X3[:, i, :], func=AF.Square, scale=1.0 / 32.0,
                                                    accum_out=mx[:, i:i + 1]), ["xc", "mx"], ["junk", "mxa%d" % i])
                rsq(mx[:, 2:NT], ["mx"] + ["mxa%d" % i for i in range(2, NT)], ["mx"])
                for i in range(2, NT):
                    dve(_R("scalar_tensor_tensor", out=X3[:, i, :], in0=X3[:, i, :], scalar=mx[:, i:i + 1], in1=fgb[:, :],
                           op0=ALU.mult, op1=ALU.mult), ["xc", "mx", "fgb"], ["xc"])
                dma("sp", out_d[s].rearrange("(n p) d -> p n d", p=128), X3[:, 2:NT, :], ["xc"], ["out"])
            P.barrier()

    P.barrier()
    P.emit(nc, stack)
    stack.close()
    return nc


def _consts():
    ident = np.eye(128, dtype=np.float32)
    k = np.arange(128)[:, None]
    q = np.arange(128)[None, :]
    masks = np.concatenate([(k >= q), (k <= q), (k < q)], axis=1).astype(np.float32)
    rows = SEQ // 64
    row = np.repeat(np.arange(rows, dtype=np.float32), 64)
    col = np.tile(np.arange(64, dtype=np.float32), rows)

    def table(rot):
        nf = rot // 4
        inv = (np.float32(10000.0) ** (-np.arange(nf, dtype=np.float32) / np.float32(nf))).astype(np.float32)
        ang = np.concatenate([row[:, None] * inv, col[:, None] * inv], axis=-1).astype(np.float32)
        cs, sn = np.cos(ang), np.sin(ang)
        t = np.concatenate([cs, cs, sn, sn], axis=-1).astype(np.float32)
        return np.ascontiguousarray(t.reshape(16, 128, 2 * rot).transpose(1, 0, 2).reshape(128, 32 * rot))

    iota = np.ascontiguousarray(np.broadcast_to(np.arange(256, dtype=np.float32)[None, :], (128, 256)))
    return ident, masks, table(32), table(64), iota


def kernel(**inputs):
    NS = 4
    ident, masks, ropem, ropes, iota = _consts()
    nc = build(NS=NS, NL=2)
    f = lambda a: np.ascontiguousarray(np.asarray(a, dtype=np.float32))
    shared = {k: f(inputs[k]) for k in ("ada_w", "ada_b", "norm1_g", "w_in", "mla_q_norm_g", "mla_w_uq",
                                        "mla_kv_norm_g", "mla_w_ukv", "swa_sink", "conv_w", "w_o", "norm2_g",
                                        "router_w", "exp_w_gate", "exp_w_up", "exp_w_down", "final_norm_g")}
    shared.update(k_ident=ident, k_masks=masks, k_ropem=ropem, k_ropes=ropes, k_iota=iota)
    x = f(inputs["x"])
    ctx = f(inputs["ctx"])
    c = f(inputs["c"])
    c_ctx = f(inputs["c_ctx"])
    in_maps = []
    for core in range(N_CORES):
        sl = slice(core * NS, (core + 1) * NS)
        m = dict(shared)
        m["x"] = np.ascontiguousarray(x[sl])
        m["ctx"] = np.ascontiguousarray(ctx[sl])
        m["cc"] = np.ascontiguousarray(np.concatenate([c[sl], c_ctx[None, :]], axis=0))
        in_maps.append(m)
    res = run_bass_kernel_spmd(nc, in_maps, core_ids=list(range(N_CORES)))
    return np.concatenate([r["out"] for r in res.results], axis=0)
```
